# Optimizing a Trainium2 kernel written in Bass

```python
import numpy as np
import jax
import jax.numpy as jnp
from jax import lax

D_MODEL = 1024
BATCH = 8
SEQ = 2048
DEPTH = 4

MEM_LEN = 256
HEAD_DIM = 64
N_MIXERS = 4
MIX_W = 256
Q_BLOCK = 128

NSA_HEADS = 4
NSA_KV_HEADS = 2
NSA_GROUP = NSA_HEADS // NSA_KV_HEADS
CMP_LEN = 32
CMP_STRIDE = 16
CMP_HID = 256
SEL_BLOCK = 64
SEL_TOPK = 8
WINDOW = 512
FORCE_SCORE = 1e4

SB_HEADS = 4

RNN_W = 256
RNN_BLOCKS = 4
RNN_BW = RNN_W // RNN_BLOCKS
CONV_W = 4
LRU_C = 8.0

MLA_HEADS = 4
MLA_Q_RANK = 192
MLA_KV_RANK = 128
MLA_NOPE = 64
MLA_ROPE = 32
MLA_V = 64
ROPE_THETA = 10000.0

X_HEADS = 4
X_HEAD_DIM = 128

N_GROUPS = 4
EXPERTS_PER_GROUP = 8
N_EXPERTS = N_GROUPS * EXPERTS_PER_GROUP
TOPK_IN_GROUP = 2
D_EXPERT = 512
EXPERT_CHUNK = 256

DN_ALPHA = (2.0 * DEPTH) ** 0.25
DN_BETA = (8.0 * DEPTH) ** -0.25
LN_EPS = 1e-5
RMS_EPS = 1e-6

IN_SPLITS = ((NSA_HEADS * HEAD_DIM,) + (NSA_KV_HEADS * HEAD_DIM,) * 6 + (NSA_HEADS * 3,)
             + (SB_HEADS * HEAD_DIM,) * 3
             + (RNN_W, RNN_W)
             + (MLA_Q_RANK, MLA_KV_RANK, MLA_ROPE)
             + (N_MIXERS * D_MODEL,))
IN_OFFSETS = tuple(int(o) for o in np.concatenate([[0], np.cumsum(IN_SPLITS)[:-1]]))
N_IN = sum(IN_SPLITS)

kernel_name = 'hybrid_nsa_sb_rglru_mla_hmoe_deepnorm'


def layer_norm(x, g, b):
    xf = x.astype(jnp.float32)
    mu = jnp.mean(xf, -1, keepdims=True)
    var = jnp.mean(jnp.square(xf - mu), -1, keepdims=True)
    return ((xf - mu) * lax.rsqrt(var + LN_EPS) * g + b).astype(x.dtype)


def rms_norm(x, g):
    xf = x.astype(jnp.float32)
    return (xf * lax.rsqrt(jnp.mean(jnp.square(xf), -1, keepdims=True) + RMS_EPS) * g).astype(x.dtype)


def masked_softmax(s, mask):
    s = jnp.where(mask, s.astype(jnp.float32), -jnp.inf)
    m = jnp.max(s, -1, keepdims=True)
    p = jnp.exp(s - jnp.where(jnp.isfinite(m), m, 0.0))
    return p / jnp.maximum(jnp.sum(p, -1, keepdims=True), 1e-30)


def alibi_slopes(n):
    return 2.0 ** (-8.0 * jnp.arange(1, n + 1, dtype=jnp.float32) / n)


def rope(x, pos):
    d = x.shape[-1]
    inv = ROPE_THETA ** (-jnp.arange(0, d, 2, dtype=jnp.float32) / d)
    ang = pos.astype(jnp.float32)[:, None] * inv[None, :]
    cos, sin = jnp.cos(ang)[:, None, :], jnp.sin(ang)[:, None, :]
    xf = x.astype(jnp.float32)
    x1, x2 = xf[..., : d // 2], xf[..., d // 2:]
    return jnp.concatenate([x1 * cos - x2 * sin, x1 * sin + x2 * cos], -1).astype(x.dtype)


def q_blocks(t):
    b, s = t.shape[:2]
    return jnp.moveaxis(t.reshape(b, s // Q_BLOCK, Q_BLOCK, *t.shape[2:]), 1, 0)


def unblock(t):
    nq, b, q = t.shape[:3]
    return jnp.moveaxis(t, 0, 1).reshape(b, nq * q, *t.shape[3:])


def nsa_attention(q, k_cmp, v_cmp, k_sel, v_sel, k_win, v_win, gate_logits, cmp_pos, cmp_w1, cmp_w2):
    B, S, H, dh = q.shape
    G = k_cmp.shape[2]
    f32 = jnp.float32
    scale = dh ** -0.5
    slopes = alibi_slopes(H).reshape(G, NSA_GROUP)
    t_pos = jnp.arange(S)
    nq = S // Q_BLOCK
    qpos = t_pos.reshape(nq, Q_BLOCK)
    qg = q.reshape(B, S, G, NSA_GROUP, dh)

    n_cmp = (S - CMP_LEN) // CMP_STRIDE + 1
    blk_idx = jnp.asarray(np.arange(n_cmp)[:, None] * CMP_STRIDE + np.arange(CMP_LEN)[None, :], jnp.int32)

    def compress(t, j):
        tb = t[:, blk_idx] + cmp_pos[j][None, None, :, None, :]
        tb = jnp.moveaxis(tb, 3, 2).reshape(B, n_cmp, G, CMP_LEN * dh)
        hid = jax.nn.gelu(jnp.einsum('bcgf,fh->bcgh', tb, cmp_w1[j]))
        return jnp.einsum('bcgh,hd->bcgd', hid, cmp_w2[j])

    kc, vc = compress(k_cmp, 0), compress(v_cmp, 1)
    blk_end = jnp.arange(n_cmp) * CMP_STRIDE + CMP_LEN - 1
    dist_c = (t_pos[:, None] - blk_end[None, :]).astype(f32)
    s_c = jnp.einsum('bsgnd,bcgd->bgnsc', qg, kc).astype(f32) * scale - slopes[None, :, :, None, None] * dist_c
    p_c = masked_softmax(s_c, dist_c >= 0)
    o_c = jnp.einsum('bgnsc,bcgd->bsgnd', p_c.astype(vc.dtype), vc)

    n_sel = S // SEL_BLOCK
    c0 = np.arange(n_cmp)[:, None] * CMP_STRIDE
    j0 = np.arange(n_sel)[None, :] * SEL_BLOCK
    cover = np.clip(np.minimum(c0 + CMP_LEN, j0 + SEL_BLOCK) - np.maximum(c0, j0), 0, None) / CMP_LEN
    imp = jnp.einsum('bgnsc,cj->bgsj', p_c, jnp.asarray(cover, f32))
    blk = jnp.arange(n_sel)[None, :]
    forced = (blk == 0) | (blk == (t_pos // SEL_BLOCK)[:, None])
    valid = blk * SEL_BLOCK <= t_pos[:, None]
    imp = jnp.where(forced, FORCE_SCORE, jnp.where(valid, imp, -1.0))
    n_top = min(SEL_TOPK, n_sel)
    _, sel_idx = lax.top_k(imp, n_top)

    kbt = jnp.moveaxis(k_sel.reshape(B, n_sel, SEL_BLOCK, G, dh), 3, 1)
    vbt = jnp.moveaxis(v_sel.reshape(B, n_sel, SEL_BLOCK, G, dh), 3, 1)
    bi = jnp.arange(B)[:, None, None, None]
    gi = jnp.arange(G)[None, :, None, None]
    n_keys = n_top * SEL_BLOCK

    def selected_block(args):
        qb, idx, qp = args
        kb = kbt[bi, gi, idx].reshape(B, G, Q_BLOCK, n_keys, dh)
        vb = vbt[bi, gi, idx].reshape(B, G, Q_BLOCK, n_keys, dh)
        kpos = (idx[..., None] * SEL_BLOCK + jnp.arange(SEL_BLOCK)).reshape(B, G, Q_BLOCK, n_keys)
        dist = (qp[None, None, :, None] - kpos).astype(f32)[:, :, None]
        s = jnp.einsum('bqgnd,bgqkd->bgnqk', qb, kb).astype(f32) * scale - slopes[None, :, :, None, None] * dist
        p = masked_softmax(s, dist >= 0)
        return jnp.einsum('bgnqk,bgqkd->bqgnd', p.astype(vb.dtype), vb)

    sel_blocks = jnp.moveaxis(sel_idx.reshape(B, G, nq, Q_BLOCK, n_top), 2, 0)
    o_s = unblock(lax.map(selected_block, (q_blocks(qg), sel_blocks, qpos)))

    band = Q_BLOCK + WINDOW
    band_np = np.arange(nq)[:, None] * Q_BLOCK + np.arange(band)[None, :]
    band_idx = jnp.asarray(band_np, jnp.int32)
    kpos_w = jnp.asarray(band_np - WINDOW, jnp.int32)
    pad = ((0, 0), (WINDOW, 0), (0, 0), (0, 0))
    kw = jnp.pad(k_win, pad)[:, band_idx]
    vw = jnp.pad(v_win, pad)[:, band_idx]
    dist_w = qpos[:, :, None] - kpos_w[:, None, :]
    mask_w = (dist_w >= 0) & (dist_w < WINDOW) & (kpos_w[:, None, :] >= 0)
    qw = qg.reshape(B, nq, Q_BLOCK, G, NSA_GROUP, dh)
    s_w = (jnp.einsum('bqtgnd,bqkgd->bgnqtk', qw, kw).astype(f32) * scale
           - slopes[None, :, :, None, None, None] * dist_w.astype(f32))
    p_w = masked_softmax(s_w, mask_w)
    o_w = jnp.einsum('bgnqtk,bqkgd->bqtgnd', p_w.astype(vw.dtype), vw).reshape(B, S, G, NSA_GROUP, dh)

    g = jax.nn.sigmoid(gate_logits.astype(f32)).reshape(B, S, G, NSA_GROUP, 3).astype(q.dtype)
    o = g[..., 0:1] * o_c + g[..., 1:2] * o_s + g[..., 2:3] * o_w
    return o.reshape(B, S, H * dh)


def stick_breaking_attention(q, k, v):
    B, S, H, dh = q.shape
    scale = dh ** -0.5
    key_pos = jnp.arange(S)

    def block(args):
        qb, qp = args
        z = jnp.einsum('bqhd,bkhd->bhqk', qb, k).astype(jnp.float32) * scale
        strict = key_pos[None, :] < qp[:, None]
        log_1m = jnp.where(strict, jax.nn.log_sigmoid(-z), 0.0)
        tail = lax.cumsum(log_1m, axis=3, reverse=True) - log_1m
        a = jnp.where(strict, jnp.exp(jax.nn.log_sigmoid(z) + tail), 0.0)
        return jnp.einsum('bhqk,bkhd->bqhd', a.astype(v.dtype), v)

    o = unblock(lax.map(block, (q_blocks(q), key_pos.reshape(-1, Q_BLOCK))))
    return o.reshape(B, S, H * dh)


def rglru_block(xr, xg, conv_w, conv_b, ga_w, ga_b, gx_w, gx_b, lru_lambda):
    B, S, W = xr.shape
    f32 = jnp.float32
    xp = jnp.pad(xr, ((0, 0), (CONV_W - 1, 0), (0, 0)))
    u = conv_b + xp[:, 0:S] * conv_w[0]
    for tap in range(1, CONV_W):
        u = u + xp[:, tap:tap + S] * conv_w[tap]
    ub = u.reshape(B, S, RNN_BLOCKS, RNN_BW)
    r = jax.nn.sigmoid(jnp.einsum('bsnc,ncd->bsnd', ub, ga_w).reshape(B, S, W) + ga_b)
    i = jax.nn.sigmoid(jnp.einsum('bsnc,ncd->bsnd', ub, gx_w).reshape(B, S, W) + gx_b)
    log_a = -LRU_C * r.astype(f32) * jax.nn.softplus(-lru_lambda.astype(f32))
    a = jnp.exp(log_a)
    b_in = jnp.sqrt(-jnp.expm1(2.0 * log_a)) * (i * u).astype(f32)

    def combine(left, right):
        a1, b1 = left
        a2, b2 = right
        return a1 * a2, a2 * b1 + b2

    _, h = lax.associative_scan(combine, (a, b_in), axis=1)
    return h.astype(xr.dtype) * jax.nn.gelu(xg)


def mla_attention(c_q_raw, c_kv_raw, k_rope_raw, q_norm, kv_norm, w_uq, w_ukv, pos):
    B, S, _ = c_q_raw.shape
    q = jnp.einsum('bsr,rf->bsf', rms_norm(c_q_raw, q_norm), w_uq).reshape(B, S, MLA_HEADS, MLA_NOPE + MLA_ROPE)
    q_nope, q_rope = q[..., :MLA_NOPE], rope(q[..., MLA_NOPE:], pos)
    kv = jnp.einsum('bsr,rf->bsf', rms_norm(c_kv_raw, kv_norm), w_ukv).reshape(B, S, MLA_HEADS, MLA_NOPE + MLA_V)
    k_nope, v = kv[..., :MLA_NOPE], kv[..., MLA_NOPE:]
    k_rope = rope(k_rope_raw[:, :, None, :], pos)[:, :, 0]
    scale = (MLA_NOPE + MLA_ROPE) ** -0.5
    key_pos = jnp.arange(S)

    def block(args):
        qn, qr, qp = args
        s = (jnp.einsum('bqhd,bkhd->bhqk', qn, k_nope) + jnp.einsum('bqhd,bkd->bhqk', qr, k_rope)) * scale
        p = masked_softmax(s, key_pos[None, :] <= qp[:, None])
        return jnp.einsum('bhqk,bkhd->bqhd', p.astype(v.dtype), v)

    o = unblock(lax.map(block, (q_blocks(q_nope), q_blocks(q_rope), key_pos.reshape(-1, Q_BLOCK))))
    return o.reshape(B, S, MLA_HEADS * MLA_V)


def hybrid_mixer(x, pos, w_in, cmp_pos, cmp_w1, cmp_w2, conv_w, conv_b, ga_w, ga_b, gx_w, gx_b, lru_lambda,
                 q_norm, kv_norm, w_uq, w_ukv, w_branch, w_out):
    B, S, D = x.shape
    h = jnp.einsum('bsd,df->bsf', x, w_in)
    (a_q, a_kc, a_vc, a_ks, a_vs, a_kw, a_vw, a_g, b_q, b_k, b_v, c_x, c_g, d_cq, d_ckv, d_kr,
     merge_g) = [h[..., o:o + w] for o, w in zip(IN_OFFSETS, IN_SPLITS)]

    def heads(t):
        return t.reshape(B, S, -1, HEAD_DIM)

    o_a = nsa_attention(heads(a_q), heads(a_kc), heads(a_vc), heads(a_ks), heads(a_vs), heads(a_kw), heads(a_vw),
                        a_g.reshape(B, S, NSA_HEADS, 3), cmp_pos, cmp_w1, cmp_w2)
    o_b = stick_breaking_attention(heads(b_q), heads(b_k), heads(b_v))
    o_c = rglru_block(c_x, c_g, conv_w, conv_b, ga_w, ga_b, gx_w, gx_b, lru_lambda)
    o_d = mla_attention(d_cq, d_ckv, d_kr, q_norm, kv_norm, w_uq, w_ukv, pos)
    branches = jnp.stack([o_a, o_b, o_c, o_d], axis=2)
    up = jnp.einsum('bsnc,ncd->bsnd', branches, w_branch)
    gates = jax.nn.sigmoid(merge_g.reshape(B, S, N_MIXERS, D))
    return jnp.einsum('bsd,de->bse', jnp.sum(gates * up, axis=2), w_out)


def memory_cross_attention(x, mem, wq, wkv, wo):
    B, S, _ = x.shape
    M = mem.shape[1]
    F = X_HEADS * X_HEAD_DIM
    q = jnp.einsum('bsd,df->bsf', x, wq).reshape(B, S, X_HEADS, X_HEAD_DIM)
    kv = jnp.einsum('bmd,df->bmf', mem, wkv)
    k = kv[..., :F].reshape(B, M, X_HEADS, X_HEAD_DIM)
    v = kv[..., F:].reshape(B, M, X_HEADS, X_HEAD_DIM)
    s = jnp.einsum('bshd,bmhd->bhsm', q, k).astype(jnp.float32) * X_HEAD_DIM ** -0.5
    p = jax.nn.softmax(s, axis=-1).astype(v.dtype)
    o = jnp.einsum('bhsm,bmhd->bshd', p, v).reshape(B, S, F)
    return jnp.einsum('bsf,fd->bsd', o, wo)


def grouped_expert_ffn(xt, expert_idx, weights, w_gu, w_down):
    N, D = xt.shape
    K = expert_idx.shape[1]
    E = w_gu.shape[0]
    F = w_down.shape[1]
    C = EXPERT_CHUNK
    A = N * K
    flat_e = expert_idx.reshape(A)
    flat_tok = jnp.arange(A, dtype=jnp.int32) // K
    order = jnp.argsort(flat_e)
    se, st = flat_e[order], flat_tok[order]
    counts = jax.ops.segment_sum(jnp.ones((A,), jnp.int32), flat_e, num_segments=E)
    padded = (counts + C - 1) // C * C
    pad_end = jnp.cumsum(padded)
    pad_start = pad_end - padded
    start = jnp.cumsum(counts) - counts
    dest = pad_start[se] + jnp.arange(A, dtype=jnp.int32) - start[se]
    n_chunks = -(-(A + E * (C - 1)) // C)
    P = n_chunks * C
    slot_tok = jnp.full((P,), N, jnp.int32).at[dest].set(st)
    x_pad = jnp.concatenate([xt, jnp.zeros((1, D), xt.dtype)], axis=0)
    xb = x_pad[slot_tok].reshape(n_chunks, C, D)
    chunk_start = jnp.arange(n_chunks, dtype=jnp.int32) * C
    chunk_e = jnp.minimum(jnp.sum((pad_end[None, :] <= chunk_start[:, None]).astype(jnp.int32), axis=1), E - 1)

    def run(args):
        xc, e = args
        gu = xc @ w_gu[e]
        return (jax.nn.silu(gu[:, :F]) * gu[:, F:]) @ w_down[e]

    yb = lax.map(run, (xb, chunk_e)).reshape(P, D)
    contrib = yb[dest] * weights.reshape(A)[order][:, None].astype(yb.dtype)
    return jnp.zeros((N, D), yb.dtype).at[st].add(contrib)


def hier_moe(x, rg_w, rg_b, re_w, re_b, w_gu, w_down):
    B, S, D = x.shape
    xt = x.reshape(B * S, D)
    N = xt.shape[0]
    rows = jnp.arange(N)
    p_grp = jax.nn.softmax((xt @ rg_w).astype(jnp.float32) + rg_b, axis=-1)
    grp = jnp.argmax(p_grp, axis=-1).astype(jnp.int32)
    p_g = p_grp[rows, grp]
    e_logits = ((xt @ re_w).astype(jnp.float32) + re_b).reshape(N, N_GROUPS, EXPERTS_PER_GROUP)
    p_e = jax.nn.softmax(e_logits[rows, grp], axis=-1)
    top_p, top_i = lax.top_k(p_e, TOPK_IN_GROUP)
    w = p_g[:, None] * top_p / jnp.sum(top_p, -1, keepdims=True)
    expert_idx = (grp[:, None] * EXPERTS_PER_GROUP + top_i).astype(jnp.int32)
    return grouped_expert_ffn(xt, expert_idx, w, w_gu, w_down).reshape(B, S, D)


def setup_inputs(seed: int = 0) -> dict:
    key = jax.random.key(seed)
    keys = jax.random.split(key, 48)
    ks = iter([keys[i] for i in range(48)])
    f32 = jnp.float32
    L = DEPTH

    def nrm(shape, scale):
        return jax.random.normal(next(ks), shape, f32) * scale

    def gain(n):
        return 1.0 + nrm((L, n), 0.02)

    u = jax.random.uniform(next(ks), (L, RNN_W), f32, 0.9, 0.999)
    a0 = u ** (1.0 / LRU_C)
    return {
        'x': nrm((BATCH, SEQ, D_MODEL), 1.0),
        'mem': nrm((BATCH, MEM_LEN, D_MODEL), 1.0),
        'w_in': nrm((L, D_MODEL, N_IN), D_MODEL ** -0.5),
        'nsa_cmp_pos': nrm((L, 2, CMP_LEN, HEAD_DIM), 0.1),
        'nsa_cmp_w1': nrm((L, 2, CMP_LEN * HEAD_DIM, CMP_HID), (CMP_LEN * HEAD_DIM) ** -0.5),
        'nsa_cmp_w2': nrm((L, 2, CMP_HID, HEAD_DIM), CMP_HID ** -0.5),
        'rnn_conv_w': nrm((L, CONV_W, RNN_W), CONV_W ** -0.5),
        'rnn_conv_b': nrm((L, RNN_W), 0.01),
        'rnn_ga_w': nrm((L, RNN_BLOCKS, RNN_BW, RNN_BW), RNN_BW ** -0.5),
        'rnn_ga_b': nrm((L, RNN_W), 0.01),
        'rnn_gx_w': nrm((L, RNN_BLOCKS, RNN_BW, RNN_BW), RNN_BW ** -0.5),
        'rnn_gx_b': nrm((L, RNN_W), 0.01),
        'rnn_lambda': jnp.log(a0) - jnp.log1p(-a0),
        'mla_q_norm': gain(MLA_Q_RANK),
        'mla_kv_norm': gain(MLA_KV_RANK),
        'mla_w_uq': nrm((L, MLA_Q_RANK, MLA_HEADS * (MLA_NOPE + MLA_ROPE)), MLA_Q_RANK ** -0.5),
        'mla_w_ukv': nrm((L, MLA_KV_RANK, MLA_HEADS * (MLA_NOPE + MLA_V)), MLA_KV_RANK ** -0.5),
        'w_branch': nrm((L, N_MIXERS, MIX_W, D_MODEL), MIX_W ** -0.5),
        'w_out': nrm((L, D_MODEL, D_MODEL), DN_BETA * D_MODEL ** -0.5),
        'ln1_g': gain(D_MODEL),
        'ln1_b': nrm((L, D_MODEL), 0.02),
        'x_wq': nrm((L, D_MODEL, X_HEADS * X_HEAD_DIM), D_MODEL ** -0.5),
        'x_wkv': nrm((L, D_MODEL, 2 * X_HEADS * X_HEAD_DIM), D_MODEL ** -0.5),
        'x_wo': nrm((L, X_HEADS * X_HEAD_DIM, D_MODEL), DN_BETA * (X_HEADS * X_HEAD_DIM) ** -0.5),
        'ln2_g': gain(D_MODEL),
        'ln2_b': nrm((L, D_MODEL), 0.02),
        'moe_rg_w': nrm((L, D_MODEL, N_GROUPS), D_MODEL ** -0.5),
        'moe_rg_b': nrm((L, N_GROUPS), 0.01),
        'moe_re_w': nrm((L, D_MODEL, N_EXPERTS), D_MODEL ** -0.5),
        'moe_re_b': nrm((L, N_EXPERTS), 0.01),
        'moe_w_gu': nrm((L, N_EXPERTS, D_MODEL, 2 * D_EXPERT), D_MODEL ** -0.5),
        'moe_w_down': nrm((L, N_EXPERTS, D_EXPERT, D_MODEL), DN_BETA * D_EXPERT ** -0.5),
        'ln3_g': gain(D_MODEL),
        'ln3_b': nrm((L, D_MODEL), 0.02),
    }


def reference(x, mem, w_in, nsa_cmp_pos, nsa_cmp_w1, nsa_cmp_w2, rnn_conv_w, rnn_conv_b, rnn_ga_w, rnn_ga_b,
              rnn_gx_w, rnn_gx_b, rnn_lambda, mla_q_norm, mla_kv_norm, mla_w_uq, mla_w_ukv, w_branch, w_out,
              ln1_g, ln1_b, x_wq, x_wkv, x_wo, ln2_g, ln2_b, moe_rg_w, moe_rg_b, moe_re_w, moe_re_b,
              moe_w_gu, moe_w_down, ln3_g, ln3_b):
    pos = jnp.arange(x.shape[1])
    for l in range(DEPTH):
        y = hybrid_mixer(x, pos, w_in[l], nsa_cmp_pos[l], nsa_cmp_w1[l], nsa_cmp_w2[l], rnn_conv_w[l], rnn_conv_b[l],
                         rnn_ga_w[l], rnn_ga_b[l], rnn_gx_w[l], rnn_gx_b[l], rnn_lambda[l], mla_q_norm[l],
                         mla_kv_norm[l], mla_w_uq[l], mla_w_ukv[l], w_branch[l], w_out[l])
        x = layer_norm(DN_ALPHA * x + y, ln1_g[l], ln1_b[l])
        y = memory_cross_attention(x, mem, x_wq[l], x_wkv[l], x_wo[l])
        x = layer_norm(DN_ALPHA * x + y, ln2_g[l], ln2_b[l])
        y = hier_moe(x, moe_rg_w[l], moe_rg_b[l], moe_re_w[l], moe_re_b[l], moe_w_gu[l], moe_w_down[l])
        x = layer_norm(DN_ALPHA * x + y, ln3_g[l], ln3_b[l])
    return x
```

```python
import numpy as np
import contextlib
import concourse.bass as bass
import concourse.mybir as mybir
from concourse.bass_utils import run_bass_kernel_spmd

F32 = mybir.dt.float32
BF16 = mybir.dt.bfloat16
AF = mybir.ActivationFunctionType
ALU = mybir.AluOpType
AX = mybir.AxisListType
ENGS = ("tensor", "vector", "scalar", "gpsimd", "sync")
DSIZE = {F32: 4, BF16: 2}
NEG = -30000.0
S = 2048
DM = 1024
DN_ALPHA = (2.0 * 4) ** 0.25


class Sched:
    NSLOT = 4

    def __init__(self, nc):
        self.nc = nc
        self.ops = {e: [] for e in ENGS}
        self.ncomp = {e: 0 for e in ENGS}
        self.ndma = {e: 0 for e in ENGS}
        self.lastw = {}
        self.readers = {}
        self.synced = {e: {} for e in ENGS}
        self.sb_off = 16640
        self.sb_stack = []
        self.uid = 0

    def sb(self, shape, dtype, name=None):
        self.uid += 1
        name = f"{name or 't'}_{self.uid}"
        nbytes = int(np.prod(shape[1:])) * DSIZE[dtype]
        off = (self.sb_off + 63) // 64 * 64
        assert off + nbytes <= 228000, f"SBUF overflow {name} {off + nbytes}"
        t = self.nc.alloc_sbuf_tensor_at(name, list(shape), dtype, offset=off)
        self.sb_off = off + nbytes
        return t

    def push(self):
        self.sb_stack.append(self.sb_off)

    def pop(self):
        self.barrier()
        self.sb_off = self.sb_stack.pop()

    def _need(self, E, dep, waits):
        key, val = dep
        if key == ("c", "tensor") and E == "tensor":
            return
        if self.synced[E].get(key, 0) >= val:
            return
        if waits.get(key, 0) < val:
            waits[key] = val

    def op(self, E, fn, reads=(), writes=(), dma=False):
        waits = {}
        for r in reads:
            lw = self.lastw.get(r)
            if lw is not None:
                self._need(E, lw, waits)
        for w in writes:
            lw = self.lastw.get(w)
            if lw is not None:
                self._need(E, lw, waits)
            for k, v in self.readers.get(w, {}).items():
                self._need(E, (k, v), waits)
        if dma:
            k = self.ndma[E]
            self.ndma[E] += 1
            key = ("d", E, k % self.NSLOT)
            val = 16 * (k // self.NSLOT + 1)
            if val > 16:
                self._need(E, (key, val - 16), waits)
        else:
            self.ncomp[E] += 1
            key = ("c", E)
            val = self.ncomp[E]
        for k_, v_ in waits.items():
            self.synced[E][k_] = v_
        me = (key, val)
        self.ops[E].append((fn, waits, me))
        for r in reads:
            d = self.readers.setdefault(r, {})
            if d.get(key, 0) < val:
                d[key] = val
        for w in writes:
            self.lastw[w] = me
            self.readers[w] = {}
        return me

    def barrier(self):
        state = {}
        for e in ENGS:
            if self.ncomp[e]:
                state[("c", e)] = self.ncomp[e]
            for sl in range(self.NSLOT):
                k = self.ndma[e]
                cnt = (k - sl + self.NSLOT - 1) // self.NSLOT if k > sl else 0
                if cnt:
                    state[("d", e, sl)] = 16 * cnt
        for e in ENGS:
            waits = {}
            for k_, v_ in state.items():
                if self.synced[e].get(k_, 0) < v_:
                    waits[k_] = v_
                    self.synced[e][k_] = v_
            if waits:
                self.ops[e].append((None, waits, None))
        self.lastw = {}
        self.readers = {}

    def emit(self):
        nc = self.nc
        sems = {}
        with contextlib.ExitStack() as st:
            for e in ENGS:
                sems[("c", e)] = st.enter_context(nc.semaphore(f"c_{e}"))
                for sl in range(self.NSLOT):
                    sems[("d", e, sl)] = st.enter_context(nc.semaphore(f"d_{e}_{sl}"))
            block = st.enter_context(nc.Block())

            def run(eng, name):
                for fn, waits, me in self.ops[name]:
                    for k_, v_ in waits.items():
                        eng.wait_ge(sems[k_], v_)
                    if fn is not None:
                        fn(eng).then_inc(sems[me[0]], 16 if me[0][0] == "d" else 1)

            @block.tensor
            def _(e):
                run(e, "tensor")

            @block.vector
            def _(e):
                run(e, "vector")

            @block.scalar
            def _(e):
                run(e, "scalar")

            @block.gpsimd
            def _(e):
                run(e, "gpsimd")

            @block.sync
            def _(e):
                run(e, "sync")


def host_consts():
    c = {}
    c["ident"] = np.eye(128, dtype=np.float32)
    t = np.arange(S)
    slopes = 2.0 ** (-2.0 * (np.arange(4) + 1))
    qaug = np.zeros((4, 4, S), np.float32)
    for h in range(4):
        qaug[h, 0] = -slopes[h] * 128 * (t // 128)
        qaug[h, 1] = -slopes[h] * (t % 128)
        qaug[h, 2] = slopes[h]
        qaug[h, 3] = slopes[h]
    c["qaug"] = qaug
    c["kaug"] = np.stack([np.ones(S), np.ones(S), 128.0 * (t // 128), (t % 128)]).astype(np.float32)
    be = np.arange(128) * 16 + 31
    c["kcaug"] = np.stack([np.ones(128), np.ones(128), 128.0 * (be // 128), (be % 128)]).astype(np.float32)
    k = np.arange(128)[:, None]
    q = np.arange(512)[None, :]
    masks = np.zeros((128, 12, 512), np.float32)
    for j in range(4):
        masks[:, j] = np.where(q >= 128 * j + k, 0.0, NEG)
        masks[:, 4 + j] = np.where(128 * j + k < q, 0.0, NEG)
        masks[:, 8 + j] = np.where(q < 128 * j + k, 0.0, NEG)
    c["masks"] = masks
    cc = np.arange(128)[:, None]
    c["cmask"] = np.where((t[None, :] >= 16 * cc + 31) & (cc < 127), 0.0, NEG).astype(np.float32)
    tt = t[:, None]
    j = np.arange(32)[None, :]
    forced = (j == 0) | (j == tt // 64)
    valid = j * 64 <= tt
    vm = (valid & ~forced).astype(np.float32)
    am = np.where(forced, 1e4, np.where(valid, 0.0, -1.0)).astype(np.float32)
    c["impvm"] = vm.reshape(16, 128, 32).transpose(1, 0, 2).copy()
    c["impam"] = am.reshape(16, 128, 32).transpose(1, 0, 2).copy()
    E = np.zeros((32, 16, 128), np.float32)
    for kc in range(16):
        for kk in range(128):
            E[2 * kc + kk // 64, kc, kk] = 1.0
    c["selE"] = E
    c0 = np.arange(128)[:, None] * 16
    j0 = np.arange(32)[None, :] * 64
    cover = np.clip(np.minimum(c0 + 32, j0 + 64) - np.maximum(c0, j0), 0, None) / 32.0
    cover[127] = 0.0
    c["cover"] = cover.astype(np.float32)
    inv = (10000.0 ** (-np.arange(0, 32, 2, dtype=np.float32) / 32)).astype(np.float32)
    ang = t.astype(np.float32)[:, None] * inv[None, :]
    cs, sn = np.cos(ang).astype(np.float32).T, np.sin(ang).astype(np.float32).T
    rc = np.zeros((96, S), np.float32)
    rs = np.zeros((96, S), np.float32)
    rc[64:80] = cs
    rc[80:96] = cs
    rs[64:80] = -sn
    rs[80:96] = sn
    c["ropec"] = rc
    c["ropes"] = rs
    jj = np.arange(128)[:, None]
    ss = np.arange(128)[None, :]
    c["negU"] = np.where(jj >= ss, -1.0, 0.0).astype(np.float32)
    sr = np.zeros((32, 32, 128), np.float32)
    for e_ in range(32):
        sr[e_, e_, :] = 1.0
    c["selrow"] = sr
    return c


WNAMES = ["w_in", "nsa_cmp_pos", "nsa_cmp_w1", "nsa_cmp_w2", "rnn_conv_w", "rnn_conv_b", "rnn_ga_w", "rnn_ga_b",
          "rnn_gx_w", "rnn_gx_b", "rnn_lambda", "mla_q_norm", "mla_kv_norm", "mla_w_uq", "mla_w_ukv", "w_branch",
          "w_out", "ln1_g", "ln1_b", "x_wq", "x_wkv", "x_wo", "ln2_g", "ln2_b", "moe_rg_w", "moe_rg_b", "moe_re_w",
          "moe_re_b", "moe_w_gu", "moe_w_down", "ln3_g", "ln3_b"]
WSHAPES = {"w_in": (1024, 6764), "nsa_cmp_pos": (2, 32, 64), "nsa_cmp_w1": (2, 2048, 256), "nsa_cmp_w2": (2, 256, 64),
           "rnn_conv_w": (4, 256), "rnn_conv_b": (256,), "rnn_ga_w": (4, 64, 64), "rnn_ga_b": (256,),
           "rnn_gx_w": (4, 64, 64), "rnn_gx_b": (256,), "rnn_lambda": (256,), "mla_q_norm": (192,),
           "mla_kv_norm": (128,), "mla_w_uq": (192, 384), "mla_w_ukv": (128, 512), "w_branch": (4, 256, 1024),
           "w_out": (1024, 1024), "ln1_g": (1024,), "ln1_b": (1024,), "x_wq": (1024, 512), "x_wkv": (1024, 1024),
           "x_wo": (512, 1024), "ln2_g": (1024,), "ln2_b": (1024,), "moe_rg_w": (1024, 4), "moe_rg_b": (4,),
           "moe_re_w": (1024, 32), "moe_re_b": (32,), "moe_w_gu": (32, 1024, 1024), "moe_w_down": (32, 512, 1024),
           "ln3_g": (1024,), "ln3_b": (1024,)}


def build(NL=4, stages=("mix", "cross", "moe"), dump=None):
    nc = bass.Bass("TRN2", target_bir_lowering=False)
    s = Sched(nc)
    D = {}

    def din(name, shape):
        D[name] = nc.dram_tensor(name, list(shape), F32, kind="ExternalInput").ap()
        return D[name]

    din("x", (S, DM))
    din("mem", (256, DM))
    for n in WNAMES:
        din(n, (NL,) + WSHAPES[n])
    HC = host_consts()
    for n, a in HC.items():
        din("c_" + n, a.shape)
    out_d = nc.dram_tensor("out", [S, DM], F32, kind="ExternalOutput").ap()
    xs = nc.dram_tensor("xs_scr", [S, DM], F32, kind="Internal").ap()
    dumps = {}

    PS = [nc.alloc_psum_tensor(f"ps{i}", [128, 512], F32) for i in range(8)]

    def V(fn, r=(), w=()):
        return s.op("vector", fn, r, w)

    def A(fn, r=(), w=()):
        return s.op("scalar", fn, r, w)

    def G(fn, r=(), w=()):
        return s.op("gpsimd", fn, r, w)

    def MM(out, lhsT, rhs, start, stop, r, w):
        return s.op("tensor", lambda e: e.matmul(out, lhsT=lhsT, rhs=rhs, start=start, stop=stop), r, w)

    def MMs(out, lhsT, rhs, start, stop, r, w):
        return s.op("tensor", lambda e: e.matmul(out, lhsT=lhsT, rhs=rhs, start=start, stop=stop, skip_group_check=True), r, w)

    def TR(out, in_, idn, r, w):
        return s.op("tensor", lambda e: e.transpose(out, in_, idn), r, w)

    def DMA(q, out, in_, r=(), w=()):
        return s.op(q, lambda e: e.dma_start(out=out, in_=in_), r, w, dma=True)

    def vcopy(out, in_, r, w):
        return V(lambda e: e.tensor_copy(out=out, in_=in_), r, w)

    def acopy(out, in_, r, w):
        return A(lambda e: e.activation(out=out, in_=in_, func=AF.Copy), r, w)

    def dump_sb(name, ap, shape):
        if dump is None or name not in dump:
            return
        d = nc.dram_tensor("dbg_" + name, list(shape), ap.dtype if hasattr(ap, "dtype") else F32, kind="ExternalOutput").ap()
        dumps[name] = d
        s.barrier()
        DMA("sync", d, ap)
        s.barrier()

    ident = s.sb([128, 128], F32, "ident")
    identb = s.sb([128, 128], BF16, "identb")
    xT = s.sb([128, 8, S], BF16, "xT")
    wbuf = None
    masks = None
    stat = s.sb([128, 2, 6], F32, "stat")
    mv = s.sb([128, 4], F32, "mv")
    DMA("sync", ident[:], D["c_ident"], w=["ident"])
    DMA("gpsimd", identb[:], D["c_ident"], w=["identb"])
    cnt = {"w": 0, "g": 0}

    def next_wbuf():
        cnt["w"] += 1
        return cnt["w"] % 3

    def gbank():
        cnt["g"] += 1
        return 4 + cnt["g"] % 2

    def xkeys(tq):
        return [("xT", 4 * tq + u) for u in range(4)]

    def transpose_tile(src, key, tt, f32dst=None, f32key=None):
        for half in range(2):
            b = 6 + half
            for j in range(4):
                c = half * 4 + j
                TR(PS[b][:, j * 128:(j + 1) * 128], src[:, c * 128:(c + 1) * 128], ident[:], [key, "ident"], [("ps", b)])
            vcopy(xT[:, half * 4:(half + 1) * 4, tt * 128:(tt + 1) * 128], PS[b][:].rearrange("p (j q) -> p j q", j=4),
                  [("ps", b)], [("xT", tt)])
            if f32dst is not None:
                acopy(f32dst[:, half * 4:(half + 1) * 4, :], PS[b][:].rearrange("p (j q) -> p j q", j=4), [("ps", b)], [f32key])

    def load_xT(src):
        s.push()
        xtile = [s.sb([128, DM], F32, f"xtile{i}") for i in range(2)]
        for tt in range(16):
            xt = xtile[tt % 2]
            DMA("sync", xt[:], src[tt * 128:(tt + 1) * 128, :], w=[("xtile", tt % 2)])
            transpose_tile(xt, ("xtile", tt % 2), tt)
        s.pop()

    def load_ln(l, gname, bname):
        DMA("sync", LN["g"][:], D[gname][l].partition_broadcast(128), w=["lng"])
        DMA("sync", LN["b"][:], D[bname][l].partition_broadcast(128), w=["lnb"])

    def ln_tile(xt, key, tt, dst, f32dst=None, f32key=None, do_T=True):
        for hf in range(2):
            V(lambda e, hf=hf: e.bn_stats(out=stat[:, hf, :], in_=xt[:, hf * 512:(hf + 1) * 512]), [key], ["stat"])
        V(lambda e: e.bn_aggr(out=mv[:, 0:2], in_=stat[:]), ["stat"], ["mv"])
        V(lambda e: e.tensor_scalar(out=mv[:, 3:4], in0=mv[:, 1:2], scalar1=1e-5, scalar2=None, op0=ALU.add), ["mv"], ["mv3"])
        A(lambda e: e.activation(out=mv[:, 3:4], in_=mv[:, 3:4], func=AF.Ln), ["mv3"], ["mv3"])
        A(lambda e: e.activation(out=mv[:, 2:3], in_=mv[:, 3:4], func=AF.Exp, scale=-0.5), ["mv3"], ["mv2"])
        V(lambda e: e.tensor_scalar(out=xt[:], in0=xt[:], scalar1=mv[:, 0:1], scalar2=mv[:, 2:3], op0=ALU.subtract,
                                    op1=ALU.mult), [key, "mv", "mv2"], [key])
        lg_, lb_ = LN["g"], LN["b"]
        G(lambda e: e.tensor_tensor(out=xt[:], in0=xt[:], in1=lg_[:], op=ALU.mult), [key, "lng"], [key])
        G(lambda e: e.tensor_tensor(out=xt[:], in0=xt[:], in1=lb_[:], op=ALU.add), [key, "lnb"], [key])
        DMA("sync", dst[tt * 128:(tt + 1) * 128, :], xt[:], r=[key], w=[("xs", tt)])
        if do_T:
            transpose_tile(xt, key, tt, f32dst, f32key)

    def proj_fm(pieces, m, evac, kchunks=8, rhsT=None, rkeys=None):
        i = next_wbuf()
        wb = wbuf[i]
        wk = []
        for pi, (o, ap) in enumerate(pieces):
            wd = ap.shape[1]
            DMA("gpsimd", wb[:, 0:kchunks, o:o + wd], ap.rearrange("(c p) m -> p c m", p=128), w=[("wbuf", i, pi)])
            wk.append(("wbuf", i, pi))
        for tq in range(4):
            b = gbank()
            for c in range(kchunks):
                MM(PS[b][0:m, :], wb[:, c, 0:m], xT[:, c, tq * 512:(tq + 1) * 512], c == 0, c == kchunks - 1,
                   wk + xkeys(tq), [("ps", b)])
            evac(PS[b], ("ps", b), tq)

    def proj_tm(pieces, n, evac):
        i = next_wbuf()
        wb = wbuf[i]
        wk = []
        for pi, (o, ap) in enumerate(pieces):
            wd = ap.shape[1]
            DMA("gpsimd", wb[:, :, o:o + wd], ap.rearrange("(c p) m -> p c m", p=128), w=[("wbuf", i, pi)])
            wk.append(("wbuf", i, pi))
        for tt in range(16):
            b = gbank()
            for c in range(8):
                MM(PS[b][:, 0:n], xT[:, c, tt * 128:(tt + 1) * 128], wb[:, c, 0:n], c == 0, c == 7,
                   wk + [("xT", tt)], [("ps", b)])
            evac(PS[b], ("ps", b), tt)

    ctr = {"sc": 0, "p": 0}
    AT = {}

    def attn(terms, kcs, vaug, ncols, pv_ok, accmap, scale=1.0):
        Pt = AT["P"]
        kcs = list(kcs)
        rng = {}
        for qs in range(4):
            ok = [k for k in kcs if pv_ok(k, qs)]
            rng[qs] = (ok[0], ok[-1])

        def score(kc):
            ctr["sc"] += 1
            sbk = ctr["sc"] % 2
            tl = terms(kc)
            for i, (lt, rh, rk) in enumerate(tl):
                MM(PS[sbk][:], lt, rh, i == 0, i == len(tl) - 1, rk, [("ps", sbk)])
            return sbk

        nxt = score(kcs[0])
        started = set()
        for idx, kc in enumerate(kcs):
            sbk = nxt
            ctr["p"] += 1
            pi = ctr["p"] % 3
            A(lambda e, pi=pi, sbk=sbk: e.activation(out=Pt[pi][:], in_=PS[sbk][:], func=AF.Exp, scale=scale),
              [("ps", sbk)], [("P", pi)])
            if idx + 1 < len(kcs):
                nxt = score(kcs[idx + 1])
            vap, vkey = vaug(kc)
            for qs in range(4):
                if not pv_ok(kc, qs):
                    continue
                bank, c0 = accmap(qs)
                first = bank not in started
                started.add(bank)
                MMs(PS[bank][:, c0:c0 + ncols], Pt[pi][:, qs * 128:(qs + 1) * 128], vap, first, kc == rng[qs][1],
                    [("P", pi), vkey], [("ps", bank)])

    def attn_out(accmap, dv, tt0, gate, dst, accumulate, normalize=True):
        rd = AT["rd"]
        for qs in range(4):
            bank, c0 = accmap(qs)
            tt = tt0 + qs
            src = PS[bank][:, c0:c0 + dv]
            if not normalize:
                vcopy(dst(tt), src, [("ps", bank)], [("oacc", tt)])
                continue
            V(lambda e, bank=bank, c0=c0: e.tensor_scalar(out=rd[:, 2:3], in0=PS[bank][:, c0 + dv:c0 + dv + 1], scalar1=1e-30,
                                                         scalar2=None, op0=ALU.max), [("ps", bank)], ["rd2"])
            V(lambda e: e.reciprocal(out=rd[:, 0:1], in_=rd[:, 2:3]), ["rd2"], ["rd0"])
            sc = rd[:, 0:1]
            rk = ["rd0"]
            if gate is not None:
                gap = gate(tt)
                V(lambda e, gap=gap: e.tensor_tensor(out=rd[:, 1:2], in0=rd[:, 0:1], in1=gap, op=ALU.mult), ["rd0", "gate"], ["rd1"])
                sc = rd[:, 1:2]
                rk = ["rd1"]
            d = dst(tt)
            if accumulate:
                V(lambda e, d=d, src=src, sc=sc: e.scalar_tensor_tensor(out=d, in0=src, scalar=sc, in1=d, op0=ALU.mult, op1=ALU.add),
                  [("ps", bank), ("oacc", tt)] + rk, [("oacc", tt)])
            else:
                V(lambda e, d=d, src=src, sc=sc: e.tensor_scalar(out=d, in0=src, scalar1=sc, scalar2=None, op0=ALU.mult),
                  [("ps", bank)] + rk, [("oacc", tt)])

    def to_fm(src, nchunk, dstT, dkey):
        for tt in range(16):
            b = 6 + tt % 2
            for c in range(nchunk):
                TR(PS[b][:, c * 128:(c + 1) * 128], src[:, tt, c * 128:(c + 1) * 128], ident[:], [("oacc", tt), "ident"], [("ps", b)])
            vcopy(dstT[:, 0:nchunk, tt * 128:(tt + 1) * 128], PS[b][:, 0:nchunk * 128].rearrange("p (j q) -> p j q", j=nchunk),
                  [("ps", b)], [(dkey, tt)])

    def tqs(tq):
        return slice(tq * 512, (tq + 1) * 512)

    def nsa(l, W, oacc):
        s.push()
        qa = [s.sb([68, S], BF16, f"qa{h}") for h in range(4)]
        kcA = [s.sb([68, 128], BF16, f"kcA{g}") for g in range(2)]
        vcA = [s.sb([128, 97], BF16, f"vcA{g}") for g in range(2)]
        gate = s.sb([128, 16, 12], F32, "gate")
        selTb = [s.sb([32, S], BF16, f"selTb{g}") for g in range(2)]
        cmaskb = s.sb([128, S], BF16, "cmaskb")
        selE = s.sb([32, 16, 128], BF16, "selE")
        vm = s.sb([128, 16, 32], F32, "vm")
        am = s.sb([128, 16, 32], F32, "am")
        imp = s.sb([128, 32], F32, "imp")
        impf = s.sb([128, 32], F32, "impf")
        top8 = s.sb([128, 8], F32, "top8")
        selb = s.sb([128, 32], F32, "selb")
        rdn = s.sb([128, 8], F32, "rdn")
        DMA("gpsimd", cmaskb[:], D["c_cmask"], w=["cmaskb"])
        DMA("gpsimd", selE[:], D["c_selE"], w=["selE"])
        DMA("sync", vm[:], D["c_impvm"], w=["vm"])
        DMA("sync", am[:], D["c_impam"], w=["am"])
        for h in range(4):
            DMA("gpsimd", qa[h][64:68, :], D["c_qaug"][h], w=[("qa", h, "aug")])
        for g in range(2):
            DMA("gpsimd", kcA[g][64:68, :], D["c_kcaug"], w=[("kcA", g, "aug")])
            V(lambda e, g=g: e.memset(vcA[g][:, 0:64], 0.0), [], [("vcA", g, "v")])
            V(lambda e, g=g: e.memset(vcA[g][:, 64:65], 1.0), [], [("vcA", g, "one")])
            DMA("gpsimd", vcA[g][:, 65:97], D["c_cover"], w=[("vcA", g, "cov")])
            V(lambda e, g=g: e.memset(kcA[g][0:64, :], 0.0), [], [("kcA", g, "k")])
        for h in range(4):
            def ev(ps, key, tq, h=h):
                V(lambda e: e.tensor_scalar(out=qa[h][0:64, tqs(tq)], in0=ps[0:64, :], scalar1=0.125, scalar2=None, op0=ALU.mult),
                  [key], [("qa", h, tq)])
            proj_fm([(0, W[:, h * 64:(h + 1) * 64])], 64, ev)

        s.push()
        srcT = [s.sb([64, S], BF16, f"srcT{g}") for g in range(2)]
        w1t = s.sb([64, 32, 256], BF16, "w1t")
        w2t = s.sb([128, 2, 64], BF16, "w2t")
        posr = s.sb([32, 64], F32, "posr")
        posT = s.sb([64, 32], BF16, "posT")
        hb = s.sb([128, 2], F32, "hb")
        hidT = s.sb([128, 2, 128], BF16, "hidT")
        for j in range(2):
            for g in range(2):
                def ev(ps, key, tq, g=g):
                    vcopy(srcT[g][:, tqs(tq)], ps[0:64, :], [key], [("srcT", g, tq)])
                c0 = 256 + 128 * j + 64 * g
                proj_fm([(0, W[:, c0:c0 + 64])], 64, ev)
            DMA("gpsimd", w1t[:], D["nsa_cmp_w1"][l, j].rearrange("(l d) h -> d l h", d=64), w=["w1t"])
            DMA("gpsimd", w2t[:], D["nsa_cmp_w2"][l, j].rearrange("(c p) d -> p c d", p=128), w=["w2t"])
            DMA("sync", posr[:], D["nsa_cmp_pos"][l, j], w=["posr"])
            TR(PS[6][0:64, 0:32], posr[:, :], ident[0:32, 0:32], ["posr", "ident"], [("ps", 6)])
            vcopy(posT[:], PS[6][0:64, 0:32], [("ps", 6)], ["posT"])
            for hc in range(2):
                for li in range(32):
                    MM(PS[7][:, hc:hc + 1], w1t[:, li, hc * 128:(hc + 1) * 128], posT[:, li:li + 1], li == 0, li == 31,
                       ["w1t", "posT"], [("ps", 7)])
            vcopy(hb[:], PS[7][:, 0:2], [("ps", 7)], ["hb"])
            for g in range(2):
                sk = [("srcT", g, tq) for tq in range(4)]
                for hc in range(2):
                    b = gbank()
                    for li in range(32):
                        MM(PS[b][:, 0:127], w1t[:, li, hc * 128:(hc + 1) * 128], srcT[g][:, li:li + 2017:16], li == 0, li == 31,
                           ["w1t"] + sk, [("ps", b)])
                    A(lambda e, b=b, hc=hc: e.activation(out=hidT[:, hc, 0:127], in_=PS[b][:, 0:127], func=AF.Gelu, bias=hb[:, hc:hc + 1]),
                      [("ps", b), "hb"], [("hidT", hc)])
                b = gbank()
                if j == 0:
                    for hc in range(2):
                        MM(PS[b][0:64, 0:127], w2t[:, hc, :], hidT[:, hc, 0:127], hc == 0, hc == 1, ["w2t", ("hidT", hc)], [("ps", b)])
                    vcopy(kcA[g][0:64, 0:127], PS[b][0:64, 0:127], [("ps", b)], [("kcA", g, "k")])
                else:
                    for hc in range(2):
                        MM(PS[b][0:127, 0:64], hidT[:, hc, 0:127], w2t[:, hc, :], hc == 0, hc == 1, ["w2t", ("hidT", hc)], [("ps", b)])
                    vcopy(vcA[g][0:127, 0:64], PS[b][0:127, 0:64], [("ps", b)], [("vcA", g, "v")])
        s.pop()

        s.push()
        ksA = [s.sb([68, S], BF16, f"ksA{g}") for g in range(2)]
        kwA = [s.sb([68, S], BF16, f"kwA{g}") for g in range(2)]
        vsA = s.sb([128, 16, 2, 65], BF16, "vsA")
        vwA = s.sb([128, 16, 2, 65], BF16, "vwA")
        V(lambda e: e.memset(vsA[:, :, :, 64:65], 1.0), [], [("vsA", tt) for tt in range(16)])
        V(lambda e: e.memset(vwA[:, :, :, 64:65], 1.0), [], [("vwA", tt) for tt in range(16)])
        for g in range(2):
            DMA("gpsimd", ksA[g][64:68, :], D["c_kaug"], w=[("ksA", g, "aug")])
            DMA("gpsimd", kwA[g][64:68, :], D["c_kaug"], w=[("kwA", g, "aug")])
            for nm, dst, c0 in (("ksA", ksA, 512), ("kwA", kwA, 768)):
                def ev(ps, key, tq, dst=dst, nm=nm, g=g):
                    vcopy(dst[g][0:64, tqs(tq)], ps[0:64, :], [key], [(nm, g, tq)])
                proj_fm([(0, W[:, c0 + 64 * g:c0 + 64 * g + 64])], 64, ev)

        def evv(ps, key, tt):
            vcopy(vsA[:, tt, :, 0:64], ps[:, 0:128].rearrange("p (g d) -> p g d", g=2), [key], [("vsA", tt)])
            vcopy(vwA[:, tt, :, 0:64], ps[:, 128:256].rearrange("p (g d) -> p g d", g=2), [key], [("vwA", tt)])
            vcopy(gate[:, tt, :], ps[:, 256:268], [key], [("gateraw", tt)])
            A(lambda e: e.activation(out=gate[:, tt, :], in_=gate[:, tt, :], func=AF.Sigmoid), [("gateraw", tt)], ["gate"])
        proj_tm([(0, W[:, 640:768]), (128, W[:, 896:1024]), (256, W[:, 1024:1036])], 268, evv)

        for g in range(2):
            for qt in range(4):
                for n in range(2):
                    h = 2 * g + n
                    MM(PS[n][:], kcA[g][:, :], qa[h][:, tqs(qt)], True, False,
                       [("kcA", g, "k"), ("kcA", g, "aug"), ("qa", h, qt), ("qa", h, "aug")], [("ps", n)])
                    MM(PS[n][:], identb[:], cmaskb[:, tqs(qt)], False, True, ["identb", "cmaskb"], [("ps", n)])
                    Pn = AT["P"][n]
                    A(lambda e, n=n, Pn=Pn: e.activation(out=Pn[:], in_=PS[n][:], func=AF.Exp), [("ps", n)], [("P", n)])
                    for qs in range(4):
                        MM(PS[2 + n][:, qs * 97:(qs + 1) * 97], Pn[:, qs * 128:(qs + 1) * 128], vcA[g][:, :], True, True,
                           [("P", n), ("vcA", g, "v"), ("vcA", g, "one"), ("vcA", g, "cov")], [("ps", 2 + n)])
                for qs in range(4):
                    tt = 4 * qt + qs
                    for n in range(2):
                        h = 2 * g + n
                        c0 = qs * 97
                        V(lambda e, n=n, c0=c0: e.tensor_scalar(out=rdn[:, 4 + n:5 + n], in0=PS[2 + n][:, c0 + 64:c0 + 65], scalar1=1e-30,
                                                               scalar2=None, op0=ALU.max), [("ps", 2 + n)], [("rdn", 4 + n)])
                        V(lambda e, n=n: e.reciprocal(out=rdn[:, n:n + 1], in_=rdn[:, 4 + n:5 + n]), [("rdn", 4 + n)], [("rdn", n)])
                        V(lambda e, n=n, h=h, tt=tt: e.tensor_tensor(out=rdn[:, 2 + n:3 + n], in0=rdn[:, n:n + 1], in1=gate[:, tt, 3 * h:3 * h + 1],
                                                                      op=ALU.mult), [("rdn", n), "gate"], [("rdn", 2 + n)])
                        V(lambda e, n=n, h=h, tt=tt, c0=c0: e.tensor_scalar(out=oacc[:, tt, h * 64:(h + 1) * 64], in0=PS[2 + n][:, c0:c0 + 64],
                                                                         scalar1=rdn[:, 2 + n:3 + n], scalar2=None, op0=ALU.mult),
                          [("ps", 2 + n), ("rdn", 2 + n)], [("oacc", tt)])
                    c0 = qs * 97
                    V(lambda e, c0=c0: e.tensor_scalar(out=imp[:], in0=PS[2][:, c0 + 65:c0 + 97], scalar1=rdn[:, 0:1], scalar2=None, op0=ALU.mult),
                      [("ps", 2), ("rdn", 0)], ["imp"])
                    V(lambda e, c0=c0: e.scalar_tensor_tensor(out=imp[:], in0=PS[3][:, c0 + 65:c0 + 97], scalar=rdn[:, 1:2], in1=imp[:],
                                                             op0=ALU.mult, op1=ALU.add), [("ps", 3), ("rdn", 1), "imp"], ["imp"])
                    V(lambda e, tt=tt: e.tensor_tensor(out=impf[:], in0=imp[:], in1=vm[:, tt, :], op=ALU.mult), ["imp", "vm"], ["impf"])
                    V(lambda e, tt=tt: e.tensor_tensor(out=impf[:], in0=impf[:], in1=am[:, tt, :], op=ALU.add), ["impf", "am"], ["impf"])
                    V(lambda e: e.max(out=top8[:], in_=impf[:]), ["impf"], ["top8"])
                    V(lambda e: e.tensor_scalar(out=selb[:], in0=impf[:], scalar1=top8[:, 7:8], scalar2=None, op0=ALU.is_ge),
                      ["impf", "top8"], ["selb"])
                    V(lambda e: e.tensor_scalar(out=selb[:], in0=selb[:], scalar1=-1.0, scalar2=-NEG, op0=ALU.add, op1=ALU.mult),
                      ["selb"], ["selb"])
                    TR(PS[6][0:32, qs * 128:(qs + 1) * 128], selb[:, :], ident[:], ["selb", "ident"], [("ps", 6)])
                vcopy(selTb[g][:, tqs(qt)], PS[6][0:32, :], [("ps", 6)], [("selTb", g, qt)])

        it = 0
        for br, kA, vA, nm, vnm in ((1, ksA, vsA, "ksA", "vsA"), (2, kwA, vwA, "kwA", "vwA")):
            for h in range(4):
                g = h // 2
                for qt in range(4):
                    it += 1
                    bank = 2 + it % 2
                    if br == 1:
                        kcs = range(0, 4 * qt + 4)
                        pv_ok = lambda kc, qs, qt=qt: kc <= 4 * qt + qs
                    else:
                        kcs = range(max(0, 4 * qt - 4), 4 * qt + 4)
                        pv_ok = lambda kc, qs, qt=qt: 4 * qt + qs - 4 <= kc <= 4 * qt + qs

                    def terms(kc, h=h, g=g, qt=qt, br=br, kA=kA, nm=nm):
                        tl = [(kA[g][:, kc * 128:(kc + 1) * 128], qa[h][:, tqs(qt)],
                               [(nm, g, kc // 4), (nm, g, "aug"), ("qa", h, qt), ("qa", h, "aug")])]
                        if br == 1:
                            tl.append((selE[:, kc, :], selTb[g][:, tqs(qt)], ["selE", ("selTb", g, qt)]))
                        if kc >= 4 * qt:
                            tl.append((identb[:], masks[:, kc - 4 * qt, :], ["identb", "masks"]))
                        elif br == 2:
                            tl.append((identb[:], masks[:, 8 + kc - (4 * qt - 4), :], ["identb", "masks"]))
                        return tl

                    def vaug(kc, g=g, vA=vA, vnm=vnm):
                        return vA[:, kc, g, :], (vnm, kc)

                    accmap = lambda qs, bank=bank: (bank, qs * 65)
                    attn(terms, kcs, vaug, 65, pv_ok, accmap)
                    attn_out(accmap, 64, 4 * qt, lambda tt, h=h, br=br: gate[:, tt, 3 * h + br:3 * h + br + 1],
                             lambda tt, h=h: oacc[:, tt, h * 64:(h + 1) * 64], True)
        s.pop()
        s.pop()

    def sbranch(l, W, oacc):
        s.push()
        qT = [s.sb([64, S], BF16, f"sbq{h}") for h in range(4)]
        kT = [s.sb([64, S], BF16, f"sbk{h}") for h in range(4)]
        vB = s.sb([128, 16, 256], BF16, "sbv")
        negU = s.sb([128, 128], BF16, "negU")
        negO = s.sb([128, 128], F32, "negO")
        et = [s.sb([128, 512], F32, f"et{i}") for i in range(2)]
        spt = [s.sb([128, 512], BF16, f"spt{i}") for i in range(3)]
        acc = [s.sb([128, 512], F32, f"sbacc{i}") for i in range(2)]
        DMA("gpsimd", negU[:], D["c_negU"], w=["negU"])
        V(lambda e: e.memset(negO[:], -1.0), [], ["negO"])
        for h in range(4):
            def evq(ps, key, tq, h=h):
                V(lambda e: e.tensor_scalar(out=qT[h][:, tqs(tq)], in0=ps[0:64, :], scalar1=0.125, scalar2=None, op0=ALU.mult),
                  [key], [("sbq", h, tq)])
            proj_fm([(0, W[:, 1036 + h * 64:1036 + (h + 1) * 64])], 64, evq)

            def evk(ps, key, tq, h=h):
                vcopy(kT[h][:, tqs(tq)], ps[0:64, :], [key], [("sbk", h, tq)])
            proj_fm([(0, W[:, 1292 + h * 64:1292 + (h + 1) * 64])], 64, evk)

        def evv(ps, key, tt):
            vcopy(vB[:, tt, :], ps[:, 0:256], [key], [("sbv", tt)])
        proj_tm([(0, W[:, 1548:1804])], 256, evv)

        SB3 = (0, 1, 5)
        st = {"i": 0, "a": 0, "it": 0}
        for h in range(4):
            for qt in range(4):
                st["it"] += 1
                bank = 2 + st["it"] % 2
                kcs = list(range(4 * qt + 3, -1, -1))

                def stageA(kc, h=h, qt=qt):
                    st["i"] += 1
                    i = st["i"]
                    sbk = SB3[i % 3]
                    MM(PS[sbk][:], kT[h][:, kc * 128:(kc + 1) * 128], qT[h][:, tqs(qt)], True, False,
                       [("sbk", h, kc // 4), ("sbq", h, qt)], [("ps", sbk)])
                    if kc >= 4 * qt:
                        MM(PS[sbk][:], identb[:], masks[:, 4 + kc - 4 * qt, :], False, False, ["identb", "masks"], [("ps", sbk)])
                    A(lambda e: e.activation(out=et[i % 2][:], in_=PS[sbk][:], func=AF.Exp), [("ps", sbk)], [("et", i % 2)])
                    A(lambda e: e.activation(out=spt[i % 3][:], in_=et[i % 2][:], func=AF.Ln, bias=1.0), [("et", i % 2)], [("spt", i % 3)])
                    return i

                def stageB(kc, i, idx, h=h, qt=qt, bank=bank):
                    sbk = SB3[i % 3]
                    a = st["a"]
                    MM(PS[sbk][:], negU[:], spt[i % 3][:], False, idx == 0, ["negU", ("spt", i % 3)], [("ps", sbk)])
                    if idx > 0:
                        MM(PS[sbk][:], negO[:], acc[a % 2][:], False, True, ["negO", ("sbacc", a % 2)], [("ps", sbk)])
                    ctr["p"] += 1
                    pi = ctr["p"] % 3
                    Pp = AT["P"][pi]
                    A(lambda e: e.activation(out=Pp[:], in_=PS[sbk][:], func=AF.Exp), [("ps", sbk)], [("P", pi)])
                    for qs in range(4):
                        if kc > 4 * qt + qs:
                            continue
                        MMs(PS[bank][:, qs * 64:(qs + 1) * 64], Pp[:, qs * 128:(qs + 1) * 128], vB[:, kc, h * 64:(h + 1) * 64],
                            idx == 0 and qs == 3, kc == 0, [("P", pi), ("sbv", kc)], [("ps", bank)])
                    if kc > 0:
                        if idx == 0:
                            G(lambda e: e.tensor_copy(out=acc[(a + 1) % 2][:], in_=spt[i % 3][:]), [("spt", i % 3)], [("sbacc", (a + 1) % 2)])
                        else:
                            G(lambda e: e.tensor_tensor(out=acc[(a + 1) % 2][:], in0=acc[a % 2][:], in1=spt[i % 3][:], op=ALU.add),
                              [("spt", i % 3), ("sbacc", a % 2)], [("sbacc", (a + 1) % 2)])
                        st["a"] += 1

                cur = stageA(kcs[0])
                for idx, kc in enumerate(kcs):
                    nxt = stageA(kcs[idx + 1]) if idx + 1 < len(kcs) else None
                    stageB(kc, cur, idx)
                    cur = nxt
                accmap = lambda qs, bank=bank: (bank, qs * 64)
                attn_out(accmap, 64, 4 * qt, None, lambda tt, h=h: oacc[:, tt, h * 64:(h + 1) * 64], False, normalize=False)
        s.pop()

    def rglru(l, W, oT):
        s.push()
        xr = s.sb([128, S + 3], F32, "xr")
        xg = s.sb([128, S], F32, "xg")
        u = s.sb([128, S], F32, "u")
        ub = s.sb([128, S], BF16, "ub")
        ra = s.sb([128, S], F32, "ra")
        ib = s.sb([128, S], F32, "ib")
        hh = s.sb([128, S], F32, "hh")
        prm = s.sb([128, 12], F32, "prm")
        gw = [s.sb([128, 128], BF16, f"gw{i}") for i in range(2)]
        for ch in range(2):
            cs = slice(ch * 128, (ch + 1) * 128)
            for tap in range(4):
                DMA("sync", prm[:, tap:tap + 1], D["rnn_conv_w"][l, tap, cs].rearrange("(c o) -> c o", o=1), w=[("prm", tap)])
            for i, nm in enumerate(("rnn_conv_b", "rnn_ga_b", "rnn_gx_b", "rnn_lambda")):
                DMA("sync", prm[:, 4 + i:5 + i], D[nm][l, cs].rearrange("(c o) -> c o", o=1), w=[("prm", 4 + i)])
            for i, nm in enumerate(("rnn_ga_w", "rnn_gx_w")):
                V(lambda e, i=i: e.memset(gw[i][:], 0.0), [], [("gw", i)])
                for n in range(2):
                    DMA("gpsimd", gw[i][n * 64:(n + 1) * 64, n * 64:(n + 1) * 64], D[nm][l, 2 * ch + n], w=[("gw", i)])
            A(lambda e: e.activation(out=prm[:, 9:10], in_=prm[:, 7:8], func=AF.Exp, scale=-1.0), [("prm", 7)], [("prm", 9)])
            A(lambda e: e.activation(out=prm[:, 9:10], in_=prm[:, 9:10], func=AF.Ln, bias=1.0), [("prm", 9)], [("prm", 9)])
            V(lambda e: e.tensor_scalar(out=prm[:, 8:9], in0=prm[:, 9:10], scalar1=-8.0, scalar2=None, op0=ALU.mult), [("prm", 9)], [("prm", 8)])
            V(lambda e: e.memset(xr[:, 0:3], 0.0), [], [("xr", "pad")])

            def evx(ps, key, tq):
                vcopy(xr[:, 3 + tq * 512:3 + (tq + 1) * 512], ps[:, :], [key], [("xr", tq)])
            proj_fm([(0, W[:, 1804 + ch * 128:1804 + (ch + 1) * 128])], 128, evx)

            def evg(ps, key, tq):
                A(lambda e: e.activation(out=xg[:, tqs(tq)], in_=ps[:, :], func=AF.Gelu), [key], [("xg", tq)])
            proj_fm([(0, W[:, 2060 + ch * 128:2060 + (ch + 1) * 128])], 128, evg)
            xk = [("xr", tq) for tq in range(4)] + [("xr", "pad")]
            V(lambda e: e.tensor_scalar(out=u[:], in0=xr[:, 0:S], scalar1=prm[:, 0:1], scalar2=prm[:, 4:5], op0=ALU.mult, op1=ALU.add),
              xk + [("prm", 0), ("prm", 4)], ["u"])
            for tap in range(1, 4):
                V(lambda e, tap=tap: e.scalar_tensor_tensor(out=u[:], in0=xr[:, tap:tap + S], scalar=prm[:, tap:tap + 1], in1=u[:],
                                                            op0=ALU.mult, op1=ALU.add), xk + [("prm", tap), "u"], ["u"])
            vcopy(ub[:], u[:], ["u"], ["ub"])
            for tq in range(4):
                for i, dst, bcol in ((0, ra, 5), (1, ib, 6)):
                    b = gbank()
                    MM(PS[b][:], gw[i][:], ub[:, tqs(tq)], True, True, [("gw", i), "ub"], [("ps", b)])
                    A(lambda e, b=b, dst=dst, bcol=bcol, tq=tq: e.activation(out=dst[:, tqs(tq)], in_=PS[b][:], func=AF.Sigmoid,
                                                                             bias=prm[:, bcol:bcol + 1]), [("ps", b), ("prm", bcol)], [(("ra", "ib")[i], tq)])
            rk = [("ra", tq) for tq in range(4)]
            ik = [("ib", tq) for tq in range(4)]
            A(lambda e: e.activation(out=ra[:], in_=ra[:], func=AF.Exp, scale=prm[:, 8:9]), rk + [("prm", 8)], rk)
            V(lambda e: e.tensor_tensor(out=ib[:], in0=ib[:], in1=u[:], op=ALU.mult), ik + ["u"], ik)
            V(lambda e: e.tensor_tensor(out=hh[:], in0=ra[:], in1=ra[:], op=ALU.mult), rk, ["hh"])
            V(lambda e: e.tensor_scalar(out=hh[:], in0=hh[:], scalar1=-1.0, scalar2=1.0, op0=ALU.mult, op1=ALU.add), ["hh"], ["hh"])
            V(lambda e: e.tensor_scalar(out=hh[:], in0=hh[:], scalar1=0.0, scalar2=None, op0=ALU.max), ["hh"], ["hh"])
            A(lambda e: e.activation(out=hh[:], in_=hh[:], func=AF.Sqrt), ["hh"], ["hh"])
            V(lambda e: e.tensor_tensor(out=ib[:], in0=ib[:], in1=hh[:], op=ALU.mult), ik + ["hh"], ik)
            V(lambda e: e.tensor_tensor_scan(out=hh[:], data0=ra[:], data1=ib[:], initial=0.0, op0=ALU.mult, op1=ALU.add),
              rk + ik + ["hh"], ["hh"])
            V(lambda e, ch=ch: e.tensor_tensor(out=oT[:, 2, ch, :], in0=hh[:], in1=xg[:], op=ALU.mult),
              ["hh"] + [("xg", tq) for tq in range(4)], [("oT", 2, ch)])
        s.pop()

    def mla(l, W, oacc):
        s.push()
        cqn = s.sb([128, 2, S], BF16, "cqn")
        ckvn = s.sb([128, S], BF16, "ckvn")
        wuq = s.sb([128, 2, 384], BF16, "wuq")
        wuqS = s.sb([128, 2, 384], BF16, "wuqS")
        wukv = s.sb([128, 512], BF16, "wukv")
        gq = s.sb([128, 2], F32, "gq")
        gkv = s.sb([128, 1], F32, "gkv")
        onesF = s.sb([128, 128], F32, "onesF")
        V(lambda e: e.memset(onesF[:], 1.0), [], ["onesF"])
        V(lambda e: e.memset(wuqS[:], 0.0), [], ["wuqS"])
        V(lambda e: e.memset(cqn[:], 0.0), [], ["cqn0"])
        Wq = D["mla_w_uq"][l]
        DMA("gpsimd", wuq[:, 0, :], Wq[0:128, :], w=[("wuq", 0)])
        DMA("gpsimd", wuq[0:64, 1, :], Wq[128:192, :], w=[("wuq", 1)])
        for h in range(4):
            for (r0, r1, cidx) in ((0, 128, 0), (128, 192, 1)):
                np_ = r1 - r0
                DMA("gpsimd", wuqS[0:np_, cidx, h * 96 + 64:h * 96 + 80], Wq[r0:r1, h * 96 + 80:h * 96 + 96], r=["wuqS"], w=[("wuqS", h, cidx, 0)])
                DMA("gpsimd", wuqS[0:np_, cidx, h * 96 + 80:h * 96 + 96], Wq[r0:r1, h * 96 + 64:h * 96 + 80], r=["wuqS"], w=[("wuqS", h, cidx, 1)])
        DMA("gpsimd", wukv[:], D["mla_w_ukv"][l], w=["wukv"])
        DMA("sync", gq[:, 0:1], D["mla_q_norm"][l, 0:128].rearrange("(c o) -> c o", o=1), w=[("gq", 0)])
        DMA("sync", gq[0:64, 1:2], D["mla_q_norm"][l, 128:192].rearrange("(c o) -> c o", o=1), w=[("gq", 1)])
        DMA("sync", gkv[:, 0:1], D["mla_kv_norm"][l].rearrange("(c o) -> c o", o=1), w=["gkv"])

        s.push()
        cq = s.sb([128, 2, S], F32, "cq")
        ckv = s.sb([128, S], F32, "ckv")
        sq = s.sb([128, 2, 512], F32, "sq")
        rstd = s.sb([128, 512], F32, "rstd")

        def ev0(ps, key, tq):
            vcopy(cq[:, 0, tqs(tq)], ps[:, :], [key], [("cq", 0, tq)])
        proj_fm([(0, W[:, 2316:2444])], 128, ev0)

        def ev1(ps, key, tq):
            vcopy(cq[0:64, 1, tqs(tq)], ps[0:64, :], [key], [("cq", 1, tq)])
        proj_fm([(0, W[:, 2444:2508])], 64, ev1)

        def ev2(ps, key, tq):
            vcopy(ckv[:, tqs(tq)], ps[:, :], [key], [("ckv", tq)])
        proj_fm([(0, W[:, 2508:2636])], 128, ev2)
        for tq in range(4):
            V(lambda e, tq=tq: e.tensor_tensor(out=sq[:, 0, :], in0=cq[:, 0, tqs(tq)], in1=cq[:, 0, tqs(tq)], op=ALU.mult), [("cq", 0, tq)], [("sq", 0)])
            V(lambda e, tq=tq: e.tensor_tensor(out=sq[0:64, 1, :], in0=cq[0:64, 1, tqs(tq)], in1=cq[0:64, 1, tqs(tq)], op=ALU.mult), [("cq", 1, tq)], [("sq", 1)])
            b = gbank()
            MM(PS[b][:], onesF[:, :], sq[:, 0, :], True, False, ["onesF", ("sq", 0)], [("ps", b)])
            MM(PS[b][:], onesF[0:64, :], sq[0:64, 1, :], False, True, ["onesF", ("sq", 1)], [("ps", b)])
            V(lambda e, b=b: e.tensor_scalar(out=rstd[:], in0=PS[b][:], scalar1=1.0 / 192, scalar2=1e-6, op0=ALU.mult, op1=ALU.add), [("ps", b)], ["rstd"])
            A(lambda e: e.activation(out=rstd[:], in_=rstd[:], func=AF.Ln), ["rstd"], ["rstd"])
            A(lambda e: e.activation(out=rstd[:], in_=rstd[:], func=AF.Exp, scale=-0.5), ["rstd"], ["rstd"])
            V(lambda e, tq=tq: e.scalar_tensor_tensor(out=cqn[:, 0, tqs(tq)], in0=cq[:, 0, tqs(tq)], scalar=gq[:, 0:1], in1=rstd[:], op0=ALU.mult, op1=ALU.mult),
              [("cq", 0, tq), ("gq", 0), "rstd", "cqn0"], [("cqn", tq, 0)])
            V(lambda e, tq=tq: e.scalar_tensor_tensor(out=cqn[0:64, 1, tqs(tq)], in0=cq[0:64, 1, tqs(tq)], scalar=gq[0:64, 1:2], in1=rstd[0:64, :], op0=ALU.mult, op1=ALU.mult),
              [("cq", 1, tq), ("gq", 1), "rstd", "cqn0"], [("cqn", tq, 1)])
            V(lambda e, tq=tq: e.tensor_tensor(out=sq[:, 0, :], in0=ckv[:, tqs(tq)], in1=ckv[:, tqs(tq)], op=ALU.mult), [("ckv", tq)], [("sq", 0)])
            b = gbank()
            MM(PS[b][:], onesF[:, :], sq[:, 0, :], True, True, ["onesF", ("sq", 0)], [("ps", b)])
            V(lambda e, b=b: e.tensor_scalar(out=rstd[:], in0=PS[b][:], scalar1=1.0 / 128, scalar2=1e-6, op0=ALU.mult, op1=ALU.add), [("ps", b)], ["rstd"])
            A(lambda e: e.activation(out=rstd[:], in_=rstd[:], func=AF.Ln), ["rstd"], ["rstd"])
            A(lambda e: e.activation(out=rstd[:], in_=rstd[:], func=AF.Exp, scale=-0.5), ["rstd"], ["rstd"])
            V(lambda e, tq=tq: e.scalar_tensor_tensor(out=ckvn[:, tqs(tq)], in0=ckv[:, tqs(tq)], scalar=gkv[:, 0:1], in1=rstd[:], op0=ALU.mult, op1=ALU.mult),
              [("ckv", tq), "gkv", "rstd"], [("ckvn", tq)])
        s.pop()

        s.push()
        QT = [s.sb([96, S], BF16, f"mq{h}") for h in range(4)]
        KT = [s.sb([96, S], BF16, f"mk{h}") for h in range(4)]
        vA = s.sb([128, 16, 4, 65], BF16, "mv")
        ropec = s.sb([96, S], F32, "ropec")
        ropes = s.sb([96, S], F32, "ropes")
        t1 = s.sb([96, 512], F32, "t1")
        t2 = s.sb([96, 512], F32, "t2")
        krw = s.sb([128, 8, 96], BF16, "krw")
        krwS = s.sb([128, 8, 96], BF16, "krwS")
        DMA("sync", ropec[:], D["c_ropec"], w=["ropec"])
        DMA("sync", ropes[:], D["c_ropes"], w=["ropes"])
        V(lambda e: e.memset(vA[:, :, :, 64:65], 1.0), [], [("mv", tt) for tt in range(16)])
        V(lambda e: e.memset(krw[:], 0.0), [], ["krw"])
        V(lambda e: e.memset(krwS[:], 0.0), [], ["krwS"])
        DMA("gpsimd", krw[:, :, 64:96], W[:, 2636:2668].rearrange("(c p) m -> p c m", p=128), r=["krw"], w=["krw1"])
        DMA("gpsimd", krwS[:, :, 64:80], W[:, 2652:2668].rearrange("(c p) m -> p c m", p=128), r=["krwS"], w=["krwS1"])
        DMA("gpsimd", krwS[:, :, 80:96], W[:, 2636:2652].rearrange("(c p) m -> p c m", p=128), r=["krwS"], w=["krwS2"])

        def rope_comb(pa, pb, ka, kb, dst, dkey, tq):
            V(lambda e: e.tensor_tensor(out=t1[64:96, :], in0=pa[64:96, :], in1=ropec[64:96, tqs(tq)], op=ALU.mult), [ka, "ropec"], ["t1"])
            V(lambda e: e.tensor_tensor(out=t2[64:96, :], in0=pb[64:96, :], in1=ropes[64:96, tqs(tq)], op=ALU.mult), [kb, "ropes"], ["t2"])
            V(lambda e: e.tensor_tensor(out=dst[64:96, tqs(tq)], in0=t1[64:96, :], in1=t2[64:96, :], op=ALU.add), ["t1", "t2"], [dkey])

        for tq in range(4):
            for c in range(8):
                MM(PS[4][0:96, :], krw[:, c, :], xT[:, c, tqs(tq)], c == 0, c == 7, ["krw", "krw1"] + xkeys(tq), [("ps", 4)])
            for c in range(8):
                MM(PS[5][0:96, :], krwS[:, c, :], xT[:, c, tqs(tq)], c == 0, c == 7, ["krwS", "krwS1", "krwS2"] + xkeys(tq), [("ps", 5)])
            rope_comb(PS[4], PS[5], ("ps", 4), ("ps", 5), KT[0], ("mk", 0, tq, "r"), tq)
            for h in range(1, 4):
                vcopy(KT[h][64:96, tqs(tq)], KT[0][64:96, tqs(tq)], [("mk", 0, tq, "r")], [("mk", h, tq, "r")])
        cqk = lambda tq: [("cqn", tq, 0), ("cqn", tq, 1), "cqn0"]
        for h in range(4):
            for tq in range(4):
                hs = slice(h * 96, (h + 1) * 96)
                MM(PS[4][0:96, :], wuq[:, 0, hs], cqn[:, 0, tqs(tq)], True, False, [("wuq", 0)] + cqk(tq), [("ps", 4)])
                MM(PS[4][0:96, :], wuq[0:64, 1, hs], cqn[0:64, 1, tqs(tq)], False, True, [("wuq", 1)] + cqk(tq), [("ps", 4)])
                wsk = ["wuqS"] + [("wuqS", h, ci, j) for ci in range(2) for j in range(2)]
                MM(PS[5][0:96, :], wuqS[:, 0, hs], cqn[:, 0, tqs(tq)], True, False, wsk + cqk(tq), [("ps", 5)])
                MM(PS[5][0:96, :], wuqS[0:64, 1, hs], cqn[0:64, 1, tqs(tq)], False, True, wsk + cqk(tq), [("ps", 5)])
                vcopy(QT[h][0:64, tqs(tq)], PS[4][0:64, :], [("ps", 4)], [("mq", h, tq)])
                rope_comb(PS[4], PS[5], ("ps", 4), ("ps", 5), QT[h], ("mq", h, tq, "r"), tq)
                b = 6 + tq % 2
                MM(PS[b][0:64, :], wukv[:, h * 128:h * 128 + 64], ckvn[:, tqs(tq)], True, True, ["wukv", ("ckvn", tq)], [("ps", b)])
                vcopy(KT[h][0:64, tqs(tq)], PS[b][0:64, :], [("ps", b)], [("mk", h, tq)])
        for tt in range(16):
            b = 6 + tt % 2
            for h in range(4):
                MM(PS[b][:, h * 64:(h + 1) * 64], ckvn[:, tt * 128:(tt + 1) * 128], wukv[:, h * 128 + 64:h * 128 + 128], True, True,
                   ["wukv", ("ckvn", tt // 4)], [("ps", b)])
            vcopy(vA[:, tt, :, 0:64], PS[b][:, 0:256].rearrange("p (h d) -> p h d", h=4), [("ps", b)], [("mv", tt)])
        it = 0
        for h in range(4):
            for qt in range(4):
                it += 1
                bank = 2 + it % 2

                def terms(kc, h=h, qt=qt):
                    tl = [(KT[h][:, kc * 128:(kc + 1) * 128], QT[h][:, tqs(qt)],
                           [("mk", h, kc // 4), ("mk", h, kc // 4, "r"), ("mq", h, qt), ("mq", h, qt, "r")])]
                    if kc >= 4 * qt:
                        tl.append((identb[:], masks[:, kc - 4 * qt, :], ["identb", "masks"]))
                    return tl
                accmap = lambda qs, bank=bank: (bank, qs * 65)
                attn(terms, range(0, 4 * qt + 4), lambda kc, h=h: (vA[:, kc, h, :], ("mv", kc)), 65,
                     lambda kc, qs, qt=qt: kc <= 4 * qt + qs, accmap, scale=96 ** -0.5)
                attn_out(accmap, 64, 4 * qt, None, lambda tt, h=h: oacc[:, tt, h * 64:(h + 1) * 64], False)
        s.pop()
        s.pop()

    def merge_ln1(l, W, oT, xsrc):
        s.push()
        mT = s.sb([128, 8, S], BF16, "mT")
        macc = s.sb([128, S], F32, "macc")
        sg = [s.sb([128, 512], F32, f"sg{i}") for i in range(2)]
        wbr = [s.sb([128, 2, 128], BF16, f"wbr{i}") for i in range(2)]
        k = 0
        for dc in range(8):
            for n in range(4):
                k += 1
                i = next_wbuf()
                wb = wbuf[i]
                c0 = 2668 + n * 1024 + dc * 128
                DMA("gpsimd", wb[:, :, 0:128], W[:, c0:c0 + 128].rearrange("(c p) m -> p c m", p=128), w=[("wbuf", i, 0)])
                DMA("gpsimd", wbr[k % 2][:], D["w_branch"][l, n][:, dc * 128:(dc + 1) * 128].rearrange("(c p) m -> p c m", p=128), w=[("wbr", k % 2)])
                for tq in range(4):
                    bg = 4 + tq % 2
                    bu = 6 + tq % 2
                    for c in range(8):
                        MM(PS[bg][:], wb[:, c, 0:128], xT[:, c, tqs(tq)], c == 0, c == 7, [("wbuf", i, 0)] + xkeys(tq), [("ps", bg)])
                    for c in range(2):
                        MM(PS[bu][:], wbr[k % 2][:, c, :], oT[:, n, c, tqs(tq)], c == 0, c == 1, [("wbr", k % 2), ("oT", n, c)] + [("oT", n, tt) for tt in range(4 * tq, 4 * tq + 4)], [("ps", bu)])
                    A(lambda e, bg=bg, tq=tq: e.activation(out=sg[tq % 2][:], in_=PS[bg][:], func=AF.Sigmoid), [("ps", bg)], [("sg", tq % 2)])
                    if n == 0:
                        V(lambda e, bu=bu, tq=tq: e.tensor_tensor(out=macc[:, tqs(tq)], in0=sg[tq % 2][:], in1=PS[bu][:], op=ALU.mult),
                          [("sg", tq % 2), ("ps", bu)], [("macc", tq)])
                    else:
                        V(lambda e, bu=bu, tq=tq: e.tensor_tensor(out=sg[tq % 2][:], in0=sg[tq % 2][:], in1=PS[bu][:], op=ALU.mult),
                          [("sg", tq % 2), ("ps", bu)], [("sg", tq % 2)])
                        if n < 3:
                            G(lambda e, tq=tq: e.tensor_tensor(out=macc[:, tqs(tq)], in0=macc[:, tqs(tq)], in1=sg[tq % 2][:], op=ALU.add),
                              [("sg", tq % 2), ("macc", tq)], [("macc", tq)])
                        else:
                            G(lambda e, tq=tq, dc=dc: e.tensor_tensor(out=mT[:, dc, tqs(tq)], in0=macc[:, tqs(tq)], in1=sg[tq % 2][:], op=ALU.add),
                              [("sg", tq % 2), ("macc", tq)], [("mT", dc, tq)])
        s.barrier()
        wo = s.sb([128, 8, 1024], BF16, "wo")
        xt2 = [s.sb([128, DM], F32, f"xt2{i}") for i in range(2)]
        lng_, lnb_ = s.sb([128, DM], F32, "lng"), s.sb([128, DM], F32, "lnb")
        LN["g"], LN["b"] = lng_, lnb_
        load_ln(l, "ln1_g", "ln1_b")
        DMA("gpsimd", wo[:], D["w_out"][l].rearrange("(c p) m -> p c m", p=128), w=["wo"])
        for tt in range(16):
            xt = xt2[tt % 2]
            key = ("xt2", tt % 2)
            DMA("sync", xt[:], xsrc[tt * 128:(tt + 1) * 128, :], w=[key])
            for hf in range(2):
                b = 4 + hf
                for dc in range(8):
                    MM(PS[b][:], mT[:, dc, tt * 128:(tt + 1) * 128], wo[:, dc, hf * 512:(hf + 1) * 512], dc == 0, dc == 7,
                       ["wo", ("mT", dc, tt // 4)], [("ps", b)])
                V(lambda e, xt=xt, b=b, hf=hf: e.scalar_tensor_tensor(out=xt[:, hf * 512:(hf + 1) * 512], in0=xt[:, hf * 512:(hf + 1) * 512],
                                                                    scalar=DN_ALPHA, in1=PS[b][:], op0=ALU.mult, op1=ALU.add), [key, ("ps", b)], [key])
            ln_tile(xt, key, tt, xs)
        s.pop()

    LN = {}

    def cross(l):
        s.push()
        AT["P"] = [s.sb([128, 512], BF16, f"P{i}") for i in range(3)]
        AT["rd"] = s.sb([128, 4], F32, "rd")
        memT = s.sb([128, 8, 256], BF16, "memT")
        KxT = [s.sb([128, 256], BF16, f"KxT{h}") for h in range(4)]
        vxA = s.sb([128, 2, 4, 129], BF16, "vxA")
        QxT = [s.sb([128, S], BF16, f"QxT{h}") for h in range(4)]
        ox = s.sb([128, 16, 512], F32, "ox")
        mt = [s.sb([128, DM], F32, f"mt{i}") for i in range(2)]
        V(lambda e: e.memset(vxA[:, :, :, 128:129], 1.0), [], [("vxA", 0), ("vxA", 1)])
        for mc in range(2):
            DMA("sync", mt[mc][:], D["mem"][mc * 128:(mc + 1) * 128, :], w=[("mt", mc)])
            for half in range(2):
                b = 6 + half
                for j in range(4):
                    c = half * 4 + j
                    TR(PS[b][:, j * 128:(j + 1) * 128], mt[mc][:, c * 128:(c + 1) * 128], ident[:], [("mt", mc), "ident"], [("ps", b)])
                vcopy(memT[:, half * 4:(half + 1) * 4, mc * 128:(mc + 1) * 128], PS[b][:].rearrange("p (j q) -> p j q", j=4), [("ps", b)], [("memT", mc)])
        mk = [("memT", 0), ("memT", 1)]
        Wkv = D["x_wkv"][l]
        for h in range(4):
            i = next_wbuf()
            wb = wbuf[i]
            DMA("gpsimd", wb[:, :, 0:128], Wkv[:, h * 128:(h + 1) * 128].rearrange("(c p) m -> p c m", p=128), w=[("wbuf", i, 0)])
            b = gbank()
            for c in range(8):
                MM(PS[b][:, 0:256], wb[:, c, 0:128], memT[:, c, :], c == 0, c == 7, [("wbuf", i, 0)] + mk, [("ps", b)])
            vcopy(KxT[h][:], PS[b][:, 0:256], [("ps", b)], [("KxT", h)])
        i = next_wbuf()
        wb = wbuf[i]
        DMA("gpsimd", wb[:, :, 0:512], Wkv[:, 512:1024].rearrange("(c p) m -> p c m", p=128), w=[("wbuf", i, 0)])
        for mc in range(2):
            b = gbank()
            for c in range(8):
                MM(PS[b][:], memT[:, c, mc * 128:(mc + 1) * 128], wb[:, c, 0:512], c == 0, c == 7, [("wbuf", i, 0)] + mk, [("ps", b)])
            vcopy(vxA[:, mc, :, 0:128], PS[b][:].rearrange("p (h d) -> p h d", h=4), [("ps", b)], [("vxA", mc)])
        for h in range(4):
            def evq(ps, key, tq, h=h):
                vcopy(QxT[h][:, tqs(tq)], ps[:, :], [key], [("QxT", h, tq)])
            proj_fm([(0, D["x_wq"][l][:, h * 128:(h + 1) * 128])], 128, evq)
        for h in range(4):
            for qt in range(4):
                accmap = lambda qs: (2 + qs // 2, (qs % 2) * 129)
                attn(lambda kc, h=h, qt=qt: [(KxT[h][:, kc * 128:(kc + 1) * 128], QxT[h][:, tqs(qt)], [("KxT", h), ("QxT", h, qt)])],
                     range(2), lambda kc, h=h: (vxA[:, kc, h, :], ("vxA", kc)), 129, lambda kc, qs: True, accmap, scale=128 ** -0.5)
                attn_out(accmap, 128, 4 * qt, None, lambda tt, h=h: ox[:, tt, h * 128:(h + 1) * 128], False)
        s.barrier()
        oxT = s.sb([128, 4, S], BF16, "oxT")
        to_fm(ox, 4, oxT, "oxT")
        wo = s.sb([128, 4, 1024], BF16, "xwo")
        lng_, lnb_ = s.sb([128, DM], F32, "lng"), s.sb([128, DM], F32, "lnb")
        LN["g"], LN["b"] = lng_, lnb_
        load_ln(l, "ln2_g", "ln2_b")
        DMA("gpsimd", wo[:], D["x_wo"][l].rearrange("(c p) m -> p c m", p=128), w=["xwo"])
        for tt in range(16):
            xt = mt[tt % 2]
            key = ("mt", tt % 2)
            DMA("sync", xt[:], xs[tt * 128:(tt + 1) * 128, :], w=[key])
            for hf in range(2):
                b = 4 + hf
                for c in range(4):
                    MM(PS[b][:], oxT[:, c, tt * 128:(tt + 1) * 128], wo[:, c, hf * 512:(hf + 1) * 512], c == 0, c == 3,
                       ["xwo", ("oxT", tt)], [("ps", b)])
                V(lambda e, xt=xt, b=b, hf=hf: e.scalar_tensor_tensor(out=xt[:, hf * 512:(hf + 1) * 512], in0=xt[:, hf * 512:(hf + 1) * 512],
                                                                    scalar=DN_ALPHA, in1=PS[b][:], op0=ALU.mult, op1=ALU.add), [key, ("ps", b)], [key])
            ln_tile(xt, key, tt, xs)
        s.pop()

    def moe(l, dst, last):
        s.push()
        yacc = s.sb([128, 16, DM], F32, "yacc")
        cwT = s.sb([32, S], F32, "cwT")
        selrow = s.sb([32, 32, 128], F32, "selrow")
        s.push()
        xt = [s.sb([128, DM], F32, f"rx{i}") for i in range(2)]
        xTf = s.sb([128, 8, 128], F32, "xTf")
        wr = s.sb([128, 8, 36], F32, "wr")
        brow = s.sb([1, 36], F32, "brow")
        ones1 = s.sb([1, 128], F32, "ones1")
        lg = s.sb([128, 36], F32, "lg")
        sm = s.sb([128, 16], F32, "sm")
        gm = s.sb([128, 4], F32, "gm")
        es = s.sb([128, 8], F32, "es")
        t8 = s.sb([128, 8], F32, "t8")
        ta = s.sb([128, 8], F32, "ta")
        tb = s.sb([128, 8], F32, "tb")
        cw = s.sb([128, 32], F32, "cw")
        DMA("sync", selrow[:], D["c_selrow"], w=["selrow2"])
        V(lambda e: e.memset(ones1[:], 1.0), [], ["ones1"])
        DMA("sync", wr[:, :, 0:4], D["moe_rg_w"][l].rearrange("(c p) m -> p c m", p=128), w=["wr0"])
        DMA("sync", wr[:, :, 4:36], D["moe_re_w"][l].rearrange("(c p) m -> p c m", p=128), w=["wr1"])
        DMA("sync", brow[:, 0:4], D["moe_rg_b"][l].rearrange("(o m) -> o m", o=1), w=["br0"])
        DMA("sync", brow[:, 4:36], D["moe_re_b"][l].rearrange("(o m) -> o m", o=1), w=["br1"])
        for tt in range(16):
            x_ = xt[tt % 2]
            key = ("rx", tt % 2)
            DMA("sync", x_[:], xs[tt * 128:(tt + 1) * 128, :], w=[key])
            for half in range(2):
                b = 6 + half
                for j in range(4):
                    c = half * 4 + j
                    TR(PS[b][:, j * 128:(j + 1) * 128], x_[:, c * 128:(c + 1) * 128], ident[:], [key, "ident"], [("ps", b)])
                vcopy(xTf[:, half * 4:(half + 1) * 4, :], PS[b][:].rearrange("p (j q) -> p j q", j=4), [("ps", b)], [("xTf", half)])
            for c in range(8):
                MM(PS[4][:, 0:36], xTf[:, c, :], wr[:, c, :], c == 0, False, [("xTf", c // 4), "wr0", "wr1"], [("ps", 4)])
            MM(PS[4][:, 0:36], ones1[:, :], brow[:, :], False, True, ["ones1", "br0", "br1"], [("ps", 4)])
            vcopy(lg[:], PS[4][:, 0:36], [("ps", 4)], ["lg"])
            V(lambda e: e.tensor_reduce(out=sm[:, 0:1], in_=lg[:, 0:4], axis=AX.X, op=ALU.max), ["lg"], [("sm", 0)])
            V(lambda e: e.tensor_scalar(out=sm[:, 1:2], in0=sm[:, 0:1], scalar1=-1.0, scalar2=None, op0=ALU.mult), [("sm", 0)], [("sm", 1)])
            A(lambda e: e.activation(out=gm[:], in_=lg[:, 0:4], func=AF.Exp, bias=sm[:, 1:2]), ["lg", ("sm", 1)], ["gm"])
            V(lambda e: e.tensor_reduce(out=sm[:, 2:3], in_=gm[:], axis=AX.X, op=ALU.add), ["gm"], [("sm", 2)])
            V(lambda e: e.reciprocal(out=sm[:, 3:4], in_=sm[:, 2:3]), [("sm", 2)], [("sm", 3)])
            V(lambda e: e.tensor_scalar(out=gm[:], in0=lg[:, 0:4], scalar1=sm[:, 0:1], scalar2=None, op0=ALU.is_ge), ["lg", ("sm", 0), "gm"], ["gm"])
            V(lambda e: e.tensor_scalar(out=es[:], in0=lg[:, 4:12], scalar1=gm[:, 0:1], scalar2=None, op0=ALU.mult), ["lg", "gm"], ["es"])
            for g in range(1, 4):
                V(lambda e, g=g: e.scalar_tensor_tensor(out=es[:], in0=lg[:, 4 + 8 * g:12 + 8 * g], scalar=gm[:, g:g + 1], in1=es[:], op0=ALU.mult, op1=ALU.add),
                  ["lg", "gm", "es"], ["es"])
            V(lambda e: e.max(out=t8[:], in_=es[:]), ["es"], ["t8"])
            V(lambda e: e.tensor_tensor(out=sm[:, 4:5], in0=t8[:, 0:1], in1=t8[:, 1:2], op=ALU.subtract), ["t8"], [("sm", 4)])
            A(lambda e: e.activation(out=sm[:, 5:6], in_=sm[:, 4:5], func=AF.Sigmoid), [("sm", 4)], [("sm", 5)])
            V(lambda e: e.tensor_tensor(out=sm[:, 6:7], in0=sm[:, 5:6], in1=sm[:, 3:4], op=ALU.mult), [("sm", 5), ("sm", 3)], [("sm", 6)])
            V(lambda e: e.tensor_tensor(out=sm[:, 7:8], in0=sm[:, 3:4], in1=sm[:, 6:7], op=ALU.subtract), [("sm", 6), ("sm", 3)], [("sm", 7)])
            for g in range(4):
                eg = lg[:, 4 + 8 * g:12 + 8 * g]
                V(lambda e, eg=eg: e.tensor_scalar(out=ta[:], in0=eg, scalar1=t8[:, 0:1], scalar2=sm[:, 6:7], op0=ALU.is_equal, op1=ALU.mult),
                  ["lg", "t8", ("sm", 6), "ta"], ["ta"])
                V(lambda e, eg=eg: e.tensor_scalar(out=tb[:], in0=eg, scalar1=t8[:, 1:2], scalar2=sm[:, 7:8], op0=ALU.is_equal, op1=ALU.mult),
                  ["lg", "t8", ("sm", 7), "tb"], ["tb"])
                V(lambda e: e.tensor_tensor(out=ta[:], in0=ta[:], in1=tb[:], op=ALU.add), ["ta", "tb"], ["ta"])
                V(lambda e, g=g: e.tensor_scalar(out=cw[:, 8 * g:8 * g + 8], in0=ta[:], scalar1=gm[:, g:g + 1], scalar2=None, op0=ALU.mult),
                  ["ta", "gm", "cw"], ["cw"])
            TR(PS[5][0:32, 0:128], cw[:, :], ident[:], ["cw", "ident"], [("ps", 5)])
            vcopy(cwT[:, tt * 128:(tt + 1) * 128], PS[5][0:32, 0:128], [("ps", 5)], [("cwT", tt)])
        s.pop()
        s.push()
        wgu = [s.sb([128, 8, 1024], BF16, f"wgu{i}") for i in range(2)]
        wd = [s.sb([128, 4, 1024], BF16, f"wd{i}") for i in range(2)]
        actT = s.sb([128, 4, S], BF16, "actT")
        sl = [s.sb([128, 512], F32, f"sl{i}") for i in range(2)]
        k = 0
        for ee in range(32):
            wi = ee % 2
            DMA("gpsimd", wgu[wi][:], D["moe_w_gu"][l, ee].rearrange("(c p) m -> p c m", p=128), w=[("wgu", wi)])
            DMA("gpsimd", wd[wi][:], D["moe_w_down"][l, ee].rearrange("(c p) m -> p c m", p=128), w=[("wd", wi)])
            for tq in range(4):
                MM(PS[7][:], selrow[:, ee, :], cwT[:, tqs(tq)], True, True, ["selrow2"] + [("cwT", tt) for tt in range(4 * tq, 4 * tq + 4)], [("ps", 7)])
                for fc in range(4):
                    k += 1
                    bg = 0 + k % 2
                    bu = 2 + k % 2
                    for c in range(8):
                        MM(PS[bg][:], wgu[wi][:, c, fc * 128:(fc + 1) * 128], xT[:, c, tqs(tq)], c == 0, c == 7, [("wgu", wi)] + xkeys(tq), [("ps", bg)])
                    for c in range(8):
                        MM(PS[bu][:], wgu[wi][:, c, 512 + fc * 128:512 + (fc + 1) * 128], xT[:, c, tqs(tq)], c == 0, c == 7, [("wgu", wi)] + xkeys(tq), [("ps", bu)])
                    A(lambda e, bg=bg, k=k: e.activation(out=sl[k % 2][:], in_=PS[bg][:], func=AF.Silu), [("ps", bg)], [("sl", k % 2)])
                    V(lambda e, bu=bu, k=k: e.tensor_tensor(out=sl[k % 2][:], in0=sl[k % 2][:], in1=PS[bu][:], op=ALU.mult), [("sl", k % 2), ("ps", bu)], [("sl", k % 2)])
                    V(lambda e, k=k, fc=fc, tq=tq: e.tensor_tensor(out=actT[:, fc, tqs(tq)], in0=sl[k % 2][:], in1=PS[7][:], op=ALU.mult),
                      [("sl", k % 2), ("ps", 7)], [("actT", fc, tq)])
            for tt in range(16):
                for hf in range(2):
                    b = 4 + (2 * tt + hf) % 2
                    for fc in range(4):
                        MM(PS[b][:], actT[:, fc, tt * 128:(tt + 1) * 128], wd[wi][:, fc, hf * 512:(hf + 1) * 512], fc == 0, fc == 3,
                           [("wd", wi), ("actT", fc, tt // 4)], [("ps", b)])
                    ya = yacc[:, tt, hf * 512:(hf + 1) * 512]
                    if ee == 0:
                        vcopy(ya, PS[b][:], [("ps", b)], [("yacc", tt, hf)])
                    else:
                        V(lambda e, ya=ya, b=b: e.tensor_tensor(out=ya, in0=ya, in1=PS[b][:], op=ALU.add), [("ps", b), ("yacc", tt, hf)], [("yacc", tt, hf)])
        s.pop()
        xt3 = [s.sb([128, DM], F32, f"xt3{i}") for i in range(2)]
        lng_, lnb_ = s.sb([128, DM], F32, "lng"), s.sb([128, DM], F32, "lnb")
        LN["g"], LN["b"] = lng_, lnb_
        load_ln(l, "ln3_g", "ln3_b")
        for tt in range(16):
            xt_ = xt3[tt % 2]
            key = ("xt3", tt % 2)
            DMA("sync", xt_[:], xs[tt * 128:(tt + 1) * 128, :], w=[key])
            V(lambda e, xt_=xt_, tt=tt: e.scalar_tensor_tensor(out=xt_[:], in0=xt_[:], scalar=DN_ALPHA, in1=yacc[:, tt, :], op0=ALU.mult, op1=ALU.add),
              [key, ("yacc", tt, 0), ("yacc", tt, 1)], [key])
            ln_tile(xt_, key, tt, dst, do_T=not last)
        s.pop()

    def mixer(l, xsrc):
        W = D["w_in"][l]
        s.push()
        oT = s.sb([128, 4, 2, S], BF16, "oT")
        s.push()
        oacc = s.sb([128, 16, 256], F32, "oacc")
        AT["P"] = [s.sb([128, 512], BF16, f"P{i}") for i in range(3)]
        AT["rd"] = s.sb([128, 4], F32, "rd")
        nsa(l, W, oacc)
        to_fm(oacc, 2, oT[:, 0], ("oT", 0))
        sbranch(l, W, oacc)
        to_fm(oacc, 2, oT[:, 1], ("oT", 1))
        rglru(l, W, oT)
        mla(l, W, oacc)
        to_fm(oacc, 2, oT[:, 3], ("oT", 3))
        s.pop()
        if l == 0:
            dump_sb("oT", oT[:], [128, 4, 2, S])
        merge_ln1(l, W, oT, xsrc)
        s.pop()

    load_xT(D["x"])
    for l in range(NL):
        s.push()
        wbuf = [s.sb([128, 8, 512], BF16, f"wbuf{i}") for i in range(3)]
        masks = s.sb([128, 12, 512], BF16, "masks")
        DMA("gpsimd", masks[:], D["c_masks"], w=["masks"])
        if "mix" in stages:
            mixer(l, D["x"] if l == 0 else xs)
        if "cross" in stages:
            cross(l)
        s.pop()
        if "moe" in stages:
            last = l == NL - 1
            moe(l, out_d if last else xs, last)
    if "moe" not in stages:
        s.barrier()
        s.push()
        tmp = s.sb([128, DM], F32, "fin")
        for tt in range(16):
            DMA("sync", tmp[:], xs[tt * 128:(tt + 1) * 128, :], w=["fin"])
            DMA("sync", out_d[tt * 128:(tt + 1) * 128, :], tmp[:], r=["fin"])
        s.pop()
    s.barrier()
    s.emit()
    return nc, dumps


_CACHE = {}


def kernel(**inputs):
    NL = 4
    if "nc" not in _CACHE:
        _CACHE["nc"] = build(NL)[0]
    nc = _CACHE["nc"]
    HC = host_consts()
    shared = {n: np.ascontiguousarray(np.asarray(inputs[n], dtype=np.float32)) for n in WNAMES}
    for n, a in HC.items():
        shared["c_" + n] = a
    x = np.asarray(inputs["x"], dtype=np.float32)
    mem = np.asarray(inputs["mem"], dtype=np.float32)
    in_maps = []
    for b in range(8):
        m = dict(shared)
        m["x"] = np.ascontiguousarray(x[b])
        m["mem"] = np.ascontiguousarray(mem[b])
        in_maps.append(m)
    res = run_bass_kernel_spmd(nc, in_maps, core_ids=list(range(8)))
    return np.stack([np.asarray(r["out"], dtype=np.float32) for r in res.results], axis=0)
```

```python
import numpy as np
import contextlib
import concourse.bass as bass
import concourse.mybir as mybir
from concourse.bass_utils import run_bass_kernel_spmd

F32 = mybir.dt.float32
BF16 = mybir.dt.bfloat16
AF = mybir.ActivationFunctionType
ALU = mybir.AluOpType
AX = mybir.AxisListType
ENGS = ("tensor", "vector", "scalar", "gpsimd", "sync")
DSIZE = {F32: 4, BF16: 2}
NEG = -30000.0
S = 2048
DM = 1024
DN_ALPHA = (2.0 * 4) ** 0.25
CAP = 384
NSL = 32 * CAP
I32 = mybir.dt.int32


class Sched:
    NSLOT = 4

    def __init__(self, nc):
        self.nc = nc
        self.ops = {e: [] for e in ENGS}
        self.ncomp = {e: 0 for e in ENGS}
        self.ndma = {e: 0 for e in ENGS}
        self.lastw = {}
        self.readers = {}
        self.synced = {e: {} for e in ENGS}
        self.sb_off = 16640
        self.sb_stack = []
        self.uid = 0

    def sb(self, shape, dtype, name=None):
        self.uid += 1
        name = f"{name or 't'}_{self.uid}"
        nbytes = int(np.prod(shape[1:])) * DSIZE[dtype]
        off = (self.sb_off + 63) // 64 * 64
        assert off + nbytes <= 228000, f"SBUF overflow {name} {off + nbytes}"
        t = self.nc.alloc_sbuf_tensor_at(name, list(shape), dtype, offset=off)
        self.sb_off = off + nbytes
        return t

    def push(self):
        self.sb_stack.append(self.sb_off)

    def pop(self):
        self.barrier()
        self.sb_off = self.sb_stack.pop()

    def _need(self, E, dep, waits):
        key, val = dep
        if key == ("c", "tensor") and E == "tensor":
            return
        if self.synced[E].get(key, 0) >= val:
            return
        if waits.get(key, 0) < val:
            waits[key] = val

    def op(self, E, fn, reads=(), writes=(), dma=False):
        waits = {}
        for r in reads:
            lw = self.lastw.get(r)
            if lw is not None:
                self._need(E, lw, waits)
        for w in writes:
            lw = self.lastw.get(w)
            if lw is not None:
                self._need(E, lw, waits)
            for k, v in self.readers.get(w, {}).items():
                self._need(E, (k, v), waits)
        if dma:
            k = self.ndma[E]
            self.ndma[E] += 1
            key = ("d", E, k % self.NSLOT)
            val = 16 * (k // self.NSLOT + 1)
            if val > 16:
                self._need(E, (key, val - 16), waits)
        else:
            self.ncomp[E] += 1
            key = ("c", E)
            val = self.ncomp[E]
        for k_, v_ in waits.items():
            self.synced[E][k_] = v_
        me = (key, val)
        self.ops[E].append((fn, waits, me))
        for r in reads:
            d = self.readers.setdefault(r, {})
            if d.get(key, 0) < val:
                d[key] = val
        for w in writes:
            self.lastw[w] = me
            self.readers[w] = {}
        return me

    def raw(self, E, fn):
        self.ops[E].append((fn, {}, None))

    def barrier(self):
        state = {}
        for e in ENGS:
            if self.ncomp[e]:
                state[("c", e)] = self.ncomp[e]
            for sl in range(self.NSLOT):
                k = self.ndma[e]
                cnt = (k - sl + self.NSLOT - 1) // self.NSLOT if k > sl else 0
                if cnt:
                    state[("d", e, sl)] = 16 * cnt
        for e in ENGS:
            waits = {}
            for k_, v_ in state.items():
                if self.synced[e].get(k_, 0) < v_:
                    waits[k_] = v_
                    self.synced[e][k_] = v_
            if waits:
                self.ops[e].append((None, waits, None))
        self.lastw = {}
        self.readers = {}

    def emit(self):
        nc = self.nc
        sems = {}
        with contextlib.ExitStack() as st:
            for e in ENGS:
                sems[("c", e)] = st.enter_context(nc.semaphore(f"c_{e}"))
                for sl in range(self.NSLOT):
                    sems[("d", e, sl)] = st.enter_context(nc.semaphore(f"d_{e}_{sl}"))
            block = st.enter_context(nc.Block())

            def run(eng, name):
                for fn, waits, me in self.ops[name]:
                    for k_, v_ in waits.items():
                        eng.wait_ge(sems[k_], v_)
                    if fn is not None and me is None:
                        fn(eng)
                    elif fn is not None:
                        fn(eng).then_inc(sems[me[0]], 16 if me[0][0] == "d" else 1)

            @block.tensor
            def _(e):
                run(e, "tensor")

            @block.vector
            def _(e):
                run(e, "vector")

            @block.scalar
            def _(e):
                run(e, "scalar")

            @block.gpsimd
            def _(e):
                run(e, "gpsimd")

            @block.sync
            def _(e):
                run(e, "sync")


def host_consts():
    c = {}
    c["ident"] = np.eye(128, dtype=np.float32)
    t = np.arange(S)
    slopes = 2.0 ** (-2.0 * (np.arange(4) + 1))
    qaug = np.zeros((4, 4, S), np.float32)
    for h in range(4):
        qaug[h, 0] = -slopes[h] * 128 * (t // 128)
        qaug[h, 1] = -slopes[h] * (t % 128)
        qaug[h, 2] = slopes[h]
        qaug[h, 3] = slopes[h]
    c["qaug"] = qaug
    c["kaug"] = np.stack([np.ones(S), np.ones(S), 128.0 * (t // 128), (t % 128)]).astype(np.float32)
    be = np.arange(128) * 16 + 31
    c["kcaug"] = np.stack([np.ones(128), np.ones(128), 128.0 * (be // 128), (be % 128)]).astype(np.float32)
    k = np.arange(128)[:, None]
    q = np.arange(512)[None, :]
    masks = np.zeros((128, 12, 512), np.float32)
    for j in range(4):
        masks[:, j] = np.where(q >= 128 * j + k, 0.0, NEG)
        masks[:, 4 + j] = np.where(128 * j + k < q, 0.0, NEG)
        masks[:, 8 + j] = np.where(q < 128 * j + k, 0.0, NEG)
    c["masks"] = masks
    cc = np.arange(128)[:, None]
    c["cmask"] = np.where((t[None, :] >= 16 * cc + 31) & (cc < 127), 0.0, NEG).astype(np.float32)
    tt = t[:, None]
    j = np.arange(32)[None, :]
    forced = (j == 0) | (j == tt // 64)
    valid = j * 64 <= tt
    vm = (valid & ~forced).astype(np.float32)
    am = np.where(forced, 1e4, np.where(valid, 0.0, -1.0)).astype(np.float32)
    c["impvm"] = vm.reshape(16, 128, 32).transpose(1, 0, 2).copy()
    c["impam"] = am.reshape(16, 128, 32).transpose(1, 0, 2).copy()
    E = np.zeros((32, 16, 128), np.float32)
    for kc in range(16):
        for kk in range(128):
            E[2 * kc + kk // 64, kc, kk] = 1.0
    c["selE"] = E
    c0 = np.arange(128)[:, None] * 16
    j0 = np.arange(32)[None, :] * 64
    cover = np.clip(np.minimum(c0 + 32, j0 + 64) - np.maximum(c0, j0), 0, None) / 32.0
    cover[127] = 0.0
    c["cover"] = cover.astype(np.float32)
    inv = (10000.0 ** (-np.arange(0, 32, 2, dtype=np.float32) / 32)).astype(np.float32)
    ang = t.astype(np.float32)[:, None] * inv[None, :]
    cs, sn = np.cos(ang).astype(np.float32).T, np.sin(ang).astype(np.float32).T
    rc = np.zeros((96, S), np.float32)
    rs = np.zeros((96, S), np.float32)
    rc[64:80] = cs
    rc[80:96] = cs
    rs[64:80] = -sn
    rs[80:96] = sn
    c["ropec"] = rc
    c["ropes"] = rs
    jj = np.arange(128)[:, None]
    ss = np.arange(128)[None, :]
    c["negU"] = np.where(jj >= ss, -1.0, 0.0).astype(np.float32)
    sr = np.zeros((32, 32, 128), np.float32)
    for e_ in range(32):
        sr[e_, e_, :] = 1.0
    c["selrow"] = sr
    c["ltri"] = (jj < ss).astype(np.float32)
    c["ebase"] = np.tile((np.arange(32) * CAP + 1).astype(np.float32)[None, :], (128, 1))
    return c


WNAMES = ["w_in", "nsa_cmp_pos", "nsa_cmp_w1", "nsa_cmp_w2", "rnn_conv_w", "rnn_conv_b", "rnn_ga_w", "rnn_ga_b",
          "rnn_gx_w", "rnn_gx_b", "rnn_lambda", "mla_q_norm", "mla_kv_norm", "mla_w_uq", "mla_w_ukv", "w_branch",
          "w_out", "ln1_g", "ln1_b", "x_wq", "x_wkv", "x_wo", "ln2_g", "ln2_b", "moe_rg_w", "moe_rg_b", "moe_re_w",
          "moe_re_b", "moe_w_gu", "moe_w_down", "ln3_g", "ln3_b"]
WSHAPES = {"w_in": (1024, 6764), "nsa_cmp_pos": (2, 32, 64), "nsa_cmp_w1": (2, 2048, 256), "nsa_cmp_w2": (2, 256, 64),
           "rnn_conv_w": (4, 256), "rnn_conv_b": (256,), "rnn_ga_w": (4, 64, 64), "rnn_ga_b": (256,),
           "rnn_gx_w": (4, 64, 64), "rnn_gx_b": (256,), "rnn_lambda": (256,), "mla_q_norm": (192,),
           "mla_kv_norm": (128,), "mla_w_uq": (192, 384), "mla_w_ukv": (128, 512), "w_branch": (4, 256, 1024),
           "w_out": (1024, 1024), "ln1_g": (1024,), "ln1_b": (1024,), "x_wq": (1024, 512), "x_wkv": (1024, 1024),
           "x_wo": (512, 1024), "ln2_g": (1024,), "ln2_b": (1024,), "moe_rg_w": (1024, 4), "moe_rg_b": (4,),
           "moe_re_w": (1024, 32), "moe_re_b": (32,), "moe_w_gu": (32, 1024, 1024), "moe_w_down": (32, 512, 1024),
           "ln3_g": (1024,), "ln3_b": (1024,)}


def build(NL=4, stages=("mix", "cross", "moe"), dump=None):
    nc = bass.Bass("TRN2", target_bir_lowering=False)
    s = Sched(nc)
    D = {}

    def din(name, shape):
        D[name] = nc.dram_tensor(name, list(shape), F32, kind="ExternalInput").ap()
        return D[name]

    din("x", (S, DM))
    din("mem", (256, DM))
    for n in WNAMES:
        din(n, (NL,) + WSHAPES[n])
    HC = host_consts()
    for n, a in HC.items():
        din("c_" + n, a.shape)
    out_d = nc.dram_tensor("out", [S, DM], F32, kind="ExternalOutput").ap()
    xs = nc.dram_tensor("xs_scr", [S, DM], F32, kind="Internal").ap()
    xg = nc.dram_tensor("xg_scr", [NSL, DM], F32, kind="Internal").ap()
    yg = nc.dram_tensor("yg_scr", [NSL, DM], F32, kind="Internal").ap()
    dumps = {}

    PS = [nc.alloc_psum_tensor(f"ps{i}", [128, 512], F32) for i in range(8)]
    bc_reg = nc.gpsimd.alloc_register("bc_reg")
    s.raw("gpsimd", lambda e: e.reg_mov(bc_reg, NSL - 1))

    def V(fn, r=(), w=()):
        return s.op("vector", fn, r, w)

    def A(fn, r=(), w=()):
        return s.op("scalar", fn, r, w)

    def G(fn, r=(), w=()):
        return s.op("gpsimd", fn, r, w)

    def MM(out, lhsT, rhs, start, stop, r, w):
        return s.op("tensor", lambda e: e.matmul(out, lhsT=lhsT, rhs=rhs, start=start, stop=stop), r, w)

    def MMs(out, lhsT, rhs, start, stop, r, w):
        return s.op("tensor", lambda e: e.matmul(out, lhsT=lhsT, rhs=rhs, start=start, stop=stop, skip_group_check=True), r, w)

    def TR(out, in_, idn, r, w):
        return s.op("tensor", lambda e: e.transpose(out, in_, idn), r, w)

    def DMA(q, out, in_, r=(), w=()):
        return s.op(q, lambda e: e.dma_start(out=out, in_=in_), r, w, dma=True)

    def vcopy(out, in_, r, w):
        return V(lambda e: e.tensor_copy(out=out, in_=in_), r, w)

    def acopy(out, in_, r, w):
        return A(lambda e: e.activation(out=out, in_=in_, func=AF.Copy), r, w)

    def dump_sb(name, ap, shape):
        if dump is None or name not in dump:
            return
        d = nc.dram_tensor("dbg_" + name, list(shape), ap.dtype if hasattr(ap, "dtype") else F32, kind="ExternalOutput").ap()
        dumps[name] = d
        s.barrier()
        DMA("sync", d, ap)
        s.barrier()

    ident = s.sb([128, 128], F32, "ident")
    identb = s.sb([128, 128], BF16, "identb")
    xT = s.sb([128, 8, S], BF16, "xT")
    wbuf = None
    masks = None
    stat = s.sb([128, 2, 6], F32, "stat")
    mv = s.sb([128, 4], F32, "mv")
    DMA("sync", ident[:], D["c_ident"], w=["ident"])
    DMA("gpsimd", identb[:], D["c_ident"], w=["identb"])
    cnt = {"w": 0, "g": 0}

    def next_wbuf():
        cnt["w"] += 1
        return cnt["w"] % 3

    def gbank():
        cnt["g"] += 1
        return 4 + cnt["g"] % 2

    def xkeys(tq):
        return [("xT", 4 * tq + u) for u in range(4)]

    def transpose_tile(src, key, tt, f32dst=None, f32key=None):
        for half in range(2):
            b = 6 + half
            for j in range(4):
                c = half * 4 + j
                TR(PS[b][:, j * 128:(j + 1) * 128], src[:, c * 128:(c + 1) * 128], ident[:], [key, "ident"], [("ps", b)])
            vcopy(xT[:, half * 4:(half + 1) * 4, tt * 128:(tt + 1) * 128], PS[b][:].rearrange("p (j q) -> p j q", j=4),
                  [("ps", b)], [("xT", tt)])
            if f32dst is not None:
                acopy(f32dst[:, half * 4:(half + 1) * 4, :], PS[b][:].rearrange("p (j q) -> p j q", j=4), [("ps", b)], [f32key])

    def load_xT(src):
        s.push()
        xtile = [s.sb([128, DM], F32, f"xtile{i}") for i in range(2)]
        for tt in range(16):
            xt = xtile[tt % 2]
            DMA("sync", xt[:], src[tt * 128:(tt + 1) * 128, :], w=[("xtile", tt % 2)])
            transpose_tile(xt, ("xtile", tt % 2), tt)
        s.pop()

    def load_ln(l, gname, bname):
        DMA("sync", LN["g"][:], D[gname][l].partition_broadcast(128), w=["lng"])
        DMA("sync", LN["b"][:], D[bname][l].partition_broadcast(128), w=["lnb"])

    def ln_tile(xt, key, tt, dst, f32dst=None, f32key=None, do_T=True):
        for hf in range(2):
            V(lambda e, hf=hf: e.bn_stats(out=stat[:, hf, :], in_=xt[:, hf * 512:(hf + 1) * 512]), [key], ["stat"])
        V(lambda e: e.bn_aggr(out=mv[:, 0:2], in_=stat[:]), ["stat"], ["mv"])
        V(lambda e: e.tensor_scalar(out=mv[:, 3:4], in0=mv[:, 1:2], scalar1=1e-5, scalar2=None, op0=ALU.add), ["mv"], ["mv3"])
        A(lambda e: e.activation(out=mv[:, 3:4], in_=mv[:, 3:4], func=AF.Ln), ["mv3"], ["mv3"])
        A(lambda e: e.activation(out=mv[:, 2:3], in_=mv[:, 3:4], func=AF.Exp, scale=-0.5), ["mv3"], ["mv2"])
        V(lambda e: e.tensor_scalar(out=xt[:], in0=xt[:], scalar1=mv[:, 0:1], scalar2=mv[:, 2:3], op0=ALU.subtract,
                                    op1=ALU.mult), [key, "mv", "mv2"], [key])
        lg_, lb_ = LN["g"], LN["b"]
        G(lambda e: e.tensor_tensor(out=xt[:], in0=xt[:], in1=lg_[:], op=ALU.mult), [key, "lng"], [key])
        G(lambda e: e.tensor_tensor(out=xt[:], in0=xt[:], in1=lb_[:], op=ALU.add), [key, "lnb"], [key])
        DMA("sync", dst[tt * 128:(tt + 1) * 128, :], xt[:], r=[key], w=[("xs", tt)])
        if do_T:
            transpose_tile(xt, key, tt, f32dst, f32key)

    def proj_fm(pieces, m, evac, kchunks=8, rhsT=None, rkeys=None):
        i = next_wbuf()
        wb = wbuf[i]
        wk = []
        for pi, (o, ap) in enumerate(pieces):
            wd = ap.shape[1]
            DMA("gpsimd", wb[:, 0:kchunks, o:o + wd], ap.rearrange("(c p) m -> p c m", p=128), w=[("wbuf", i, pi)])
            wk.append(("wbuf", i, pi))
        for tq in range(4):
            b = gbank()
            for c in range(kchunks):
                MM(PS[b][0:m, :], wb[:, c, 0:m], xT[:, c, tq * 512:(tq + 1) * 512], c == 0, c == kchunks - 1,
                   wk + xkeys(tq), [("ps", b)])
            evac(PS[b], ("ps", b), tq)

    def proj_tm(pieces, n, evac):
        i = next_wbuf()
        wb = wbuf[i]
        wk = []
        for pi, (o, ap) in enumerate(pieces):
            wd = ap.shape[1]
            DMA("gpsimd", wb[:, :, o:o + wd], ap.rearrange("(c p) m -> p c m", p=128), w=[("wbuf", i, pi)])
            wk.append(("wbuf", i, pi))
        for tt in range(16):
            b = gbank()
            for c in range(8):
                MM(PS[b][:, 0:n], xT[:, c, tt * 128:(tt + 1) * 128], wb[:, c, 0:n], c == 0, c == 7,
                   wk + [("xT", tt)], [("ps", b)])
            evac(PS[b], ("ps", b), tt)

    ctr = {"sc": 0, "p": 0}
    AT = {}

    def attn(terms, kcs, vaug, ncols, pv_ok, accmap, scale=1.0):
        Pt = AT["P"]
        kcs = list(kcs)
        rng = {}
        for qs in range(4):
            ok = [k for k in kcs if pv_ok(k, qs)]
            rng[qs] = (ok[0], ok[-1])

        def score(kc):
            ctr["sc"] += 1
            sbk = ctr["sc"] % 2
            tl = terms(kc)
            for i, (lt, rh, rk) in enumerate(tl):
                MM(PS[sbk][:], lt, rh, i == 0, i == len(tl) - 1, rk, [("ps", sbk)])
            return sbk

        nxt = score(kcs[0])
        started = set()
        for idx, kc in enumerate(kcs):
            sbk = nxt
            ctr["p"] += 1
            pi = ctr["p"] % 3
            A(lambda e, pi=pi, sbk=sbk: e.activation(out=Pt[pi][:], in_=PS[sbk][:], func=AF.Exp, scale=scale),
              [("ps", sbk)], [("P", pi)])
            if idx + 1 < len(kcs):
                nxt = score(kcs[idx + 1])
            vap, vkey = vaug(kc)
            for qs in range(4):
                if not pv_ok(kc, qs):
                    continue
                bank, c0 = accmap(qs)
                first = bank not in started
                started.add(bank)
                MMs(PS[bank][:, c0:c0 + ncols], Pt[pi][:, qs * 128:(qs + 1) * 128], vap, first, kc == rng[qs][1],
                    [("P", pi), vkey], [("ps", bank)])

    def attn_out(accmap, dv, tt0, gate, dst, accumulate, normalize=True):
        rd = AT["rd"]
        for qs in range(4):
            bank, c0 = accmap(qs)
            tt = tt0 + qs
            src = PS[bank][:, c0:c0 + dv]
            if not normalize:
                vcopy(dst(tt), src, [("ps", bank)], [("oacc", tt)])
                continue
            V(lambda e, bank=bank, c0=c0: e.tensor_scalar(out=rd[:, 2:3], in0=PS[bank][:, c0 + dv:c0 + dv + 1], scalar1=1e-30,
                                                         scalar2=None, op0=ALU.max), [("ps", bank)], ["rd2"])
            V(lambda e: e.reciprocal(out=rd[:, 0:1], in_=rd[:, 2:3]), ["rd2"], ["rd0"])
            sc = rd[:, 0:1]
            rk = ["rd0"]
            if gate is not None:
                gap = gate(tt)
                V(lambda e, gap=gap: e.tensor_tensor(out=rd[:, 1:2], in0=rd[:, 0:1], in1=gap, op=ALU.mult), ["rd0", "gate"], ["rd1"])
                sc = rd[:, 1:2]
                rk = ["rd1"]
            d = dst(tt)
            if accumulate:
                V(lambda e, d=d, src=src, sc=sc: e.scalar_tensor_tensor(out=d, in0=src, scalar=sc, in1=d, op0=ALU.mult, op1=ALU.add),
                  [("ps", bank), ("oacc", tt)] + rk, [("oacc", tt)])
            else:
                V(lambda e, d=d, src=src, sc=sc: e.tensor_scalar(out=d, in0=src, scalar1=sc, scalar2=None, op0=ALU.mult),
                  [("ps", bank)] + rk, [("oacc", tt)])

    def to_fm(src, nchunk, dstT, dkey):
        for tt in range(16):
            b = 6 + tt % 2
            for c in range(nchunk):
                TR(PS[b][:, c * 128:(c + 1) * 128], src[:, tt, c * 128:(c + 1) * 128], ident[:], [("oacc", tt), "ident"], [("ps", b)])
            vcopy(dstT[:, 0:nchunk, tt * 128:(tt + 1) * 128], PS[b][:, 0:nchunk * 128].rearrange("p (j q) -> p j q", j=nchunk),
                  [("ps", b)], [(dkey, tt)])

    def tqs(tq):
        return slice(tq * 512, (tq + 1) * 512)

    def nsa(l, W, oacc):
        s.push()
        qa = [s.sb([68, S], BF16, f"qa{h}") for h in range(4)]
        kcA = [s.sb([68, 128], BF16, f"kcA{g}") for g in range(2)]
        vcA = [s.sb([128, 97], BF16, f"vcA{g}") for g in range(2)]
        gate = s.sb([128, 16, 12], F32, "gate")
        selTb = [s.sb([32, S], BF16, f"selTb{g}") for g in range(2)]
        cmaskb = s.sb([128, S], BF16, "cmaskb")
        selE = s.sb([32, 16, 128], BF16, "selE")
        vm = s.sb([128, 16, 32], F32, "vm")
        am = s.sb([128, 16, 32], F32, "am")
        imp = s.sb([128, 32], F32, "imp")
        impf = s.sb([128, 32], F32, "impf")
        top8 = s.sb([128, 8], F32, "top8")
        selb = s.sb([128, 32], F32, "selb")
        rdn = s.sb([128, 8], F32, "rdn")
        DMA("gpsimd", cmaskb[:], D["c_cmask"], w=["cmaskb"])
        DMA("gpsimd", selE[:], D["c_selE"], w=["selE"])
        DMA("sync", vm[:], D["c_impvm"], w=["vm"])
        DMA("sync", am[:], D["c_impam"], w=["am"])
        for h in range(4):
            DMA("gpsimd", qa[h][64:68, :], D["c_qaug"][h], w=[("qa", h, "aug")])
        for g in range(2):
            DMA("gpsimd", kcA[g][64:68, :], D["c_kcaug"], w=[("kcA", g, "aug")])
            V(lambda e, g=g: e.memset(vcA[g][:, 0:64], 0.0), [], [("vcA", g, "v")])
            V(lambda e, g=g: e.memset(vcA[g][:, 64:65], 1.0), [], [("vcA", g, "one")])
            DMA("gpsimd", vcA[g][:, 65:97], D["c_cover"], w=[("vcA", g, "cov")])
            V(lambda e, g=g: e.memset(kcA[g][0:64, :], 0.0), [], [("kcA", g, "k")])
        for h in range(4):
            def ev(ps, key, tq, h=h):
                V(lambda e: e.tensor_scalar(out=qa[h][0:64, tqs(tq)], in0=ps[0:64, :], scalar1=0.125, scalar2=None, op0=ALU.mult),
                  [key], [("qa", h, tq)])
            proj_fm([(0, W[:, h * 64:(h + 1) * 64])], 64, ev)

        s.push()
        srcT = [s.sb([64, S], BF16, f"srcT{g}") for g in range(2)]
        w1t = s.sb([64, 32, 256], BF16, "w1t")
        w2t = s.sb([128, 2, 64], BF16, "w2t")
        posr = s.sb([32, 64], F32, "posr")
        posT = s.sb([64, 32], BF16, "posT")
        hb = s.sb([128, 2], F32, "hb")
        hidT = s.sb([128, 2, 128], BF16, "hidT")
        for j in range(2):
            for g in range(2):
                def ev(ps, key, tq, g=g):
                    vcopy(srcT[g][:, tqs(tq)], ps[0:64, :], [key], [("srcT", g, tq)])
                c0 = 256 + 128 * j + 64 * g
                proj_fm([(0, W[:, c0:c0 + 64])], 64, ev)
            DMA("gpsimd", w1t[:], D["nsa_cmp_w1"][l, j].rearrange("(l d) h -> d l h", d=64), w=["w1t"])
            DMA("gpsimd", w2t[:], D["nsa_cmp_w2"][l, j].rearrange("(c p) d -> p c d", p=128), w=["w2t"])
            DMA("sync", posr[:], D["nsa_cmp_pos"][l, j], w=["posr"])
            TR(PS[6][0:64, 0:32], posr[:, :], ident[0:32, 0:32], ["posr", "ident"], [("ps", 6)])
            vcopy(posT[:], PS[6][0:64, 0:32], [("ps", 6)], ["posT"])
            for hc in range(2):
                for li in range(32):
                    MM(PS[7][:, hc:hc + 1], w1t[:, li, hc * 128:(hc + 1) * 128], posT[:, li:li + 1], li == 0, li == 31,
                       ["w1t", "posT"], [("ps", 7)])
            vcopy(hb[:], PS[7][:, 0:2], [("ps", 7)], ["hb"])
            for g in range(2):
                sk = [("srcT", g, tq) for tq in range(4)]
                for hc in range(2):
                    b = gbank()
                    for li in range(32):
                        MM(PS[b][:, 0:127], w1t[:, li, hc * 128:(hc + 1) * 128], srcT[g][:, li:li + 2017:16], li == 0, li == 31,
                           ["w1t"] + sk, [("ps", b)])
                    A(lambda e, b=b, hc=hc: e.activation(out=hidT[:, hc, 0:127], in_=PS[b][:, 0:127], func=AF.Gelu, bias=hb[:, hc:hc + 1]),
                      [("ps", b), "hb"], [("hidT", hc)])
                b = gbank()
                if j == 0:
                    for hc in range(2):
                        MM(PS[b][0:64, 0:127], w2t[:, hc, :], hidT[:, hc, 0:127], hc == 0, hc == 1, ["w2t", ("hidT", hc)], [("ps", b)])
                    vcopy(kcA[g][0:64, 0:127], PS[b][0:64, 0:127], [("ps", b)], [("kcA", g, "k")])
                else:
                    for hc in range(2):
                        MM(PS[b][0:127, 0:64], hidT[:, hc, 0:127], w2t[:, hc, :], hc == 0, hc == 1, ["w2t", ("hidT", hc)], [("ps", b)])
                    vcopy(vcA[g][0:127, 0:64], PS[b][0:127, 0:64], [("ps", b)], [("vcA", g, "v")])
        s.pop()

        s.push()
        ksA = [s.sb([68, S], BF16, f"ksA{g}") for g in range(2)]
        kwA = [s.sb([68, S], BF16, f"kwA{g}") for g in range(2)]
        vsA = s.sb([128, 16, 2, 65], BF16, "vsA")
        vwA = s.sb([128, 16, 2, 65], BF16, "vwA")
        V(lambda e: e.memset(vsA[:, :, :, 64:65], 1.0), [], [("vsA", tt) for tt in range(16)])
        V(lambda e: e.memset(vwA[:, :, :, 64:65], 1.0), [], [("vwA", tt) for tt in range(16)])
        for g in range(2):
            DMA("gpsimd", ksA[g][64:68, :], D["c_kaug"], w=[("ksA", g, "aug")])
            DMA("gpsimd", kwA[g][64:68, :], D["c_kaug"], w=[("kwA", g, "aug")])
            for nm, dst, c0 in (("ksA", ksA, 512), ("kwA", kwA, 768)):
                def ev(ps, key, tq, dst=dst, nm=nm, g=g):
                    vcopy(dst[g][0:64, tqs(tq)], ps[0:64, :], [key], [(nm, g, tq)])
                proj_fm([(0, W[:, c0 + 64 * g:c0 + 64 * g + 64])], 64, ev)

        def evv(ps, key, tt):
            vcopy(vsA[:, tt, :, 0:64], ps[:, 0:128].rearrange("p (g d) -> p g d", g=2), [key], [("vsA", tt)])
            vcopy(vwA[:, tt, :, 0:64], ps[:, 128:256].rearrange("p (g d) -> p g d", g=2), [key], [("vwA", tt)])
            vcopy(gate[:, tt, :], ps[:, 256:268], [key], [("gateraw", tt)])
            A(lambda e: e.activation(out=gate[:, tt, :], in_=gate[:, tt, :], func=AF.Sigmoid), [("gateraw", tt)], ["gate"])
        proj_tm([(0, W[:, 640:768]), (128, W[:, 896:1024]), (256, W[:, 1024:1036])], 268, evv)

        for g in range(2):
            for qt in range(4):
                for n in range(2):
                    h = 2 * g + n
                    MM(PS[n][:], kcA[g][:, :], qa[h][:, tqs(qt)], True, False,
                       [("kcA", g, "k"), ("kcA", g, "aug"), ("qa", h, qt), ("qa", h, "aug")], [("ps", n)])
                    MM(PS[n][:], identb[:], cmaskb[:, tqs(qt)], False, True, ["identb", "cmaskb"], [("ps", n)])
                    Pn = AT["P"][n]
                    A(lambda e, n=n, Pn=Pn: e.activation(out=Pn[:], in_=PS[n][:], func=AF.Exp), [("ps", n)], [("P", n)])
                    for qs in range(4):
                        MM(PS[2 + n][:, qs * 97:(qs + 1) * 97], Pn[:, qs * 128:(qs + 1) * 128], vcA[g][:, :], True, True,
                           [("P", n), ("vcA", g, "v"), ("vcA", g, "one"), ("vcA", g, "cov")], [("ps", 2 + n)])
                for qs in range(4):
                    tt = 4 * qt + qs
                    for n in range(2):
                        h = 2 * g + n
                        c0 = qs * 97
                        V(lambda e, n=n, c0=c0: e.tensor_scalar(out=rdn[:, 4 + n:5 + n], in0=PS[2 + n][:, c0 + 64:c0 + 65], scalar1=1e-30,
                                                               scalar2=None, op0=ALU.max), [("ps", 2 + n)], [("rdn", 4 + n)])
                        V(lambda e, n=n: e.reciprocal(out=rdn[:, n:n + 1], in_=rdn[:, 4 + n:5 + n]), [("rdn", 4 + n)], [("rdn", n)])
                        V(lambda e, n=n, h=h, tt=tt: e.tensor_tensor(out=rdn[:, 2 + n:3 + n], in0=rdn[:, n:n + 1], in1=gate[:, tt, 3 * h:3 * h + 1],
                                                                      op=ALU.mult), [("rdn", n), "gate"], [("rdn", 2 + n)])
                        V(lambda e, n=n, h=h, tt=tt, c0=c0: e.tensor_scalar(out=oacc[:, tt, h * 64:(h + 1) * 64], in0=PS[2 + n][:, c0:c0 + 64],
                                                                         scalar1=rdn[:, 2 + n:3 + n], scalar2=None, op0=ALU.mult),
                          [("ps", 2 + n), ("rdn", 2 + n)], [("oacc", tt)])
                    c0 = qs * 97
                    V(lambda e, c0=c0: e.tensor_scalar(out=imp[:], in0=PS[2][:, c0 + 65:c0 + 97], scalar1=rdn[:, 0:1], scalar2=None, op0=ALU.mult),
                      [("ps", 2), ("rdn", 0)], ["imp"])
                    V(lambda e, c0=c0: e.scalar_tensor_tensor(out=imp[:], in0=PS[3][:, c0 + 65:c0 + 97], scalar=rdn[:, 1:2], in1=imp[:],
                                                             op0=ALU.mult, op1=ALU.add), [("ps", 3), ("rdn", 1), "imp"], ["imp"])
                    V(lambda e, tt=tt: e.tensor_tensor(out=impf[:], in0=imp[:], in1=vm[:, tt, :], op=ALU.mult), ["imp", "vm"], ["impf"])
                    V(lambda e, tt=tt: e.tensor_tensor(out=impf[:], in0=impf[:], in1=am[:, tt, :], op=ALU.add), ["impf", "am"], ["impf"])
                    V(lambda e: e.max(out=top8[:], in_=impf[:]), ["impf"], ["top8"])
                    V(lambda e: e.tensor_scalar(out=selb[:], in0=impf[:], scalar1=top8[:, 7:8], scalar2=None, op0=ALU.is_ge),
                      ["impf", "top8"], ["selb"])
                    V(lambda e: e.tensor_scalar(out=selb[:], in0=selb[:], scalar1=-1.0, scalar2=-NEG, op0=ALU.add, op1=ALU.mult),
                      ["selb"], ["selb"])
                    TR(PS[6][0:32, qs * 128:(qs + 1) * 128], selb[:, :], ident[:], ["selb", "ident"], [("ps", 6)])
                vcopy(selTb[g][:, tqs(qt)], PS[6][0:32, :], [("ps", 6)], [("selTb", g, qt)])

        it = 0
        for br, kA, vA, nm, vnm in ((1, ksA, vsA, "ksA", "vsA"), (2, kwA, vwA, "kwA", "vwA")):
            for h in range(4):
                g = h // 2
                for qt in range(4):
                    it += 1
                    bank = 2 + it % 2
                    if br == 1:
                        kcs = range(0, 4 * qt + 4)
                        pv_ok = lambda kc, qs, qt=qt: kc <= 4 * qt + qs
                    else:
                        kcs = range(max(0, 4 * qt - 4), 4 * qt + 4)
                        pv_ok = lambda kc, qs, qt=qt: 4 * qt + qs - 4 <= kc <= 4 * qt + qs

                    def terms(kc, h=h, g=g, qt=qt, br=br, kA=kA, nm=nm):
                        tl = [(kA[g][:, kc * 128:(kc + 1) * 128], qa[h][:, tqs(qt)],
                               [(nm, g, kc // 4), (nm, g, "aug"), ("qa", h, qt), ("qa", h, "aug")])]
                        if br == 1:
                            tl.append((selE[:, kc, :], selTb[g][:, tqs(qt)], ["selE", ("selTb", g, qt)]))
                        if kc >= 4 * qt:
                            tl.append((identb[:], masks[:, kc - 4 * qt, :], ["identb", "masks"]))
                        elif br == 2:
                            tl.append((identb[:], masks[:, 8 + kc - (4 * qt - 4), :], ["identb", "masks"]))
                        return tl

                    def vaug(kc, g=g, vA=vA, vnm=vnm):
                        return vA[:, kc, g, :], (vnm, kc)

                    accmap = lambda qs, bank=bank: (bank, qs * 65)
                    attn(terms, kcs, vaug, 65, pv_ok, accmap)
                    attn_out(accmap, 64, 4 * qt, lambda tt, h=h, br=br: gate[:, tt, 3 * h + br:3 * h + br + 1],
                             lambda tt, h=h: oacc[:, tt, h * 64:(h + 1) * 64], True)
        s.pop()
        s.pop()

    def sbranch(l, W, oacc):
        s.push()
        qT = [s.sb([64, S], BF16, f"sbq{h}") for h in range(4)]
        kT = [s.sb([64, S], BF16, f"sbk{h}") for h in range(4)]
        vB = s.sb([128, 16, 256], BF16, "sbv")
        negU = s.sb([128, 128], BF16, "negU")
        negO = s.sb([128, 128], F32, "negO")
        et = [s.sb([128, 512], F32, f"et{i}") for i in range(2)]
        spt = [s.sb([128, 512], BF16, f"spt{i}") for i in range(3)]
        acc = [s.sb([128, 512], F32, f"sbacc{i}") for i in range(2)]
        DMA("gpsimd", negU[:], D["c_negU"], w=["negU"])
        V(lambda e: e.memset(negO[:], -1.0), [], ["negO"])
        for h in range(4):
            def evq(ps, key, tq, h=h):
                V(lambda e: e.tensor_scalar(out=qT[h][:, tqs(tq)], in0=ps[0:64, :], scalar1=0.125, scalar2=None, op0=ALU.mult),
                  [key], [("sbq", h, tq)])
            proj_fm([(0, W[:, 1036 + h * 64:1036 + (h + 1) * 64])], 64, evq)

            def evk(ps, key, tq, h=h):
                vcopy(kT[h][:, tqs(tq)], ps[0:64, :], [key], [("sbk", h, tq)])
            proj_fm([(0, W[:, 1292 + h * 64:1292 + (h + 1) * 64])], 64, evk)

        def evv(ps, key, tt):
            vcopy(vB[:, tt, :], ps[:, 0:256], [key], [("sbv", tt)])
        proj_tm([(0, W[:, 1548:1804])], 256, evv)

        SB3 = (0, 1, 5)
        st = {"i": 0, "a": 0, "it": 0}
        for h in range(4):
            for qt in range(4):
                st["it"] += 1
                bank = 2 + st["it"] % 2
                kcs = list(range(4 * qt + 3, -1, -1))

                def stageA(kc, h=h, qt=qt):
                    st["i"] += 1
                    i = st["i"]
                    sbk = SB3[i % 3]
                    MM(PS[sbk][:], kT[h][:, kc * 128:(kc + 1) * 128], qT[h][:, tqs(qt)], True, False,
                       [("sbk", h, kc // 4), ("sbq", h, qt)], [("ps", sbk)])
                    if kc >= 4 * qt:
                        MM(PS[sbk][:], identb[:], masks[:, 4 + kc - 4 * qt, :], False, False, ["identb", "masks"], [("ps", sbk)])
                    A(lambda e: e.activation(out=et[i % 2][:], in_=PS[sbk][:], func=AF.Exp), [("ps", sbk)], [("et", i % 2)])
                    A(lambda e: e.activation(out=spt[i % 3][:], in_=et[i % 2][:], func=AF.Ln, bias=1.0), [("et", i % 2)], [("spt", i % 3)])
                    return i

                def stageB(kc, i, idx, h=h, qt=qt, bank=bank):
                    sbk = SB3[i % 3]
                    a = st["a"]
                    MM(PS[sbk][:], negU[:], spt[i % 3][:], False, idx == 0, ["negU", ("spt", i % 3)], [("ps", sbk)])
                    if idx > 0:
                        MM(PS[sbk][:], negO[:], acc[a % 2][:], False, True, ["negO", ("sbacc", a % 2)], [("ps", sbk)])
                    ctr["p"] += 1
                    pi = ctr["p"] % 3
                    Pp = AT["P"][pi]
                    A(lambda e: e.activation(out=Pp[:], in_=PS[sbk][:], func=AF.Exp), [("ps", sbk)], [("P", pi)])
                    for qs in range(4):
                        if kc > 4 * qt + qs:
                            continue
                        MMs(PS[bank][:, qs * 64:(qs + 1) * 64], Pp[:, qs * 128:(qs + 1) * 128], vB[:, kc, h * 64:(h + 1) * 64],
                            idx == 0 and qs == 3, kc == 0, [("P", pi), ("sbv", kc)], [("ps", bank)])
                    if kc > 0:
                        if idx == 0:
                            G(lambda e: e.tensor_copy(out=acc[(a + 1) % 2][:], in_=spt[i % 3][:]), [("spt", i % 3)], [("sbacc", (a + 1) % 2)])
                        else:
                            G(lambda e: e.tensor_tensor(out=acc[(a + 1) % 2][:], in0=acc[a % 2][:], in1=spt[i % 3][:], op=ALU.add),
                              [("spt", i % 3), ("sbacc", a % 2)], [("sbacc", (a + 1) % 2)])
                        st["a"] += 1

                cur = stageA(kcs[0])
                for idx, kc in enumerate(kcs):
                    nxt = stageA(kcs[idx + 1]) if idx + 1 < len(kcs) else None
                    stageB(kc, cur, idx)
                    cur = nxt
                accmap = lambda qs, bank=bank: (bank, qs * 64)
                attn_out(accmap, 64, 4 * qt, None, lambda tt, h=h: oacc[:, tt, h * 64:(h + 1) * 64], False, normalize=False)
        s.pop()

    def rglru(l, W, oT):
        s.push()
        xr = s.sb([128, S + 3], F32, "xr")
        xg = s.sb([128, S], F32, "xg")
        u = s.sb([128, S], F32, "u")
        ub = s.sb([128, S], BF16, "ub")
        ra = s.sb([128, S], F32, "ra")
        ib = s.sb([128, S], F32, "ib")
        hh = s.sb([128, S], F32, "hh")
        prm = s.sb([128, 12], F32, "prm")
        gw = [s.sb([128, 128], BF16, f"gw{i}") for i in range(2)]
        for ch in range(2):
            cs = slice(ch * 128, (ch + 1) * 128)
            for tap in range(4):
                DMA("sync", prm[:, tap:tap + 1], D["rnn_conv_w"][l, tap, cs].rearrange("(c o) -> c o", o=1), w=[("prm", tap)])
            for i, nm in enumerate(("rnn_conv_b", "rnn_ga_b", "rnn_gx_b", "rnn_lambda")):
                DMA("sync", prm[:, 4 + i:5 + i], D[nm][l, cs].rearrange("(c o) -> c o", o=1), w=[("prm", 4 + i)])
            for i, nm in enumerate(("rnn_ga_w", "rnn_gx_w")):
                V(lambda e, i=i: e.memset(gw[i][:], 0.0), [], [("gw", i)])
                for n in range(2):
                    DMA("gpsimd", gw[i][n * 64:(n + 1) * 64, n * 64:(n + 1) * 64], D[nm][l, 2 * ch + n], w=[("gw", i)])
            A(lambda e: e.activation(out=prm[:, 9:10], in_=prm[:, 7:8], func=AF.Exp, scale=-1.0), [("prm", 7)], [("prm", 9)])
            A(lambda e: e.activation(out=prm[:, 9:10], in_=prm[:, 9:10], func=AF.Ln, bias=1.0), [("prm", 9)], [("prm", 9)])
            V(lambda e: e.tensor_scalar(out=prm[:, 8:9], in0=prm[:, 9:10], scalar1=-8.0, scalar2=None, op0=ALU.mult), [("prm", 9)], [("prm", 8)])
            V(lambda e: e.memset(xr[:, 0:3], 0.0), [], [("xr", "pad")])

            def evx(ps, key, tq):
                vcopy(xr[:, 3 + tq * 512:3 + (tq + 1) * 512], ps[:, :], [key], [("xr", tq)])
            proj_fm([(0, W[:, 1804 + ch * 128:1804 + (ch + 1) * 128])], 128, evx)

            def evg(ps, key, tq):
                A(lambda e: e.activation(out=xg[:, tqs(tq)], in_=ps[:, :], func=AF.Gelu), [key], [("xg", tq)])
            proj_fm([(0, W[:, 2060 + ch * 128:2060 + (ch + 1) * 128])], 128, evg)
            xk = [("xr", tq) for tq in range(4)] + [("xr", "pad")]
            V(lambda e: e.tensor_scalar(out=u[:], in0=xr[:, 0:S], scalar1=prm[:, 0:1], scalar2=prm[:, 4:5], op0=ALU.mult, op1=ALU.add),
              xk + [("prm", 0), ("prm", 4)], ["u"])
            for tap in range(1, 4):
                V(lambda e, tap=tap: e.scalar_tensor_tensor(out=u[:], in0=xr[:, tap:tap + S], scalar=prm[:, tap:tap + 1], in1=u[:],
                                                            op0=ALU.mult, op1=ALU.add), xk + [("prm", tap), "u"], ["u"])
            vcopy(ub[:], u[:], ["u"], ["ub"])
            for tq in range(4):
                for i, dst, bcol in ((0, ra, 5), (1, ib, 6)):
                    b = gbank()
                    MM(PS[b][:], gw[i][:], ub[:, tqs(tq)], True, True, [("gw", i), "ub"], [("ps", b)])
                    A(lambda e, b=b, dst=dst, bcol=bcol, tq=tq: e.activation(out=dst[:, tqs(tq)], in_=PS[b][:], func=AF.Sigmoid,
                                                                             bias=prm[:, bcol:bcol + 1]), [("ps", b), ("prm", bcol)], [(("ra", "ib")[i], tq)])
            rk = [("ra", tq) for tq in range(4)]
            ik = [("ib", tq) for tq in range(4)]
            A(lambda e: e.activation(out=ra[:], in_=ra[:], func=AF.Exp, scale=prm[:, 8:9]), rk + [("prm", 8)], rk)
            V(lambda e: e.tensor_tensor(out=ib[:], in0=ib[:], in1=u[:], op=ALU.mult), ik + ["u"], ik)
            V(lambda e: e.tensor_tensor(out=hh[:], in0=ra[:], in1=ra[:], op=ALU.mult), rk, ["hh"])
            V(lambda e: e.tensor_scalar(out=hh[:], in0=hh[:], scalar1=-1.0, scalar2=1.0, op0=ALU.mult, op1=ALU.add), ["hh"], ["hh"])
            V(lambda e: e.tensor_scalar(out=hh[:], in0=hh[:], scalar1=0.0, scalar2=None, op0=ALU.max), ["hh"], ["hh"])
            A(lambda e: e.activation(out=hh[:], in_=hh[:], func=AF.Sqrt), ["hh"], ["hh"])
            V(lambda e: e.tensor_tensor(out=ib[:], in0=ib[:], in1=hh[:], op=ALU.mult), ik + ["hh"], ik)
            V(lambda e: e.tensor_tensor_scan(out=hh[:], data0=ra[:], data1=ib[:], initial=0.0, op0=ALU.mult, op1=ALU.add),
              rk + ik + ["hh"], ["hh"])
            V(lambda e, ch=ch: e.tensor_tensor(out=oT[:, 2, ch, :], in0=hh[:], in1=xg[:], op=ALU.mult),
              ["hh"] + [("xg", tq) for tq in range(4)], [("oT", 2, ch)])
        s.pop()

    def mla(l, W, oacc):
        s.push()
        cqn = s.sb([128, 2, S], BF16, "cqn")
        ckvn = s.sb([128, S], BF16, "ckvn")
        wuq = s.sb([128, 2, 384], BF16, "wuq")
        wuqS = s.sb([128, 2, 384], BF16, "wuqS")
        wukv = s.sb([128, 512], BF16, "wukv")
        gq = s.sb([128, 2], F32, "gq")
        gkv = s.sb([128, 1], F32, "gkv")
        onesF = s.sb([128, 128], F32, "onesF")
        V(lambda e: e.memset(onesF[:], 1.0), [], ["onesF"])
        V(lambda e: e.memset(wuqS[:], 0.0), [], ["wuqS"])
        V(lambda e: e.memset(cqn[:], 0.0), [], ["cqn0"])
        Wq = D["mla_w_uq"][l]
        DMA("gpsimd", wuq[:, 0, :], Wq[0:128, :], w=[("wuq", 0)])
        DMA("gpsimd", wuq[0:64, 1, :], Wq[128:192, :], w=[("wuq", 1)])
        for h in range(4):
            for (r0, r1, cidx) in ((0, 128, 0), (128, 192, 1)):
                np_ = r1 - r0
                DMA("gpsimd", wuqS[0:np_, cidx, h * 96 + 64:h * 96 + 80], Wq[r0:r1, h * 96 + 80:h * 96 + 96], r=["wuqS"], w=[("wuqS", h, cidx, 0)])
                DMA("gpsimd", wuqS[0:np_, cidx, h * 96 + 80:h * 96 + 96], Wq[r0:r1, h * 96 + 64:h * 96 + 80], r=["wuqS"], w=[("wuqS", h, cidx, 1)])
        DMA("gpsimd", wukv[:], D["mla_w_ukv"][l], w=["wukv"])
        DMA("sync", gq[:, 0:1], D["mla_q_norm"][l, 0:128].rearrange("(c o) -> c o", o=1), w=[("gq", 0)])
        DMA("sync", gq[0:64, 1:2], D["mla_q_norm"][l, 128:192].rearrange("(c o) -> c o", o=1), w=[("gq", 1)])
        DMA("sync", gkv[:, 0:1], D["mla_kv_norm"][l].rearrange("(c o) -> c o", o=1), w=["gkv"])

        s.push()
        cq = s.sb([128, 2, S], F32, "cq")
        ckv = s.sb([128, S], F32, "ckv")
        sq = s.sb([128, 2, 512], F32, "sq")
        rstd = s.sb([128, 512], F32, "rstd")

        def ev0(ps, key, tq):
            vcopy(cq[:, 0, tqs(tq)], ps[:, :], [key], [("cq", 0, tq)])
        proj_fm([(0, W[:, 2316:2444])], 128, ev0)

        def ev1(ps, key, tq):
            vcopy(cq[0:64, 1, tqs(tq)], ps[0:64, :], [key], [("cq", 1, tq)])
        proj_fm([(0, W[:, 2444:2508])], 64, ev1)

        def ev2(ps, key, tq):
            vcopy(ckv[:, tqs(tq)], ps[:, :], [key], [("ckv", tq)])
        proj_fm([(0, W[:, 2508:2636])], 128, ev2)
        for tq in range(4):
            V(lambda e, tq=tq: e.tensor_tensor(out=sq[:, 0, :], in0=cq[:, 0, tqs(tq)], in1=cq[:, 0, tqs(tq)], op=ALU.mult), [("cq", 0, tq)], [("sq", 0)])
            V(lambda e, tq=tq: e.tensor_tensor(out=sq[0:64, 1, :], in0=cq[0:64, 1, tqs(tq)], in1=cq[0:64, 1, tqs(tq)], op=ALU.mult), [("cq", 1, tq)], [("sq", 1)])
            b = gbank()
            MM(PS[b][:], onesF[:, :], sq[:, 0, :], True, False, ["onesF", ("sq", 0)], [("ps", b)])
            MM(PS[b][:], onesF[0:64, :], sq[0:64, 1, :], False, True, ["onesF", ("sq", 1)], [("ps", b)])
            V(lambda e, b=b: e.tensor_scalar(out=rstd[:], in0=PS[b][:], scalar1=1.0 / 192, scalar2=1e-6, op0=ALU.mult, op1=ALU.add), [("ps", b)], ["rstd"])
            A(lambda e: e.activation(out=rstd[:], in_=rstd[:], func=AF.Ln), ["rstd"], ["rstd"])
            A(lambda e: e.activation(out=rstd[:], in_=rstd[:], func=AF.Exp, scale=-0.5), ["rstd"], ["rstd"])
            V(lambda e, tq=tq: e.scalar_tensor_tensor(out=cqn[:, 0, tqs(tq)], in0=cq[:, 0, tqs(tq)], scalar=gq[:, 0:1], in1=rstd[:], op0=ALU.mult, op1=ALU.mult),
              [("cq", 0, tq), ("gq", 0), "rstd", "cqn0"], [("cqn", tq, 0)])
            V(lambda e, tq=tq: e.scalar_tensor_tensor(out=cqn[0:64, 1, tqs(tq)], in0=cq[0:64, 1, tqs(tq)], scalar=gq[0:64, 1:2], in1=rstd[0:64, :], op0=ALU.mult, op1=ALU.mult),
              [("cq", 1, tq), ("gq", 1), "rstd", "cqn0"], [("cqn", tq, 1)])
            V(lambda e, tq=tq: e.tensor_tensor(out=sq[:, 0, :], in0=ckv[:, tqs(tq)], in1=ckv[:, tqs(tq)], op=ALU.mult), [("ckv", tq)], [("sq", 0)])
            b = gbank()
            MM(PS[b][:], onesF[:, :], sq[:, 0, :], True, True, ["onesF", ("sq", 0)], [("ps", b)])
            V(lambda e, b=b: e.tensor_scalar(out=rstd[:], in0=PS[b][:], scalar1=1.0 / 128, scalar2=1e-6, op0=ALU.mult, op1=ALU.add), [("ps", b)], ["rstd"])
            A(lambda e: e.activation(out=rstd[:], in_=rstd[:], func=AF.Ln), ["rstd"], ["rstd"])
            A(lambda e: e.activation(out=rstd[:], in_=rstd[:], func=AF.Exp, scale=-0.5), ["rstd"], ["rstd"])
            V(lambda e, tq=tq: e.scalar_tensor_tensor(out=ckvn[:, tqs(tq)], in0=ckv[:, tqs(tq)], scalar=gkv[:, 0:1], in1=rstd[:], op0=ALU.mult, op1=ALU.mult),
              [("ckv", tq), "gkv", "rstd"], [("ckvn", tq)])
        s.pop()

        s.push()
        QT = [s.sb([96, S], BF16, f"mq{h}") for h in range(4)]
        KT = [s.sb([96, S], BF16, f"mk{h}") for h in range(4)]
        vA = s.sb([128, 16, 4, 65], BF16, "mv")
        ropec = s.sb([96, S], F32, "ropec")
        ropes = s.sb([96, S], F32, "ropes")
        t1 = s.sb([96, 512], F32, "t1")
        t2 = s.sb([96, 512], F32, "t2")
        krw = s.sb([128, 8, 96], BF16, "krw")
        krwS = s.sb([128, 8, 96], BF16, "krwS")
        DMA("sync", ropec[:], D["c_ropec"], w=["ropec"])
        DMA("sync", ropes[:], D["c_ropes"], w=["ropes"])
        V(lambda e: e.memset(vA[:, :, :, 64:65], 1.0), [], [("mv", tt) for tt in range(16)])
        V(lambda e: e.memset(krw[:], 0.0), [], ["krw"])
        V(lambda e: e.memset(krwS[:], 0.0), [], ["krwS"])
        DMA("gpsimd", krw[:, :, 64:96], W[:, 2636:2668].rearrange("(c p) m -> p c m", p=128), r=["krw"], w=["krw1"])
        DMA("gpsimd", krwS[:, :, 64:80], W[:, 2652:2668].rearrange("(c p) m -> p c m", p=128), r=["krwS"], w=["krwS1"])
        DMA("gpsimd", krwS[:, :, 80:96], W[:, 2636:2652].rearrange("(c p) m -> p c m", p=128), r=["krwS"], w=["krwS2"])

        def rope_comb(pa, pb, ka, kb, dst, dkey, tq):
            V(lambda e: e.tensor_tensor(out=t1[64:96, :], in0=pa[64:96, :], in1=ropec[64:96, tqs(tq)], op=ALU.mult), [ka, "ropec"], ["t1"])
            V(lambda e: e.tensor_tensor(out=t2[64:96, :], in0=pb[64:96, :], in1=ropes[64:96, tqs(tq)], op=ALU.mult), [kb, "ropes"], ["t2"])
            V(lambda e: e.tensor_tensor(out=dst[64:96, tqs(tq)], in0=t1[64:96, :], in1=t2[64:96, :], op=ALU.add), ["t1", "t2"], [dkey])

        for tq in range(4):
            for c in range(8):
                MM(PS[4][0:96, :], krw[:, c, :], xT[:, c, tqs(tq)], c == 0, c == 7, ["krw", "krw1"] + xkeys(tq), [("ps", 4)])
            for c in range(8):
                MM(PS[5][0:96, :], krwS[:, c, :], xT[:, c, tqs(tq)], c == 0, c == 7, ["krwS", "krwS1", "krwS2"] + xkeys(tq), [("ps", 5)])
            rope_comb(PS[4], PS[5], ("ps", 4), ("ps", 5), KT[0], ("mk", 0, tq, "r"), tq)
            for h in range(1, 4):
                vcopy(KT[h][64:96, tqs(tq)], KT[0][64:96, tqs(tq)], [("mk", 0, tq, "r")], [("mk", h, tq, "r")])
        cqk = lambda tq: [("cqn", tq, 0), ("cqn", tq, 1), "cqn0"]
        for h in range(4):
            for tq in range(4):
                hs = slice(h * 96, (h + 1) * 96)
                MM(PS[4][0:96, :], wuq[:, 0, hs], cqn[:, 0, tqs(tq)], True, False, [("wuq", 0)] + cqk(tq), [("ps", 4)])
                MM(PS[4][0:96, :], wuq[0:64, 1, hs], cqn[0:64, 1, tqs(tq)], False, True, [("wuq", 1)] + cqk(tq), [("ps", 4)])
                wsk = ["wuqS"] + [("wuqS", h, ci, j) for ci in range(2) for j in range(2)]
                MM(PS[5][0:96, :], wuqS[:, 0, hs], cqn[:, 0, tqs(tq)], True, False, wsk + cqk(tq), [("ps", 5)])
                MM(PS[5][0:96, :], wuqS[0:64, 1, hs], cqn[0:64, 1, tqs(tq)], False, True, wsk + cqk(tq), [("ps", 5)])
                vcopy(QT[h][0:64, tqs(tq)], PS[4][0:64, :], [("ps", 4)], [("mq", h, tq)])
                rope_comb(PS[4], PS[5], ("ps", 4), ("ps", 5), QT[h], ("mq", h, tq, "r"), tq)
                b = 6 + tq % 2
                MM(PS[b][0:64, :], wukv[:, h * 128:h * 128 + 64], ckvn[:, tqs(tq)], True, True, ["wukv", ("ckvn", tq)], [("ps", b)])
                vcopy(KT[h][0:64, tqs(tq)], PS[b][0:64, :], [("ps", b)], [("mk", h, tq)])
        for tt in range(16):
            b = 6 + tt % 2
            for h in range(4):
                MM(PS[b][:, h * 64:(h + 1) * 64], ckvn[:, tt * 128:(tt + 1) * 128], wukv[:, h * 128 + 64:h * 128 + 128], True, True,
                   ["wukv", ("ckvn", tt // 4)], [("ps", b)])
            vcopy(vA[:, tt, :, 0:64], PS[b][:, 0:256].rearrange("p (h d) -> p h d", h=4), [("ps", b)], [("mv", tt)])
        it = 0
        for h in range(4):
            for qt in range(4):
                it += 1
                bank = 2 + it % 2

                def terms(kc, h=h, qt=qt):
                    tl = [(KT[h][:, kc * 128:(kc + 1) * 128], QT[h][:, tqs(qt)],
                           [("mk", h, kc // 4), ("mk", h, kc // 4, "r"), ("mq", h, qt), ("mq", h, qt, "r")])]
                    if kc >= 4 * qt:
                        tl.append((identb[:], masks[:, kc - 4 * qt, :], ["identb", "masks"]))
                    return tl
                accmap = lambda qs, bank=bank: (bank, qs * 65)
                attn(terms, range(0, 4 * qt + 4), lambda kc, h=h: (vA[:, kc, h, :], ("mv", kc)), 65,
                     lambda kc, qs, qt=qt: kc <= 4 * qt + qs, accmap, scale=96 ** -0.5)
                attn_out(accmap, 64, 4 * qt, None, lambda tt, h=h: oacc[:, tt, h * 64:(h + 1) * 64], False)
        s.pop()
        s.pop()

    def merge_ln1(l, W, oT, xsrc):
        s.push()
        mT = s.sb([128, 8, S], BF16, "mT")
        macc = s.sb([128, S], F32, "macc")
        sg = [s.sb([128, 512], F32, f"sg{i}") for i in range(2)]
        wbr = [s.sb([128, 2, 128], BF16, f"wbr{i}") for i in range(2)]
        k = 0
        for dc in range(8):
            for n in range(4):
                k += 1
                i = next_wbuf()
                wb = wbuf[i]
                c0 = 2668 + n * 1024 + dc * 128
                DMA("gpsimd", wb[:, :, 0:128], W[:, c0:c0 + 128].rearrange("(c p) m -> p c m", p=128), w=[("wbuf", i, 0)])
                DMA("gpsimd", wbr[k % 2][:], D["w_branch"][l, n][:, dc * 128:(dc + 1) * 128].rearrange("(c p) m -> p c m", p=128), w=[("wbr", k % 2)])
                for tq in range(4):
                    bg = 4 + tq % 2
                    bu = 6 + tq % 2
                    for c in range(8):
                        MM(PS[bg][:], wb[:, c, 0:128], xT[:, c, tqs(tq)], c == 0, c == 7, [("wbuf", i, 0)] + xkeys(tq), [("ps", bg)])
                    for c in range(2):
                        MM(PS[bu][:], wbr[k % 2][:, c, :], oT[:, n, c, tqs(tq)], c == 0, c == 1, [("wbr", k % 2), ("oT", n, c)] + [("oT", n, tt) for tt in range(4 * tq, 4 * tq + 4)], [("ps", bu)])
                    A(lambda e, bg=bg, tq=tq: e.activation(out=sg[tq % 2][:], in_=PS[bg][:], func=AF.Sigmoid), [("ps", bg)], [("sg", tq % 2)])
                    if n == 0:
                        V(lambda e, bu=bu, tq=tq: e.tensor_tensor(out=macc[:, tqs(tq)], in0=sg[tq % 2][:], in1=PS[bu][:], op=ALU.mult),
                          [("sg", tq % 2), ("ps", bu)], [("macc", tq)])
                    else:
                        V(lambda e, bu=bu, tq=tq: e.tensor_tensor(out=sg[tq % 2][:], in0=sg[tq % 2][:], in1=PS[bu][:], op=ALU.mult),
                          [("sg", tq % 2), ("ps", bu)], [("sg", tq % 2)])
                        if n < 3:
                            G(lambda e, tq=tq: e.tensor_tensor(out=macc[:, tqs(tq)], in0=macc[:, tqs(tq)], in1=sg[tq % 2][:], op=ALU.add),
                              [("sg", tq % 2), ("macc", tq)], [("macc", tq)])
                        else:
                            G(lambda e, tq=tq, dc=dc: e.tensor_tensor(out=mT[:, dc, tqs(tq)], in0=macc[:, tqs(tq)], in1=sg[tq % 2][:], op=ALU.add),
                              [("sg", tq % 2), ("macc", tq)], [("mT", dc, tq)])
        s.barrier()
        wo = s.sb([128, 8, 1024], BF16, "wo")
        xt2 = [s.sb([128, DM], F32, f"xt2{i}") for i in range(2)]
        lng_, lnb_ = s.sb([128, DM], F32, "lng"), s.sb([128, DM], F32, "lnb")
        LN["g"], LN["b"] = lng_, lnb_
        load_ln(l, "ln1_g", "ln1_b")
        DMA("gpsimd", wo[:], D["w_out"][l].rearrange("(c p) m -> p c m", p=128), w=["wo"])
        for tt in range(16):
            xt = xt2[tt % 2]
            key = ("xt2", tt % 2)
            DMA("sync", xt[:], xsrc[tt * 128:(tt + 1) * 128, :], w=[key])
            for hf in range(2):
                b = 4 + hf
                for dc in range(8):
                    MM(PS[b][:], mT[:, dc, tt * 128:(tt + 1) * 128], wo[:, dc, hf * 512:(hf + 1) * 512], dc == 0, dc == 7,
                       ["wo", ("mT", dc, tt // 4)], [("ps", b)])
                V(lambda e, xt=xt, b=b, hf=hf: e.scalar_tensor_tensor(out=xt[:, hf * 512:(hf + 1) * 512], in0=xt[:, hf * 512:(hf + 1) * 512],
                                                                    scalar=DN_ALPHA, in1=PS[b][:], op0=ALU.mult, op1=ALU.add), [key, ("ps", b)], [key])
            ln_tile(xt, key, tt, xs)
        s.pop()

    LN = {}

    def cross(l):
        s.push()
        AT["P"] = [s.sb([128, 512], BF16, f"P{i}") for i in range(3)]
        AT["rd"] = s.sb([128, 4], F32, "rd")
        memT = s.sb([128, 8, 256], BF16, "memT")
        KxT = [s.sb([128, 256], BF16, f"KxT{h}") for h in range(4)]
        vxA = s.sb([128, 2, 4, 129], BF16, "vxA")
        QxT = [s.sb([128, S], BF16, f"QxT{h}") for h in range(4)]
        ox = s.sb([128, 16, 512], F32, "ox")
        mt = [s.sb([128, DM], F32, f"mt{i}") for i in range(2)]
        V(lambda e: e.memset(vxA[:, :, :, 128:129], 1.0), [], [("vxA", 0), ("vxA", 1)])
        for mc in range(2):
            DMA("sync", mt[mc][:], D["mem"][mc * 128:(mc + 1) * 128, :], w=[("mt", mc)])
            for half in range(2):
                b = 6 + half
                for j in range(4):
                    c = half * 4 + j
                    TR(PS[b][:, j * 128:(j + 1) * 128], mt[mc][:, c * 128:(c + 1) * 128], ident[:], [("mt", mc), "ident"], [("ps", b)])
                vcopy(memT[:, half * 4:(half + 1) * 4, mc * 128:(mc + 1) * 128], PS[b][:].rearrange("p (j q) -> p j q", j=4), [("ps", b)], [("memT", mc)])
        mk = [("memT", 0), ("memT", 1)]
        Wkv = D["x_wkv"][l]
        for h in range(4):
            i = next_wbuf()
            wb = wbuf[i]
            DMA("gpsimd", wb[:, :, 0:128], Wkv[:, h * 128:(h + 1) * 128].rearrange("(c p) m -> p c m", p=128), w=[("wbuf", i, 0)])
            b = gbank()
            for c in range(8):
                MM(PS[b][:, 0:256], wb[:, c, 0:128], memT[:, c, :], c == 0, c == 7, [("wbuf", i, 0)] + mk, [("ps", b)])
            vcopy(KxT[h][:], PS[b][:, 0:256], [("ps", b)], [("KxT", h)])
        i = next_wbuf()
        wb = wbuf[i]
        DMA("gpsimd", wb[:, :, 0:512], Wkv[:, 512:1024].rearrange("(c p) m -> p c m", p=128), w=[("wbuf", i, 0)])
        for mc in range(2):
            b = gbank()
            for c in range(8):
                MM(PS[b][:], memT[:, c, mc * 128:(mc + 1) * 128], wb[:, c, 0:512], c == 0, c == 7, [("wbuf", i, 0)] + mk, [("ps", b)])
            vcopy(vxA[:, mc, :, 0:128], PS[b][:].rearrange("p (h d) -> p h d", h=4), [("ps", b)], [("vxA", mc)])
        for h in range(4):
            def evq(ps, key, tq, h=h):
                vcopy(QxT[h][:, tqs(tq)], ps[:, :], [key], [("QxT", h, tq)])
            proj_fm([(0, D["x_wq"][l][:, h * 128:(h + 1) * 128])], 128, evq)
        for h in range(4):
            for qt in range(4):
                accmap = lambda qs: (2 + qs // 2, (qs % 2) * 129)
                attn(lambda kc, h=h, qt=qt: [(KxT[h][:, kc * 128:(kc + 1) * 128], QxT[h][:, tqs(qt)], [("KxT", h), ("QxT", h, qt)])],
                     range(2), lambda kc, h=h: (vxA[:, kc, h, :], ("vxA", kc)), 129, lambda kc, qs: True, accmap, scale=128 ** -0.5)
                attn_out(accmap, 128, 4 * qt, None, lambda tt, h=h: ox[:, tt, h * 128:(h + 1) * 128], False)
        s.barrier()
        oxT = s.sb([128, 4, S], BF16, "oxT")
        to_fm(ox, 4, oxT, "oxT")
        wo = s.sb([128, 4, 1024], BF16, "xwo")
        lng_, lnb_ = s.sb([128, DM], F32, "lng"), s.sb([128, DM], F32, "lnb")
        LN["g"], LN["b"] = lng_, lnb_
        load_ln(l, "ln2_g", "ln2_b")
        DMA("gpsimd", wo[:], D["x_wo"][l].rearrange("(c p) m -> p c m", p=128), w=["xwo"])
        for tt in range(16):
            xt = mt[tt % 2]
            key = ("mt", tt % 2)
            DMA("sync", xt[:], xs[tt * 128:(tt + 1) * 128, :], w=[key])
            for hf in range(2):
                b = 4 + hf
                for c in range(4):
                    MM(PS[b][:], oxT[:, c, tt * 128:(tt + 1) * 128], wo[:, c, hf * 512:(hf + 1) * 512], c == 0, c == 3,
                       ["xwo", ("oxT", tt)], [("ps", b)])
                V(lambda e, xt=xt, b=b, hf=hf: e.scalar_tensor_tensor(out=xt[:, hf * 512:(hf + 1) * 512], in0=xt[:, hf * 512:(hf + 1) * 512],
                                                                    scalar=DN_ALPHA, in1=PS[b][:], op0=ALU.mult, op1=ALU.add), [key, ("ps", b)], [key])
            ln_tile(xt, key, tt, xs)
        s.pop()

    def moe(l, dst, last):
        s.push()
        cwall = s.sb([128, 16, 32], F32, "cwall")
        ohall = s.sb([128, 16, 32], BF16, "ohall")
        idxs = nc.alloc_sbuf_tensor_at(f"idxs_{l}", [128, 16, 2], I32, offset=(s.sb_off + 63) // 64 * 64)
        s.sb_off = (s.sb_off + 63) // 64 * 64 + 128
        wts = s.sb([128, 16, 2], F32, "wts")
        s.push()
        xt = [s.sb([128, DM], F32, f"rx{i}") for i in range(2)]
        xTf = s.sb([128, 8, 128], F32, "xTf")
        wr = s.sb([128, 8, 36], F32, "wr")
        brow = s.sb([1, 36], F32, "brow")
        ones1 = s.sb([1, 128], F32, "ones1")
        lg = s.sb([128, 36], F32, "lg")
        sm = s.sb([128, 16], F32, "sm")
        gm = s.sb([128, 4], F32, "gm")
        es = s.sb([128, 8], F32, "es")
        t8 = s.sb([128, 8], F32, "t8")
        ta = s.sb([128, 8], F32, "ta")
        tb = s.sb([128, 8], F32, "tb")
        onesb = s.sb([128, 128], BF16, "onesb")
        ltri = s.sb([128, 128], BF16, "ltri")
        ebase = s.sb([128, 32], F32, "ebase")
        rk = s.sb([128, 32], F32, "rk")
        vl = s.sb([128, 32], F32, "vl")
        val = s.sb([128, 32], F32, "val")
        fidx = s.sb([128, 4], F32, "fidx")
        V(lambda e: e.memset(ones1[:], 1.0), [], ["ones1"])
        V(lambda e: e.memset(onesb[:], 1.0), [], ["onesb"])
        DMA("gpsimd", ltri[:], D["c_ltri"], w=["ltri"])
        DMA("sync", ebase[:], D["c_ebase"], w=["ebase"])
        DMA("sync", wr[:, :, 0:4], D["moe_rg_w"][l].rearrange("(c p) m -> p c m", p=128), w=["wr0"])
        DMA("sync", wr[:, :, 4:36], D["moe_re_w"][l].rearrange("(c p) m -> p c m", p=128), w=["wr1"])
        DMA("sync", brow[:, 0:4], D["moe_rg_b"][l].rearrange("(o m) -> o m", o=1), w=["br0"])
        DMA("sync", brow[:, 4:36], D["moe_re_b"][l].rearrange("(o m) -> o m", o=1), w=["br1"])
        for tt in range(16):
            x_ = xt[tt % 2]
            key = ("rx", tt % 2)
            DMA("sync", x_[:], xs[tt * 128:(tt + 1) * 128, :], w=[key])
            for half in range(2):
                b = 6 + half
                for j in range(4):
                    c = half * 4 + j
                    TR(PS[b][:, j * 128:(j + 1) * 128], x_[:, c * 128:(c + 1) * 128], ident[:], [key, "ident"], [("ps", b)])
                vcopy(xTf[:, half * 4:(half + 1) * 4, :], PS[b][:].rearrange("p (j q) -> p j q", j=4), [("ps", b)], [("xTf", half)])
            for c in range(8):
                MM(PS[4][:, 0:36], xTf[:, c, :], wr[:, c, :], c == 0, False, [("xTf", c // 4), "wr0", "wr1"], [("ps", 4)])
            MM(PS[4][:, 0:36], ones1[:, :], brow[:, :], False, True, ["ones1", "br0", "br1"], [("ps", 4)])
            vcopy(lg[:], PS[4][:, 0:36], [("ps", 4)], ["lg"])
            V(lambda e: e.tensor_reduce(out=sm[:, 0:1], in_=lg[:, 0:4], axis=AX.X, op=ALU.max), ["lg"], [("sm", 0)])
            V(lambda e: e.tensor_scalar(out=sm[:, 1:2], in0=sm[:, 0:1], scalar1=-1.0, scalar2=None, op0=ALU.mult), [("sm", 0)], [("sm", 1)])
            A(lambda e: e.activation(out=gm[:], in_=lg[:, 0:4], func=AF.Exp, bias=sm[:, 1:2]), ["lg", ("sm", 1)], ["gm"])
            V(lambda e: e.tensor_reduce(out=sm[:, 2:3], in_=gm[:], axis=AX.X, op=ALU.add), ["gm"], [("sm", 2)])
            V(lambda e: e.reciprocal(out=sm[:, 3:4], in_=sm[:, 2:3]), [("sm", 2)], [("sm", 3)])
            V(lambda e: e.tensor_scalar(out=gm[:], in0=lg[:, 0:4], scalar1=sm[:, 0:1], scalar2=None, op0=ALU.is_ge), ["lg", ("sm", 0), "gm"], ["gm"])
            V(lambda e: e.tensor_scalar(out=es[:], in0=lg[:, 4:12], scalar1=gm[:, 0:1], scalar2=None, op0=ALU.mult), ["lg", "gm"], ["es"])
            for g in range(1, 4):
                V(lambda e, g=g: e.scalar_tensor_tensor(out=es[:], in0=lg[:, 4 + 8 * g:12 + 8 * g], scalar=gm[:, g:g + 1], in1=es[:], op0=ALU.mult, op1=ALU.add),
                  ["lg", "gm", "es"], ["es"])
            V(lambda e: e.max(out=t8[:], in_=es[:]), ["es"], ["t8"])
            V(lambda e: e.tensor_tensor(out=sm[:, 4:5], in0=t8[:, 0:1], in1=t8[:, 1:2], op=ALU.subtract), ["t8"], [("sm", 4)])
            A(lambda e: e.activation(out=sm[:, 5:6], in_=sm[:, 4:5], func=AF.Sigmoid), [("sm", 4)], [("sm", 5)])
            V(lambda e: e.tensor_tensor(out=sm[:, 6:7], in0=sm[:, 5:6], in1=sm[:, 3:4], op=ALU.mult), [("sm", 5), ("sm", 3)], [("sm", 6)])
            V(lambda e: e.tensor_tensor(out=sm[:, 7:8], in0=sm[:, 3:4], in1=sm[:, 6:7], op=ALU.subtract), [("sm", 6), ("sm", 3)], [("sm", 7)])
            for g in range(4):
                eg = lg[:, 4 + 8 * g:12 + 8 * g]
                V(lambda e, eg=eg: e.tensor_scalar(out=ta[:], in0=eg, scalar1=t8[:, 0:1], scalar2=sm[:, 6:7], op0=ALU.is_equal, op1=ALU.mult),
                  ["lg", "t8", ("sm", 6), "ta"], ["ta"])
                V(lambda e, eg=eg: e.tensor_scalar(out=tb[:], in0=eg, scalar1=t8[:, 1:2], scalar2=sm[:, 7:8], op0=ALU.is_equal, op1=ALU.mult),
                  ["lg", "t8", ("sm", 7), "tb"], ["tb"])
                V(lambda e: e.tensor_tensor(out=ta[:], in0=ta[:], in1=tb[:], op=ALU.add), ["ta", "tb"], ["ta"])
                V(lambda e, g=g, tt=tt: e.tensor_scalar(out=cwall[:, tt, 8 * g:8 * g + 8], in0=ta[:], scalar1=gm[:, g:g + 1], scalar2=None, op0=ALU.mult),
                  ["ta", "gm"], [("cw", tt, g)])
            V(lambda e, tt=tt: e.tensor_scalar(out=ohall[:, tt, :], in0=cwall[:, tt, :], scalar1=0.0, scalar2=None, op0=ALU.is_gt),
              [("cw", tt, g) for g in range(4)], [("oh", tt)])
        for tt in range(16):
            x_ = xt[tt % 2]
            key = ("rx", tt % 2)
            DMA("sync", x_[:], xs[tt * 128:(tt + 1) * 128, :], w=[key])
            for t2 in range(tt):
                MM(PS[5][:, 0:32], onesb[:, :], ohall[:, t2, :], t2 == 0, False, ["onesb", ("oh", t2)], [("ps", 5)])
            MM(PS[5][:, 0:32], ltri[:, :], ohall[:, tt, :], tt == 0, True, ["ltri", ("oh", tt)], [("ps", 5)])
            vcopy(rk[:], PS[5][:, 0:32], [("ps", 5)], ["rk"])
            V(lambda e: e.tensor_scalar(out=vl[:], in0=rk[:], scalar1=float(CAP) - 0.5, scalar2=None, op0=ALU.is_lt), ["rk"], ["vl"])
            V(lambda e, tt=tt: e.tensor_tensor(out=vl[:], in0=vl[:], in1=ohall[:, tt, :], op=ALU.mult), ["vl", ("oh", tt)], ["vl"])
            V(lambda e, tt=tt: e.tensor_tensor(out=cwall[:, tt, :], in0=cwall[:, tt, :], in1=vl[:], op=ALU.mult), ["vl"] + [("cw", tt, g) for g in range(4)], [("cwv", tt)])
            V(lambda e: e.tensor_tensor(out=val[:], in0=rk[:], in1=ebase[:], op=ALU.add), ["rk", "ebase"], ["val"])
            V(lambda e: e.tensor_tensor(out=val[:], in0=val[:], in1=vl[:], op=ALU.mult), ["val", "vl"], ["val"])
            V(lambda e: e.tensor_reduce(out=fidx[:, 1:2], in_=val[:], axis=AX.X, op=ALU.max), ["val"], [("fidx", 1)])
            V(lambda e: e.tensor_reduce(out=fidx[:, 0:1], in_=val[:], axis=AX.X, op=ALU.add), ["val"], [("fidx", 0)])
            V(lambda e: e.tensor_tensor(out=fidx[:, 0:1], in0=fidx[:, 0:1], in1=fidx[:, 1:2], op=ALU.subtract), [("fidx", 0), ("fidx", 1)], [("fidx", 0)])
            V(lambda e: e.tensor_scalar(out=rk[:], in0=val[:], scalar1=fidx[:, 1:2], scalar2=None, op0=ALU.is_equal), ["val", ("fidx", 1), "rk"], ["rk"])
            V(lambda e, tt=tt: e.tensor_tensor(out=rk[:], in0=rk[:], in1=cwall[:, tt, :], op=ALU.mult), ["rk", ("cwv", tt)], ["rk"])
            V(lambda e, tt=tt: e.tensor_reduce(out=wts[:, tt, 1:2], in_=rk[:], axis=AX.X, op=ALU.add), ["rk"], [("wts", tt, 1)])
            V(lambda e, tt=tt: e.tensor_reduce(out=wts[:, tt, 0:1], in_=cwall[:, tt, :], axis=AX.X, op=ALU.add), [("cwv", tt)], [("wts", tt, 0)])
            V(lambda e, tt=tt: e.tensor_tensor(out=wts[:, tt, 0:1], in0=wts[:, tt, 0:1], in1=wts[:, tt, 1:2], op=ALU.subtract),
              [("wts", tt, 0), ("wts", tt, 1)], [("wts", tt, 0)])
            V(lambda e: e.tensor_scalar(out=fidx[:, 2:4], in0=fidx[:, 0:2], scalar1=0.5, scalar2=1.0e6, op0=ALU.is_lt, op1=ALU.mult),
              [("fidx", 0), ("fidx", 1)], [("fidx", 2)])
            V(lambda e: e.scalar_tensor_tensor(out=fidx[:, 2:4], in0=fidx[:, 0:2], scalar=-1.0, in1=fidx[:, 2:4], op0=ALU.add, op1=ALU.add),
              [("fidx", 0), ("fidx", 1), ("fidx", 2)], [("fidx", 2)])
            V(lambda e, tt=tt: e.tensor_copy(out=idxs[:, tt, :], in_=fidx[:, 2:4]), [("fidx", 2)], [("idx", tt)])
            for k_ in range(2):
                s.op("gpsimd", lambda e, tt=tt, k_=k_, x_=x_: e.indirect_dma_start(
                    out=xg[:, :], out_offset=bass.IndirectOffsetOnAxis(ap=idxs[:, tt, k_:k_ + 1], axis=0), in_=x_[:, :], in_offset=None,
                    bounds_check=bc_reg, oob_is_err=False), [key, ("idx", tt)], [("xgs", tt, k_)], dma=True)
        s.pop()
        s.push()
        NJ = CAP // 128
        wgu = [s.sb([128, 8, 1024], BF16, f"wgu{i}") for i in range(2)]
        wd = [s.sb([128, 4, 1024], BF16, f"wd{i}") for i in range(2)]
        xgt = [s.sb([128, NJ, DM], F32, f"xgt{i}") for i in range(2)]
        xgT = [s.sb([128, 8, CAP], BF16, f"xgT{i}") for i in range(2)]
        actT = s.sb([128, 4, CAP], BF16, "actT")
        sl = [s.sb([128, CAP], F32, f"sl{i}") for i in range(2)]
        yrow = [s.sb([128, DM], F32, f"yrow{i}") for i in range(2)]
        k = 0
        for ee in range(32):
            wi = ee % 2
            DMA("gpsimd", wgu[wi][:], D["moe_w_gu"][l, ee].rearrange("(c p) m -> p c m", p=128), w=[("wgu", wi)])
            DMA("gpsimd", wd[wi][:], D["moe_w_down"][l, ee].rearrange("(c p) m -> p c m", p=128), w=[("wd", wi)])
            DMA("sync", xgt[wi][:], xg[ee * CAP:(ee + 1) * CAP, :].rearrange("(j p) d -> p j d", p=128), w=[("xgt", wi)])
            for j in range(NJ):
                for half in range(2):
                    b = 6 + half
                    for jj in range(4):
                        c = half * 4 + jj
                        TR(PS[b][:, jj * 128:(jj + 1) * 128], xgt[wi][:, j, c * 128:(c + 1) * 128], ident[:], [("xgt", wi), "ident"], [("ps", b)])
                    vcopy(xgT[wi][:, half * 4:(half + 1) * 4, j * 128:(j + 1) * 128], PS[b][:].rearrange("p (j q) -> p j q", j=4),
                          [("ps", b)], [("xgT", wi, j, half)])
            xk = [("xgT", wi, j, half) for j in range(NJ) for half in range(2)]
            for fc in range(4):
                k += 1
                bg = 0 + k % 2
                bu = 2 + k % 2
                for c in range(8):
                    MM(PS[bg][:, 0:CAP], wgu[wi][:, c, fc * 128:(fc + 1) * 128], xgT[wi][:, c, :], c == 0, c == 7, [("wgu", wi)] + xk, [("ps", bg)])
                for c in range(8):
                    MM(PS[bu][:, 0:CAP], wgu[wi][:, c, 512 + fc * 128:512 + (fc + 1) * 128], xgT[wi][:, c, :], c == 0, c == 7, [("wgu", wi)] + xk, [("ps", bu)])
                A(lambda e, bg=bg, k=k: e.activation(out=sl[k % 2][:], in_=PS[bg][:, 0:CAP], func=AF.Silu), [("ps", bg)], [("sl", k % 2)])
                V(lambda e, bu=bu, k=k, fc=fc: e.tensor_tensor(out=actT[:, fc, :], in0=sl[k % 2][:], in1=PS[bu][:, 0:CAP], op=ALU.mult),
                  [("sl", k % 2), ("ps", bu)], [("actT", fc)])
            for j in range(NJ):
                yr = yrow[(ee * NJ + j) % 2]
                yk = ("yrow", (ee * NJ + j) % 2)
                for hf in range(2):
                    b = 4 + hf
                    for fc in range(4):
                        MM(PS[b][:], actT[:, fc, j * 128:(j + 1) * 128], wd[wi][:, fc, hf * 512:(hf + 1) * 512], fc == 0, fc == 3,
                           [("wd", wi), ("actT", fc)], [("ps", b)])
                    if hf == 0:
                        vcopy(yr[:, 0:512], PS[b][:], [("ps", b)], [yk])
                    else:
                        acopy(yr[:, 512:1024], PS[b][:], [("ps", b)], [yk])
                DMA("sync", yg[ee * CAP + j * 128:ee * CAP + (j + 1) * 128, :], yr[:], r=[yk], w=[("ygs", ee, j)])
        s.pop()
        xt3 = [s.sb([128, DM], F32, f"xt3{i}") for i in range(2)]
        gA = [s.sb([128, DM], F32, f"gA{i}") for i in range(2)]
        gB = [s.sb([128, DM], F32, f"gB{i}") for i in range(2)]
        lng_, lnb_ = s.sb([128, DM], F32, "lng"), s.sb([128, DM], F32, "lnb")
        LN["g"], LN["b"] = lng_, lnb_
        load_ln(l, "ln3_g", "ln3_b")
        for i in range(2):
            V(lambda e, i=i: e.memset(gA[i][:], 0.0), [], [("gA", i)])
            V(lambda e, i=i: e.memset(gB[i][:], 0.0), [], [("gB", i)])
        for tt in range(16):
            xt_ = xt3[tt % 2]
            key = ("xt3", tt % 2)
            ga, gb = gA[tt % 2], gB[tt % 2]
            DMA("sync", xt_[:], xs[tt * 128:(tt + 1) * 128, :], w=[key])
            for k_, gt, gk in ((0, ga, ("gA", tt % 2)), (1, gb, ("gB", tt % 2))):
                s.op("gpsimd", lambda e, tt=tt, k_=k_, gt=gt: e.indirect_dma_start(
                    out=gt[:, :], out_offset=None, in_=yg[:, :], in_offset=bass.IndirectOffsetOnAxis(ap=idxs[:, tt, k_:k_ + 1], axis=0),
                    bounds_check=bc_reg, oob_is_err=False), [("idx", tt)], [gk], dma=True)
            V(lambda e, ga=ga, tt=tt: e.tensor_scalar(out=ga[:], in0=ga[:], scalar1=wts[:, tt, 0:1], scalar2=None, op0=ALU.mult),
              [("gA", tt % 2), ("wts", tt, 0)], [("gA", tt % 2)])
            V(lambda e, ga=ga, gb=gb, tt=tt: e.scalar_tensor_tensor(out=ga[:], in0=gb[:], scalar=wts[:, tt, 1:2], in1=ga[:], op0=ALU.mult, op1=ALU.add),
              [("gA", tt % 2), ("gB", tt % 2), ("wts", tt, 1)], [("gA", tt % 2)])
            V(lambda e, xt_=xt_, ga=ga: e.scalar_tensor_tensor(out=xt_[:], in0=xt_[:], scalar=DN_ALPHA, in1=ga[:], op0=ALU.mult, op1=ALU.add),
              [key, ("gA", tt % 2)], [key])
            ln_tile(xt_, key, tt, dst, do_T=not last)
        s.pop()

    def mixer(l, xsrc):
        W = D["w_in"][l]
        s.push()
        oT = s.sb([128, 4, 2, S], BF16, "oT")
        s.push()
        oacc = s.sb([128, 16, 256], F32, "oacc")
        AT["P"] = [s.sb([128, 512], BF16, f"P{i}") for i in range(3)]
        AT["rd"] = s.sb([128, 4], F32, "rd")
        nsa(l, W, oacc)
        to_fm(oacc, 2, oT[:, 0], ("oT", 0))
        sbranch(l, W, oacc)
        to_fm(oacc, 2, oT[:, 1], ("oT", 1))
        rglru(l, W, oT)
        mla(l, W, oacc)
        to_fm(oacc, 2, oT[:, 3], ("oT", 3))
        s.pop()
        if l == 0:
            dump_sb("oT", oT[:], [128, 4, 2, S])
        merge_ln1(l, W, oT, xsrc)
        s.pop()

    s.push()
    zt = s.sb([128, DM], F32, "zt")
    V(lambda e: e.memset(zt[:], 0.0), [], ["zt"])
    for r in range(NSL // 128):
        DMA("sync", xg[r * 128:(r + 1) * 128, :], zt[:], r=["zt"], w=[("xgz", r)])
    s.pop()
    load_xT(D["x"])
    for l in range(NL):
        s.push()
        wbuf = [s.sb([128, 8, 512], BF16, f"wbuf{i}") for i in range(3)]
        masks = s.sb([128, 12, 512], BF16, "masks")
        DMA("gpsimd", masks[:], D["c_masks"], w=["masks"])
        if "mix" in stages:
            mixer(l, D["x"] if l == 0 else xs)
        if "cross" in stages:
            cross(l)
        s.pop()
        if "moe" in stages:
            last = l == NL - 1
            moe(l, out_d if last else xs, last)
    if "moe" not in stages:
        s.barrier()
        s.push()
        tmp = s.sb([128, DM], F32, "fin")
        for tt in range(16):
            DMA("sync", tmp[:], xs[tt * 128:(tt + 1) * 128, :], w=["fin"])
            DMA("sync", out_d[tt * 128:(tt + 1) * 128, :], tmp[:], r=["fin"])
        s.pop()
    s.barrier()
    s.emit()
    return nc, dumps


_CACHE = {}


def kernel(**inputs):
    NL = 4
    if "nc" not in _CACHE:
        _CACHE["nc"] = build(NL)[0]
    nc = _CACHE["nc"]
    HC = host_consts()
    shared = {n: np.ascontiguousarray(np.asarray(inputs[n], dtype=np.float32)) for n in WNAMES}
    for n, a in HC.items():
        shared["c_" + n] = a
    x = np.asarray(inputs["x"], dtype=np.float32)
    mem = np.asarray(inputs["mem"], dtype=np.float32)
    in_maps = []
    for b in range(8):
        m = dict(shared)
        m["x"] = np.ascontiguousarray(x[b])
        m["mem"] = np.ascontiguousarray(mem[b])
        in_maps.append(m)
    res = run_bass_kernel_spmd(nc, in_maps, core_ids=list(range(8)))
    return np.stack([np.asarray(r["out"], dtype=np.float32) for r in res.results], axis=0)
```

```python
import numpy as np
import contextlib
import concourse.bass as bass
import concourse.mybir as mybir
from concourse.bass_utils import run_bass_kernel_spmd

F32 = mybir.dt.float32
BF16 = mybir.dt.bfloat16
AF = mybir.ActivationFunctionType
ALU = mybir.AluOpType
AX = mybir.AxisListType
ENGS = ("tensor", "vector", "scalar", "gpsimd", "sync")
DSIZE = {F32: 4, BF16: 2}
NEG = -30000.0
S = 2048
DM = 1024
DN_ALPHA = (2.0 * 4) ** 0.25
CAP = 384
NSL = 32 * CAP
I32 = mybir.dt.int32


class Sched:
    NSLOT = 4

    def __init__(self, nc):
        self.nc = nc
        self.ops = {e: [] for e in ENGS}
        self.ncomp = {e: 0 for e in ENGS}
        self.ndma = {e: 0 for e in ENGS}
        self.lastw = {}
        self.readers = {}
        self.synced = {e: {} for e in ENGS}
        self.sb_off = 16640
        self.sb_stack = []
        self.uid = 0

    def sb(self, shape, dtype, name=None):
        self.uid += 1
        name = f"{name or 't'}_{self.uid}"
        nbytes = int(np.prod(shape[1:])) * DSIZE[dtype]
        off = (self.sb_off + 63) // 64 * 64
        assert off + nbytes <= 228000, f"SBUF overflow {name} {off + nbytes}"
        t = self.nc.alloc_sbuf_tensor_at(name, list(shape), dtype, offset=off)
        self.sb_off = off + nbytes
        return t

    def push(self):
        self.sb_stack.append(self.sb_off)

    def pop(self):
        self.barrier()
        self.sb_off = self.sb_stack.pop()

    def _need(self, E, dep, waits):
        key, val = dep
        if key == ("c", "tensor") and E == "tensor":
            return
        if self.synced[E].get(key, 0) >= val:
            return
        if waits.get(key, 0) < val:
            waits[key] = val

    def op(self, E, fn, reads=(), writes=(), dma=False):
        waits = {}
        for r in reads:
            lw = self.lastw.get(r)
            if lw is not None:
                self._need(E, lw, waits)
        for w in writes:
            lw = self.lastw.get(w)
            if lw is not None:
                self._need(E, lw, waits)
            for k, v in self.readers.get(w, {}).items():
                self._need(E, (k, v), waits)
        if dma:
            k = self.ndma[E]
            self.ndma[E] += 1
            key = ("d", E, k % self.NSLOT)
            val = 16 * (k // self.NSLOT + 1)
            if val > 16:
                self._need(E, (key, val - 16), waits)
        else:
            self.ncomp[E] += 1
            key = ("c", E)
            val = self.ncomp[E]
        for k_, v_ in waits.items():
            self.synced[E][k_] = v_
        me = (key, val)
        self.ops[E].append((fn, waits, me))
        for r in reads:
            d = self.readers.setdefault(r, {})
            if d.get(key, 0) < val:
                d[key] = val
        for w in writes:
            self.lastw[w] = me
            self.readers[w] = {}
        return me

    def raw(self, E, fn):
        self.ops[E].append((fn, {}, None))

    def barrier(self):
        state = {}
        for e in ENGS:
            if self.ncomp[e]:
                state[("c", e)] = self.ncomp[e]
            for sl in range(self.NSLOT):
                k = self.ndma[e]
                cnt = (k - sl + self.NSLOT - 1) // self.NSLOT if k > sl else 0
                if cnt:
                    state[("d", e, sl)] = 16 * cnt
        for e in ENGS:
            waits = {}
            for k_, v_ in state.items():
                if self.synced[e].get(k_, 0) < v_:
                    waits[k_] = v_
                    self.synced[e][k_] = v_
            if waits:
                self.ops[e].append((None, waits, None))
        self.lastw = {}
        self.readers = {}

    def emit(self):
        nc = self.nc
        sems = {}
        with contextlib.ExitStack() as st:
            for e in ENGS:
                sems[("c", e)] = st.enter_context(nc.semaphore(f"c_{e}"))
                for sl in range(self.NSLOT):
                    sems[("d", e, sl)] = st.enter_context(nc.semaphore(f"d_{e}_{sl}"))
            block = st.enter_context(nc.Block())

            def run(eng, name):
                for fn, waits, me in self.ops[name]:
                    for k_, v_ in waits.items():
                        eng.wait_ge(sems[k_], v_)
                    if fn is not None and me is None:
                        fn(eng)
                    elif fn is not None:
                        fn(eng).then_inc(sems[me[0]], 16 if me[0][0] == "d" else 1)

            @block.tensor
            def _(e):
                run(e, "tensor")

            @block.vector
            def _(e):
                run(e, "vector")

            @block.scalar
            def _(e):
                run(e, "scalar")

            @block.gpsimd
            def _(e):
                run(e, "gpsimd")

            @block.sync
            def _(e):
                run(e, "sync")


def host_consts():
    c = {}
    c["ident"] = np.eye(128, dtype=np.float32)
    t = np.arange(S)
    slopes = 2.0 ** (-2.0 * (np.arange(4) + 1))
    qaug = np.zeros((4, 4, S), np.float32)
    for h in range(4):
        qaug[h, 0] = -slopes[h] * 128 * (t // 128)
        qaug[h, 1] = -slopes[h] * (t % 128)
        qaug[h, 2] = slopes[h]
        qaug[h, 3] = slopes[h]
    c["qaug"] = qaug
    c["kaug"] = np.stack([np.ones(S), np.ones(S), 128.0 * (t // 128), (t % 128)]).astype(np.float32)
    be = np.arange(128) * 16 + 31
    c["kcaug"] = np.stack([np.ones(128), np.ones(128), 128.0 * (be // 128), (be % 128)]).astype(np.float32)
    k = np.arange(128)[:, None]
    q = np.arange(512)[None, :]
    masks = np.zeros((128, 12, 512), np.float32)
    for j in range(4):
        masks[:, j] = np.where(q >= 128 * j + k, 0.0, NEG)
        masks[:, 4 + j] = np.where(128 * j + k < q, 0.0, NEG)
        masks[:, 8 + j] = np.where(q < 128 * j + k, 0.0, NEG)
    c["masks"] = masks
    cc = np.arange(128)[:, None]
    c["cmask"] = np.where((t[None, :] >= 16 * cc + 31) & (cc < 127), 0.0, NEG).astype(np.float32)
    tt = t[:, None]
    j = np.arange(32)[None, :]
    forced = (j == 0) | (j == tt // 64)
    valid = j * 64 <= tt
    vm = (valid & ~forced).astype(np.float32)
    am = np.where(forced, 1e4, np.where(valid, 0.0, -1.0)).astype(np.float32)
    c["impvm"] = vm.reshape(16, 128, 32).transpose(1, 0, 2).copy()
    c["impam"] = am.reshape(16, 128, 32).transpose(1, 0, 2).copy()
    E = np.zeros((32, 16, 128), np.float32)
    for kc in range(16):
        for kk in range(128):
            E[2 * kc + kk // 64, kc, kk] = 1.0
    c["selE"] = E
    c0 = np.arange(128)[:, None] * 16
    j0 = np.arange(32)[None, :] * 64
    cover = np.clip(np.minimum(c0 + 32, j0 + 64) - np.maximum(c0, j0), 0, None) / 32.0
    cover[127] = 0.0
    c["cover"] = cover.astype(np.float32)
    inv = (10000.0 ** (-np.arange(0, 32, 2, dtype=np.float32) / 32)).astype(np.float32)
    ang = t.astype(np.float32)[:, None] * inv[None, :]
    cs, sn = np.cos(ang).astype(np.float32).T, np.sin(ang).astype(np.float32).T
    rc = np.zeros((96, S), np.float32)
    rs = np.zeros((96, S), np.float32)
    rc[64:80] = cs
    rc[80:96] = cs
    rs[64:80] = -sn
    rs[80:96] = sn
    c["ropec"] = rc
    c["ropes"] = rs
    jj = np.arange(128)[:, None]
    ss = np.arange(128)[None, :]
    c["negU"] = np.where(jj >= ss, -1.0, 0.0).astype(np.float32)
    sr = np.zeros((32, 32, 128), np.float32)
    for e_ in range(32):
        sr[e_, e_, :] = 1.0
    c["selrow"] = sr
    c["ltri"] = (jj < ss).astype(np.float32)
    c["ebase"] = np.tile((np.arange(32) * CAP + 1).astype(np.float32)[None, :], (128, 1))
    return c


WNAMES = ["w_in", "nsa_cmp_pos", "nsa_cmp_w1", "nsa_cmp_w2", "rnn_conv_w", "rnn_conv_b", "rnn_ga_w", "rnn_ga_b",
          "rnn_gx_w", "rnn_gx_b", "rnn_lambda", "mla_q_norm", "mla_kv_norm", "mla_w_uq", "mla_w_ukv", "w_branch",
          "w_out", "ln1_g", "ln1_b", "x_wq", "x_wkv", "x_wo", "ln2_g", "ln2_b", "moe_rg_w", "moe_rg_b", "moe_re_w",
          "moe_re_b", "moe_w_gu", "moe_w_down", "ln3_g", "ln3_b"]
WSHAPES = {"w_in": (1024, 6764), "nsa_cmp_pos": (2, 32, 64), "nsa_cmp_w1": (2, 2048, 256), "nsa_cmp_w2": (2, 256, 64),
           "rnn_conv_w": (4, 256), "rnn_conv_b": (256,), "rnn_ga_w": (4, 64, 64), "rnn_ga_b": (256,),
           "rnn_gx_w": (4, 64, 64), "rnn_gx_b": (256,), "rnn_lambda": (256,), "mla_q_norm": (192,),
           "mla_kv_norm": (128,), "mla_w_uq": (192, 384), "mla_w_ukv": (128, 512), "w_branch": (4, 256, 1024),
           "w_out": (1024, 1024), "ln1_g": (1024,), "ln1_b": (1024,), "x_wq": (1024, 512), "x_wkv": (1024, 1024),
           "x_wo": (512, 1024), "ln2_g": (1024,), "ln2_b": (1024,), "moe_rg_w": (1024, 4), "moe_rg_b": (4,),
           "moe_re_w": (1024, 32), "moe_re_b": (32,), "moe_w_gu": (32, 1024, 1024), "moe_w_down": (32, 512, 1024),
           "ln3_g": (1024,), "ln3_b": (1024,)}


def build(NL=4, stages=("mix", "cross", "moe"), dump=None):
    nc = bass.Bass("TRN2", target_bir_lowering=False)
    s = Sched(nc)
    D = {}

    def din(name, shape):
        D[name] = nc.dram_tensor(name, list(shape), F32, kind="ExternalInput").ap()
        return D[name]

    din("x", (S, DM))
    din("mem", (256, DM))
    for n in WNAMES:
        din(n, (NL,) + WSHAPES[n])
    HC = host_consts()
    for n, a in HC.items():
        din("c_" + n, a.shape)
    out_d = nc.dram_tensor("out", [S, DM], F32, kind="ExternalOutput").ap()
    xs = nc.dram_tensor("xs_scr", [S, DM], F32, kind="Internal").ap()
    xg = nc.dram_tensor("xg_scr", [NSL, DM], F32, kind="Internal").ap()
    yg = nc.dram_tensor("yg_scr", [NSL, DM], F32, kind="Internal").ap()
    dumps = {}

    PS = [nc.alloc_psum_tensor(f"ps{i}", [128, 512], F32) for i in range(8)]
    bc_reg = nc.gpsimd.alloc_register("bc_reg")
    s.raw("gpsimd", lambda e: e.reg_mov(bc_reg, NSL - 1))

    def V(fn, r=(), w=()):
        return s.op("vector", fn, r, w)

    def A(fn, r=(), w=()):
        return s.op("scalar", fn, r, w)

    def G(fn, r=(), w=()):
        return s.op("gpsimd", fn, r, w)

    def MM(out, lhsT, rhs, start, stop, r, w):
        return s.op("tensor", lambda e: e.matmul(out, lhsT=lhsT, rhs=rhs, start=start, stop=stop), r, w)

    def MMs(out, lhsT, rhs, start, stop, r, w):
        return s.op("tensor", lambda e: e.matmul(out, lhsT=lhsT, rhs=rhs, start=start, stop=stop, skip_group_check=True), r, w)

    def TR(out, in_, idn, r, w):
        return s.op("tensor", lambda e: e.transpose(out, in_, idn), r, w)

    def DMA(q, out, in_, r=(), w=()):
        return s.op(q, lambda e: e.dma_start(out=out, in_=in_), r, w, dma=True)

    def vcopy(out, in_, r, w):
        return V(lambda e: e.tensor_copy(out=out, in_=in_), r, w)

    def acopy(out, in_, r, w):
        return A(lambda e: e.activation(out=out, in_=in_, func=AF.Copy), r, w)

    def dump_sb(name, ap, shape):
        if dump is None or name not in dump:
            return
        d = nc.dram_tensor("dbg_" + name, list(shape), ap.dtype if hasattr(ap, "dtype") else F32, kind="ExternalOutput").ap()
        dumps[name] = d
        s.barrier()
        DMA("sync", d, ap)
        s.barrier()

    ident = s.sb([128, 128], F32, "ident")
    identb = s.sb([128, 128], BF16, "identb")
    xT = s.sb([128, 8, S], BF16, "xT")
    wbuf = None
    masks = None
    stat = s.sb([128, 2, 6], F32, "stat")
    mv = s.sb([128, 4], F32, "mv")
    DMA("sync", ident[:], D["c_ident"], w=["ident"])
    DMA("gpsimd", identb[:], D["c_ident"], w=["identb"])
    cnt = {"w": 0, "g": 0}

    def next_wbuf():
        cnt["w"] += 1
        return cnt["w"] % 3

    def gbank():
        cnt["g"] += 1
        return 4 + cnt["g"] % 2

    def xkeys(tq):
        return [("xT", 4 * tq + u) for u in range(4)]

    def transpose_tile(src, key, tt, f32dst=None, f32key=None):
        for half in range(2):
            b = 6 + half
            for j in range(4):
                c = half * 4 + j
                TR(PS[b][:, j * 128:(j + 1) * 128], src[:, c * 128:(c + 1) * 128], ident[:], [key, "ident"], [("ps", b)])
            vcopy(xT[:, half * 4:(half + 1) * 4, tt * 128:(tt + 1) * 128], PS[b][:].rearrange("p (j q) -> p j q", j=4),
                  [("ps", b)], [("xT", tt)])
            if f32dst is not None:
                acopy(f32dst[:, half * 4:(half + 1) * 4, :], PS[b][:].rearrange("p (j q) -> p j q", j=4), [("ps", b)], [f32key])

    def load_xT(src):
        s.push()
        xtile = [s.sb([128, DM], F32, f"xtile{i}") for i in range(2)]
        for tt in range(16):
            xt = xtile[tt % 2]
            DMA("sync", xt[:], src[tt * 128:(tt + 1) * 128, :], w=[("xtile", tt % 2)])
            transpose_tile(xt, ("xtile", tt % 2), tt)
        s.pop()

    def load_ln(l, gname, bname):
        DMA("sync", LN["g"][:], D[gname][l].partition_broadcast(128), w=["lng"])
        DMA("sync", LN["b"][:], D[bname][l].partition_broadcast(128), w=["lnb"])

    def ln_tile(xt, key, tt, dst, f32dst=None, f32key=None, do_T=True):
        for hf in range(2):
            V(lambda e, hf=hf: e.bn_stats(out=stat[:, hf, :], in_=xt[:, hf * 512:(hf + 1) * 512]), [key], ["stat"])
        V(lambda e: e.bn_aggr(out=mv[:, 0:2], in_=stat[:]), ["stat"], ["mv"])
        V(lambda e: e.tensor_scalar(out=mv[:, 3:4], in0=mv[:, 1:2], scalar1=1e-5, scalar2=None, op0=ALU.add), ["mv"], ["mv3"])
        A(lambda e: e.activation(out=mv[:, 3:4], in_=mv[:, 3:4], func=AF.Ln), ["mv3"], ["mv3"])
        A(lambda e: e.activation(out=mv[:, 2:3], in_=mv[:, 3:4], func=AF.Exp, scale=-0.5), ["mv3"], ["mv2"])
        V(lambda e: e.tensor_scalar(out=xt[:], in0=xt[:], scalar1=mv[:, 0:1], scalar2=mv[:, 2:3], op0=ALU.subtract,
                                    op1=ALU.mult), [key, "mv", "mv2"], [key])
        lg_, lb_ = LN["g"], LN["b"]
        G(lambda e: e.tensor_tensor(out=xt[:], in0=xt[:], in1=lg_[:], op=ALU.mult), [key, "lng"], [key])
        G(lambda e: e.tensor_tensor(out=xt[:], in0=xt[:], in1=lb_[:], op=ALU.add), [key, "lnb"], [key])
        DMA("sync", dst[tt * 128:(tt + 1) * 128, :], xt[:], r=[key], w=[("xs", tt)])
        if do_T:
            transpose_tile(xt, key, tt, f32dst, f32key)

    NBUF = 6

    def interleave(gens):
        gens = list(gens)
        while gens:
            for g_ in list(gens):
                try:
                    next(g_)
                except StopIteration:
                    gens.remove(g_)

    def ln_run(prod_phases, dst, do_T=True):
        bufs = [s.sb([128, DM], F32, f"lnx{i}") for i in range(NBUF)]
        stt = s.sb([128, NBUF, 2, 6], F32, "lnstat")
        mvt = s.sb([128, NBUF, 4], F32, "lnmv")
        lg_, lb_ = LN["g"], LN["b"]

        def p_stats(tt, xt, key, sl):
            for hf in range(2):
                V(lambda e, hf=hf: e.bn_stats(out=stt[:, sl, hf, :], in_=xt[:, hf * 512:(hf + 1) * 512]), [key], [("lnstat", sl)])
            V(lambda e: e.bn_aggr(out=mvt[:, sl, 0:2], in_=stt[:, sl, :, :]), [("lnstat", sl)], [("lnmv", sl)])
            V(lambda e: e.tensor_scalar(out=mvt[:, sl, 3:4], in0=mvt[:, sl, 1:2], scalar1=1e-5, scalar2=None, op0=ALU.add), [("lnmv", sl)], [("lnmv3", sl)])
            A(lambda e: e.activation(out=mvt[:, sl, 3:4], in_=mvt[:, sl, 3:4], func=AF.Ln), [("lnmv3", sl)], [("lnmv3", sl)])
            A(lambda e: e.activation(out=mvt[:, sl, 2:3], in_=mvt[:, sl, 3:4], func=AF.Exp, scale=-0.5), [("lnmv3", sl)], [("lnmv2", sl)])

        def p_norm(tt, xt, key, sl):
            V(lambda e: e.tensor_scalar(out=xt[:], in0=xt[:], scalar1=mvt[:, sl, 0:1], scalar2=mvt[:, sl, 2:3], op0=ALU.subtract,
                                        op1=ALU.mult), [key, ("lnmv", sl), ("lnmv2", sl)], [key])
            G(lambda e: e.tensor_tensor(out=xt[:], in0=xt[:], in1=lg_[:], op=ALU.mult), [key, "lng"], [key])
            G(lambda e: e.tensor_tensor(out=xt[:], in0=xt[:], in1=lb_[:], op=ALU.add), [key, "lnb"], [key])
            DMA("sync", dst[tt * 128:(tt + 1) * 128, :], xt[:], r=[key], w=[("xs", tt)])

        def p_T(tt, xt, key, sl):
            transpose_tile(xt, key, tt)

        phases = list(prod_phases) + [p_stats, p_norm] + ([p_T] if do_T else [])
        for step in range(16 + len(phases) - 1):
            for pi, ph in enumerate(phases):
                tt = step - pi
                if 0 <= tt < 16:
                    sl = tt % NBUF
                    ph(tt, bufs[sl], ("lnx", sl), sl)

    def proj_fm(pieces, m, evac, kchunks=8, rhsT=None, rkeys=None):
        i = next_wbuf()
        wb = wbuf[i]
        wk = []
        for pi, (o, ap) in enumerate(pieces):
            wd = ap.shape[1]
            DMA("gpsimd", wb[:, 0:kchunks, o:o + wd], ap.rearrange("(c p) m -> p c m", p=128), w=[("wbuf", i, pi)])
            wk.append(("wbuf", i, pi))
        for tq in range(4):
            b = gbank()
            for c in range(kchunks):
                MM(PS[b][0:m, :], wb[:, c, 0:m], xT[:, c, tq * 512:(tq + 1) * 512], c == 0, c == kchunks - 1,
                   wk + xkeys(tq), [("ps", b)])
            evac(PS[b], ("ps", b), tq)

    def proj_tm(pieces, n, evac):
        i = next_wbuf()
        wb = wbuf[i]
        wk = []
        for pi, (o, ap) in enumerate(pieces):
            wd = ap.shape[1]
            DMA("gpsimd", wb[:, :, o:o + wd], ap.rearrange("(c p) m -> p c m", p=128), w=[("wbuf", i, pi)])
            wk.append(("wbuf", i, pi))
        for tt in range(16):
            b = gbank()
            for c in range(8):
                MM(PS[b][:, 0:n], xT[:, c, tt * 128:(tt + 1) * 128], wb[:, c, 0:n], c == 0, c == 7,
                   wk + [("xT", tt)], [("ps", b)])
            evac(PS[b], ("ps", b), tt)

    ctr = {"sc": 0, "p": 0}
    AT = {}

    def attn(terms, kcs, vaug, ncols, pv_ok, accmap, scale=1.0):
        Pt = AT["P"]
        kcs = list(kcs)
        rng = {}
        for qs in range(4):
            ok = [k for k in kcs if pv_ok(k, qs)]
            rng[qs] = (ok[0], ok[-1])

        def score(kc):
            ctr["sc"] += 1
            sbk = ctr["sc"] % 2
            tl = terms(kc)
            for i, (lt, rh, rk) in enumerate(tl):
                MM(PS[sbk][:], lt, rh, i == 0, i == len(tl) - 1, rk, [("ps", sbk)])
            return sbk

        nxt = score(kcs[0])
        started = set()
        for idx, kc in enumerate(kcs):
            sbk = nxt
            ctr["p"] += 1
            pi = ctr["p"] % 3
            A(lambda e, pi=pi, sbk=sbk: e.activation(out=Pt[pi][:], in_=PS[sbk][:], func=AF.Exp, scale=scale),
              [("ps", sbk)], [("P", pi)])
            if idx + 1 < len(kcs):
                nxt = score(kcs[idx + 1])
            vap, vkey = vaug(kc)
            for qs in range(4):
                if not pv_ok(kc, qs):
                    continue
                bank, c0 = accmap(qs)
                first = bank not in started
                started.add(bank)
                MMs(PS[bank][:, c0:c0 + ncols], Pt[pi][:, qs * 128:(qs + 1) * 128], vap, first, kc == rng[qs][1],
                    [("P", pi), vkey], [("ps", bank)])

    def attn_out(accmap, dv, tt0, gate, dst, accumulate, normalize=True):
        rd = AT["rd"]

        def chain(qs):
            bank, c0 = accmap(qs)
            tt = tt0 + qs
            src = PS[bank][:, c0:c0 + dv]
            if not normalize:
                vcopy(dst(tt), src, [("ps", bank)], [("oacc", tt)])
                return
            V(lambda e: e.tensor_scalar(out=rd[:, qs, 2:3], in0=PS[bank][:, c0 + dv:c0 + dv + 1], scalar1=1e-30,
                                        scalar2=None, op0=ALU.max), [("ps", bank)], [("rd2", qs)])
            yield
            V(lambda e: e.reciprocal(out=rd[:, qs, 0:1], in_=rd[:, qs, 2:3]), [("rd2", qs)], [("rd0", qs)])
            yield
            sc = rd[:, qs, 0:1]
            rk = [("rd0", qs)]
            if gate is not None:
                gap = gate(tt)
                V(lambda e: e.tensor_tensor(out=rd[:, qs, 1:2], in0=rd[:, qs, 0:1], in1=gap, op=ALU.mult), [("rd0", qs), "gate"], [("rd1", qs)])
                yield
                sc = rd[:, qs, 1:2]
                rk = [("rd1", qs)]
            d = dst(tt)
            if accumulate:
                V(lambda e: e.scalar_tensor_tensor(out=d, in0=src, scalar=sc, in1=d, op0=ALU.mult, op1=ALU.add),
                  [("ps", bank), ("oacc", tt)] + rk, [("oacc", tt)])
            else:
                V(lambda e: e.tensor_scalar(out=d, in0=src, scalar1=sc, scalar2=None, op0=ALU.mult),
                  [("ps", bank)] + rk, [("oacc", tt)])
            yield
        interleave([chain(qs) for qs in range(4)])

    def to_fm(src, nchunk, dstT, dkey):
        for tt in range(16):
            b = 6 + tt % 2
            for c in range(nchunk):
                TR(PS[b][:, c * 128:(c + 1) * 128], src[:, tt, c * 128:(c + 1) * 128], ident[:], [("oacc", tt), "ident"], [("ps", b)])
            vcopy(dstT[:, 0:nchunk, tt * 128:(tt + 1) * 128], PS[b][:, 0:nchunk * 128].rearrange("p (j q) -> p j q", j=nchunk),
                  [("ps", b)], [(dkey, tt)])

    def tqs(tq):
        return slice(tq * 512, (tq + 1) * 512)

    def nsa(l, W, oacc):
        s.push()
        qa = [s.sb([68, S], BF16, f"qa{h}") for h in range(4)]
        kcA = [s.sb([68, 128], BF16, f"kcA{g}") for g in range(2)]
        vcA = [s.sb([128, 97], BF16, f"vcA{g}") for g in range(2)]
        gate = s.sb([128, 16, 12], F32, "gate")
        selTb = [s.sb([32, S], BF16, f"selTb{g}") for g in range(2)]
        cmaskb = s.sb([128, S], BF16, "cmaskb")
        selE = s.sb([32, 16, 128], BF16, "selE")
        vm = s.sb([128, 16, 32], F32, "vm")
        am = s.sb([128, 16, 32], F32, "am")
        imp = s.sb([128, 32], F32, "imp")
        impf = s.sb([128, 32], F32, "impf")
        top8 = s.sb([128, 8], F32, "top8")
        selb = s.sb([128, 32], F32, "selb")
        rdn = s.sb([128, 8], F32, "rdn")
        DMA("gpsimd", cmaskb[:], D["c_cmask"], w=["cmaskb"])
        DMA("gpsimd", selE[:], D["c_selE"], w=["selE"])
        DMA("sync", vm[:], D["c_impvm"], w=["vm"])
        DMA("sync", am[:], D["c_impam"], w=["am"])
        for h in range(4):
            DMA("gpsimd", qa[h][64:68, :], D["c_qaug"][h], w=[("qa", h, "aug")])
        for g in range(2):
            DMA("gpsimd", kcA[g][64:68, :], D["c_kcaug"], w=[("kcA", g, "aug")])
            V(lambda e, g=g: e.memset(vcA[g][:, 0:64], 0.0), [], [("vcA", g, "v")])
            V(lambda e, g=g: e.memset(vcA[g][:, 64:65], 1.0), [], [("vcA", g, "one")])
            DMA("gpsimd", vcA[g][:, 65:97], D["c_cover"], w=[("vcA", g, "cov")])
            V(lambda e, g=g: e.memset(kcA[g][0:64, :], 0.0), [], [("kcA", g, "k")])
        for h in range(4):
            def ev(ps, key, tq, h=h):
                V(lambda e: e.tensor_scalar(out=qa[h][0:64, tqs(tq)], in0=ps[0:64, :], scalar1=0.125, scalar2=None, op0=ALU.mult),
                  [key], [("qa", h, tq)])
            proj_fm([(0, W[:, h * 64:(h + 1) * 64])], 64, ev)

        s.push()
        srcT = [s.sb([64, S], BF16, f"srcT{g}") for g in range(2)]
        w1t = s.sb([64, 32, 256], BF16, "w1t")
        w2t = s.sb([128, 2, 64], BF16, "w2t")
        posr = s.sb([32, 64], F32, "posr")
        posT = s.sb([64, 32], BF16, "posT")
        hb = s.sb([128, 2], F32, "hb")
        hidT = s.sb([128, 2, 128], BF16, "hidT")
        for j in range(2):
            for g in range(2):
                def ev(ps, key, tq, g=g):
                    vcopy(srcT[g][:, tqs(tq)], ps[0:64, :], [key], [("srcT", g, tq)])
                c0 = 256 + 128 * j + 64 * g
                proj_fm([(0, W[:, c0:c0 + 64])], 64, ev)
            DMA("gpsimd", w1t[:], D["nsa_cmp_w1"][l, j].rearrange("(l d) h -> d l h", d=64), w=["w1t"])
            DMA("gpsimd", w2t[:], D["nsa_cmp_w2"][l, j].rearrange("(c p) d -> p c d", p=128), w=["w2t"])
            DMA("sync", posr[:], D["nsa_cmp_pos"][l, j], w=["posr"])
            TR(PS[6][0:64, 0:32], posr[:, :], ident[0:32, 0:32], ["posr", "ident"], [("ps", 6)])
            vcopy(posT[:], PS[6][0:64, 0:32], [("ps", 6)], ["posT"])
            for hc in range(2):
                for li in range(32):
                    MM(PS[7][:, hc:hc + 1], w1t[:, li, hc * 128:(hc + 1) * 128], posT[:, li:li + 1], li == 0, li == 31,
                       ["w1t", "posT"], [("ps", 7)])
            vcopy(hb[:], PS[7][:, 0:2], [("ps", 7)], ["hb"])
            for g in range(2):
                sk = [("srcT", g, tq) for tq in range(4)]
                for hc in range(2):
                    b = gbank()
                    for li in range(32):
                        MM(PS[b][:, 0:127], w1t[:, li, hc * 128:(hc + 1) * 128], srcT[g][:, li:li + 2017:16], li == 0, li == 31,
                           ["w1t"] + sk, [("ps", b)])
                    A(lambda e, b=b, hc=hc: e.activation(out=hidT[:, hc, 0:127], in_=PS[b][:, 0:127], func=AF.Gelu, bias=hb[:, hc:hc + 1]),
                      [("ps", b), "hb"], [("hidT", hc)])
                b = gbank()
                if j == 0:
                    for hc in range(2):
                        MM(PS[b][0:64, 0:127], w2t[:, hc, :], hidT[:, hc, 0:127], hc == 0, hc == 1, ["w2t", ("hidT", hc)], [("ps", b)])
                    vcopy(kcA[g][0:64, 0:127], PS[b][0:64, 0:127], [("ps", b)], [("kcA", g, "k")])
                else:
                    for hc in range(2):
                        MM(PS[b][0:127, 0:64], hidT[:, hc, 0:127], w2t[:, hc, :], hc == 0, hc == 1, ["w2t", ("hidT", hc)], [("ps", b)])
                    vcopy(vcA[g][0:127, 0:64], PS[b][0:127, 0:64], [("ps", b)], [("vcA", g, "v")])
        s.pop()

        s.push()
        ksA = [s.sb([68, S], BF16, f"ksA{g}") for g in range(2)]
        kwA = [s.sb([68, S], BF16, f"kwA{g}") for g in range(2)]
        vsA = s.sb([128, 16, 2, 65], BF16, "vsA")
        vwA = s.sb([128, 16, 2, 65], BF16, "vwA")
        V(lambda e: e.memset(vsA[:, :, :, 64:65], 1.0), [], [("vsA", tt) for tt in range(16)])
        V(lambda e: e.memset(vwA[:, :, :, 64:65], 1.0), [], [("vwA", tt) for tt in range(16)])
        for g in range(2):
            DMA("gpsimd", ksA[g][64:68, :], D["c_kaug"], w=[("ksA", g, "aug")])
            DMA("gpsimd", kwA[g][64:68, :], D["c_kaug"], w=[("kwA", g, "aug")])
            for nm, dst, c0 in (("ksA", ksA, 512), ("kwA", kwA, 768)):
                def ev(ps, key, tq, dst=dst, nm=nm, g=g):
                    vcopy(dst[g][0:64, tqs(tq)], ps[0:64, :], [key], [(nm, g, tq)])
                proj_fm([(0, W[:, c0 + 64 * g:c0 + 64 * g + 64])], 64, ev)

        def evv(ps, key, tt):
            vcopy(vsA[:, tt, :, 0:64], ps[:, 0:128].rearrange("p (g d) -> p g d", g=2), [key], [("vsA", tt)])
            vcopy(vwA[:, tt, :, 0:64], ps[:, 128:256].rearrange("p (g d) -> p g d", g=2), [key], [("vwA", tt)])
            vcopy(gate[:, tt, :], ps[:, 256:268], [key], [("gateraw", tt)])
            A(lambda e: e.activation(out=gate[:, tt, :], in_=gate[:, tt, :], func=AF.Sigmoid), [("gateraw", tt)], ["gate"])
        proj_tm([(0, W[:, 640:768]), (128, W[:, 896:1024]), (256, W[:, 1024:1036])], 268, evv)

        for g in range(2):
            for qt in range(4):
                for n in range(2):
                    h = 2 * g + n
                    MM(PS[n][:], kcA[g][:, :], qa[h][:, tqs(qt)], True, False,
                       [("kcA", g, "k"), ("kcA", g, "aug"), ("qa", h, qt), ("qa", h, "aug")], [("ps", n)])
                    MM(PS[n][:], identb[:], cmaskb[:, tqs(qt)], False, True, ["identb", "cmaskb"], [("ps", n)])
                    Pn = AT["P"][n]
                    A(lambda e, n=n, Pn=Pn: e.activation(out=Pn[:], in_=PS[n][:], func=AF.Exp), [("ps", n)], [("P", n)])
                    for qs in range(4):
                        MM(PS[2 + n][:, qs * 97:(qs + 1) * 97], Pn[:, qs * 128:(qs + 1) * 128], vcA[g][:, :], True, True,
                           [("P", n), ("vcA", g, "v"), ("vcA", g, "one"), ("vcA", g, "cov")], [("ps", 2 + n)])
                for qs in range(4):
                    tt = 4 * qt + qs
                    for n in range(2):
                        h = 2 * g + n
                        c0 = qs * 97
                        V(lambda e, n=n, c0=c0: e.tensor_scalar(out=rdn[:, 4 + n:5 + n], in0=PS[2 + n][:, c0 + 64:c0 + 65], scalar1=1e-30,
                                                               scalar2=None, op0=ALU.max), [("ps", 2 + n)], [("rdn", 4 + n)])
                        V(lambda e, n=n: e.reciprocal(out=rdn[:, n:n + 1], in_=rdn[:, 4 + n:5 + n]), [("rdn", 4 + n)], [("rdn", n)])
                        V(lambda e, n=n, h=h, tt=tt: e.tensor_tensor(out=rdn[:, 2 + n:3 + n], in0=rdn[:, n:n + 1], in1=gate[:, tt, 3 * h:3 * h + 1],
                                                                      op=ALU.mult), [("rdn", n), "gate"], [("rdn", 2 + n)])
                        V(lambda e, n=n, h=h, tt=tt, c0=c0: e.tensor_scalar(out=oacc[:, tt, h * 64:(h + 1) * 64], in0=PS[2 + n][:, c0:c0 + 64],
                                                                         scalar1=rdn[:, 2 + n:3 + n], scalar2=None, op0=ALU.mult),
                          [("ps", 2 + n), ("rdn", 2 + n)], [("oacc", tt)])
                    c0 = qs * 97
                    V(lambda e, c0=c0: e.tensor_scalar(out=imp[:], in0=PS[2][:, c0 + 65:c0 + 97], scalar1=rdn[:, 0:1], scalar2=None, op0=ALU.mult),
                      [("ps", 2), ("rdn", 0)], ["imp"])
                    V(lambda e, c0=c0: e.scalar_tensor_tensor(out=imp[:], in0=PS[3][:, c0 + 65:c0 + 97], scalar=rdn[:, 1:2], in1=imp[:],
                                                             op0=ALU.mult, op1=ALU.add), [("ps", 3), ("rdn", 1), "imp"], ["imp"])
                    V(lambda e, tt=tt: e.tensor_tensor(out=impf[:], in0=imp[:], in1=vm[:, tt, :], op=ALU.mult), ["imp", "vm"], ["impf"])
                    V(lambda e, tt=tt: e.tensor_tensor(out=impf[:], in0=impf[:], in1=am[:, tt, :], op=ALU.add), ["impf", "am"], ["impf"])
                    V(lambda e: e.max(out=top8[:], in_=impf[:]), ["impf"], ["top8"])
                    V(lambda e: e.tensor_scalar(out=selb[:], in0=impf[:], scalar1=top8[:, 7:8], scalar2=None, op0=ALU.is_ge),
                      ["impf", "top8"], ["selb"])
                    V(lambda e: e.tensor_scalar(out=selb[:], in0=selb[:], scalar1=-1.0, scalar2=-NEG, op0=ALU.add, op1=ALU.mult),
                      ["selb"], ["selb"])
                    TR(PS[6][0:32, qs * 128:(qs + 1) * 128], selb[:, :], ident[:], ["selb", "ident"], [("ps", 6)])
                vcopy(selTb[g][:, tqs(qt)], PS[6][0:32, :], [("ps", 6)], [("selTb", g, qt)])

        it = 0
        for br, kA, vA, nm, vnm in ((1, ksA, vsA, "ksA", "vsA"), (2, kwA, vwA, "kwA", "vwA")):
            for h in range(4):
                g = h // 2
                for qt in range(4):
                    it += 1
                    bank = 2 + it % 2
                    if br == 1:
                        kcs = range(0, 4 * qt + 4)
                        pv_ok = lambda kc, qs, qt=qt: kc <= 4 * qt + qs
                    else:
                        kcs = range(max(0, 4 * qt - 4), 4 * qt + 4)
                        pv_ok = lambda kc, qs, qt=qt: 4 * qt + qs - 4 <= kc <= 4 * qt + qs

                    def terms(kc, h=h, g=g, qt=qt, br=br, kA=kA, nm=nm):
                        tl = [(kA[g][:, kc * 128:(kc + 1) * 128], qa[h][:, tqs(qt)],
                               [(nm, g, kc // 4), (nm, g, "aug"), ("qa", h, qt), ("qa", h, "aug")])]
                        if br == 1:
                            tl.append((selE[:, kc, :], selTb[g][:, tqs(qt)], ["selE", ("selTb", g, qt)]))
                        if kc >= 4 * qt:
                            tl.append((identb[:], masks[:, kc - 4 * qt, :], ["identb", "masks"]))
                        elif br == 2:
                            tl.append((identb[:], masks[:, 8 + kc - (4 * qt - 4), :], ["identb", "masks"]))
                        return tl

                    def vaug(kc, g=g, vA=vA, vnm=vnm):
                        return vA[:, kc, g, :], (vnm, kc)

                    accmap = lambda qs, bank=bank: (bank, qs * 65)
                    attn(terms, kcs, vaug, 65, pv_ok, accmap)
                    attn_out(accmap, 64, 4 * qt, lambda tt, h=h, br=br: gate[:, tt, 3 * h + br:3 * h + br + 1],
                             lambda tt, h=h: oacc[:, tt, h * 64:(h + 1) * 64], True)
        s.pop()
        s.pop()

    def sbranch(l, W, oacc):
        s.push()
        qT = [s.sb([64, S], BF16, f"sbq{h}") for h in range(4)]
        kT = [s.sb([64, S], BF16, f"sbk{h}") for h in range(4)]
        vB = s.sb([128, 16, 256], BF16, "sbv")
        negU = s.sb([128, 128], BF16, "negU")
        negO = s.sb([128, 128], F32, "negO")
        et = [s.sb([128, 512], F32, f"et{i}") for i in range(2)]
        spt = [s.sb([128, 512], BF16, f"spt{i}") for i in range(3)]
        acc = [s.sb([128, 512], F32, f"sbacc{i}") for i in range(2)]
        DMA("gpsimd", negU[:], D["c_negU"], w=["negU"])
        V(lambda e: e.memset(negO[:], -1.0), [], ["negO"])
        for h in range(4):
            def evq(ps, key, tq, h=h):
                V(lambda e: e.tensor_scalar(out=qT[h][:, tqs(tq)], in0=ps[0:64, :], scalar1=0.125, scalar2=None, op0=ALU.mult),
                  [key], [("sbq", h, tq)])
            proj_fm([(0, W[:, 1036 + h * 64:1036 + (h + 1) * 64])], 64, evq)

            def evk(ps, key, tq, h=h):
                vcopy(kT[h][:, tqs(tq)], ps[0:64, :], [key], [("sbk", h, tq)])
            proj_fm([(0, W[:, 1292 + h * 64:1292 + (h + 1) * 64])], 64, evk)

        def evv(ps, key, tt):
            vcopy(vB[:, tt, :], ps[:, 0:256], [key], [("sbv", tt)])
        proj_tm([(0, W[:, 1548:1804])], 256, evv)

        SB3 = (0, 1, 5)
        st = {"i": 0, "a": 0, "it": 0}
        for h in range(4):
            for qt in range(4):
                st["it"] += 1
                bank = 2 + st["it"] % 2
                kcs = list(range(4 * qt + 3, -1, -1))

                def stageA(kc, h=h, qt=qt):
                    st["i"] += 1
                    i = st["i"]
                    sbk = SB3[i % 3]
                    MM(PS[sbk][:], kT[h][:, kc * 128:(kc + 1) * 128], qT[h][:, tqs(qt)], True, False,
                       [("sbk", h, kc // 4), ("sbq", h, qt)], [("ps", sbk)])
                    if kc >= 4 * qt:
                        MM(PS[sbk][:], identb[:], masks[:, 4 + kc - 4 * qt, :], False, False, ["identb", "masks"], [("ps", sbk)])
                    A(lambda e: e.activation(out=et[i % 2][:], in_=PS[sbk][:], func=AF.Exp), [("ps", sbk)], [("et", i % 2)])
                    A(lambda e: e.activation(out=spt[i % 3][:], in_=et[i % 2][:], func=AF.Ln, bias=1.0), [("et", i % 2)], [("spt", i % 3)])
                    return i

                def stageB(kc, i, idx, h=h, qt=qt, bank=bank):
                    sbk = SB3[i % 3]
                    a = st["a"]
                    MM(PS[sbk][:], negU[:], spt[i % 3][:], False, idx == 0, ["negU", ("spt", i % 3)], [("ps", sbk)])
                    if idx > 0:
                        MM(PS[sbk][:], negO[:], acc[a % 2][:], False, True, ["negO", ("sbacc", a % 2)], [("ps", sbk)])
                    ctr["p"] += 1
                    pi = ctr["p"] % 3
                    Pp = AT["P"][pi]
                    A(lambda e: e.activation(out=Pp[:], in_=PS[sbk][:], func=AF.Exp), [("ps", sbk)], [("P", pi)])
                    for qs in range(4):
                        if kc > 4 * qt + qs:
                            continue
                        MMs(PS[bank][:, qs * 64:(qs + 1) * 64], Pp[:, qs * 128:(qs + 1) * 128], vB[:, kc, h * 64:(h + 1) * 64],
                            idx == 0 and qs == 3, kc == 0, [("P", pi), ("sbv", kc)], [("ps", bank)])
                    if kc > 0:
                        if idx == 0:
                            G(lambda e: e.tensor_copy(out=acc[(a + 1) % 2][:], in_=spt[i % 3][:]), [("spt", i % 3)], [("sbacc", (a + 1) % 2)])
                        else:
                            G(lambda e: e.tensor_tensor(out=acc[(a + 1) % 2][:], in0=acc[a % 2][:], in1=spt[i % 3][:], op=ALU.add),
                              [("spt", i % 3), ("sbacc", a % 2)], [("sbacc", (a + 1) % 2)])
                        st["a"] += 1

                cur = stageA(kcs[0])
                for idx, kc in enumerate(kcs):
                    nxt = stageA(kcs[idx + 1]) if idx + 1 < len(kcs) else None
                    stageB(kc, cur, idx)
                    cur = nxt
                accmap = lambda qs, bank=bank: (bank, qs * 64)
                attn_out(accmap, 64, 4 * qt, None, lambda tt, h=h: oacc[:, tt, h * 64:(h + 1) * 64], False, normalize=False)
        s.pop()

    def rglru(l, W, oT):
        s.push()
        xr = s.sb([128, S + 3], F32, "xr")
        xg = s.sb([128, S], F32, "xg")
        u = s.sb([128, S], F32, "u")
        ub = s.sb([128, S], BF16, "ub")
        ra = s.sb([128, S], F32, "ra")
        ib = s.sb([128, S], F32, "ib")
        hh = s.sb([128, S], F32, "hh")
        prm = s.sb([128, 12], F32, "prm")
        gw = [s.sb([128, 128], BF16, f"gw{i}") for i in range(2)]
        for ch in range(2):
            cs = slice(ch * 128, (ch + 1) * 128)
            for tap in range(4):
                DMA("sync", prm[:, tap:tap + 1], D["rnn_conv_w"][l, tap, cs].rearrange("(c o) -> c o", o=1), w=[("prm", tap)])
            for i, nm in enumerate(("rnn_conv_b", "rnn_ga_b", "rnn_gx_b", "rnn_lambda")):
                DMA("sync", prm[:, 4 + i:5 + i], D[nm][l, cs].rearrange("(c o) -> c o", o=1), w=[("prm", 4 + i)])
            for i, nm in enumerate(("rnn_ga_w", "rnn_gx_w")):
                V(lambda e, i=i: e.memset(gw[i][:], 0.0), [], [("gw", i)])
                for n in range(2):
                    DMA("gpsimd", gw[i][n * 64:(n + 1) * 64, n * 64:(n + 1) * 64], D[nm][l, 2 * ch + n], w=[("gw", i)])
            A(lambda e: e.activation(out=prm[:, 9:10], in_=prm[:, 7:8], func=AF.Exp, scale=-1.0), [("prm", 7)], [("prm", 9)])
            A(lambda e: e.activation(out=prm[:, 9:10], in_=prm[:, 9:10], func=AF.Ln, bias=1.0), [("prm", 9)], [("prm", 9)])
            V(lambda e: e.tensor_scalar(out=prm[:, 8:9], in0=prm[:, 9:10], scalar1=-8.0, scalar2=None, op0=ALU.mult), [("prm", 9)], [("prm", 8)])
            V(lambda e: e.memset(xr[:, 0:3], 0.0), [], [("xr", "pad")])

            def evx(ps, key, tq):
                vcopy(xr[:, 3 + tq * 512:3 + (tq + 1) * 512], ps[:, :], [key], [("xr", tq)])
            proj_fm([(0, W[:, 1804 + ch * 128:1804 + (ch + 1) * 128])], 128, evx)

            def evg(ps, key, tq):
                A(lambda e: e.activation(out=xg[:, tqs(tq)], in_=ps[:, :], func=AF.Gelu), [key], [("xg", tq)])
            proj_fm([(0, W[:, 2060 + ch * 128:2060 + (ch + 1) * 128])], 128, evg)
            xk = [("xr", tq) for tq in range(4)] + [("xr", "pad")]
            V(lambda e: e.tensor_scalar(out=u[:], in0=xr[:, 0:S], scalar1=prm[:, 0:1], scalar2=prm[:, 4:5], op0=ALU.mult, op1=ALU.add),
              xk + [("prm", 0), ("prm", 4)], ["u"])
            for tap in range(1, 4):
                V(lambda e, tap=tap: e.scalar_tensor_tensor(out=u[:], in0=xr[:, tap:tap + S], scalar=prm[:, tap:tap + 1], in1=u[:],
                                                            op0=ALU.mult, op1=ALU.add), xk + [("prm", tap), "u"], ["u"])
            vcopy(ub[:], u[:], ["u"], ["ub"])
            for tq in range(4):
                for i, dst, bcol in ((0, ra, 5), (1, ib, 6)):
                    b = gbank()
                    MM(PS[b][:], gw[i][:], ub[:, tqs(tq)], True, True, [("gw", i), "ub"], [("ps", b)])
                    A(lambda e, b=b, dst=dst, bcol=bcol, tq=tq: e.activation(out=dst[:, tqs(tq)], in_=PS[b][:], func=AF.Sigmoid,
                                                                             bias=prm[:, bcol:bcol + 1]), [("ps", b), ("prm", bcol)], [(("ra", "ib")[i], tq)])
            rk = [("ra", tq) for tq in range(4)]
            ik = [("ib", tq) for tq in range(4)]
            A(lambda e: e.activation(out=ra[:], in_=ra[:], func=AF.Exp, scale=prm[:, 8:9]), rk + [("prm", 8)], rk)
            V(lambda e: e.tensor_tensor(out=ib[:], in0=ib[:], in1=u[:], op=ALU.mult), ik + ["u"], ik)
            V(lambda e: e.tensor_tensor(out=hh[:], in0=ra[:], in1=ra[:], op=ALU.mult), rk, ["hh"])
            V(lambda e: e.tensor_scalar(out=hh[:], in0=hh[:], scalar1=-1.0, scalar2=1.0, op0=ALU.mult, op1=ALU.add), ["hh"], ["hh"])
            V(lambda e: e.tensor_scalar(out=hh[:], in0=hh[:], scalar1=0.0, scalar2=None, op0=ALU.max), ["hh"], ["hh"])
            A(lambda e: e.activation(out=hh[:], in_=hh[:], func=AF.Sqrt), ["hh"], ["hh"])
            V(lambda e: e.tensor_tensor(out=ib[:], in0=ib[:], in1=hh[:], op=ALU.mult), ik + ["hh"], ik)
            V(lambda e: e.tensor_tensor_scan(out=hh[:], data0=ra[:], data1=ib[:], initial=0.0, op0=ALU.mult, op1=ALU.add),
              rk + ik + ["hh"], ["hh"])
            V(lambda e, ch=ch: e.tensor_tensor(out=oT[:, 2, ch, :], in0=hh[:], in1=xg[:], op=ALU.mult),
              ["hh"] + [("xg", tq) for tq in range(4)], [("oT", 2, ch)])
        s.pop()

    def mla(l, W, oacc):
        s.push()
        cqn = s.sb([128, 2, S], BF16, "cqn")
        ckvn = s.sb([128, S], BF16, "ckvn")
        wuq = s.sb([128, 2, 384], BF16, "wuq")
        wuqS = s.sb([128, 2, 384], BF16, "wuqS")
        wukv = s.sb([128, 512], BF16, "wukv")
        gq = s.sb([128, 2], F32, "gq")
        gkv = s.sb([128, 1], F32, "gkv")
        onesF = s.sb([128, 128], F32, "onesF")
        V(lambda e: e.memset(onesF[:], 1.0), [], ["onesF"])
        V(lambda e: e.memset(wuqS[:], 0.0), [], ["wuqS"])
        V(lambda e: e.memset(cqn[:], 0.0), [], ["cqn0"])
        Wq = D["mla_w_uq"][l]
        DMA("gpsimd", wuq[:, 0, :], Wq[0:128, :], w=[("wuq", 0)])
        DMA("gpsimd", wuq[0:64, 1, :], Wq[128:192, :], w=[("wuq", 1)])
        for h in range(4):
            for (r0, r1, cidx) in ((0, 128, 0), (128, 192, 1)):
                np_ = r1 - r0
                DMA("gpsimd", wuqS[0:np_, cidx, h * 96 + 64:h * 96 + 80], Wq[r0:r1, h * 96 + 80:h * 96 + 96], r=["wuqS"], w=[("wuqS", h, cidx, 0)])
                DMA("gpsimd", wuqS[0:np_, cidx, h * 96 + 80:h * 96 + 96], Wq[r0:r1, h * 96 + 64:h * 96 + 80], r=["wuqS"], w=[("wuqS", h, cidx, 1)])
        DMA("gpsimd", wukv[:], D["mla_w_ukv"][l], w=["wukv"])
        DMA("sync", gq[:, 0:1], D["mla_q_norm"][l, 0:128].rearrange("(c o) -> c o", o=1), w=[("gq", 0)])
        DMA("sync", gq[0:64, 1:2], D["mla_q_norm"][l, 128:192].rearrange("(c o) -> c o", o=1), w=[("gq", 1)])
        DMA("sync", gkv[:, 0:1], D["mla_kv_norm"][l].rearrange("(c o) -> c o", o=1), w=["gkv"])

        s.push()
        cq = s.sb([128, 2, S], F32, "cq")
        ckv = s.sb([128, S], F32, "ckv")
        sq = s.sb([128, 2, 512], F32, "sq")
        rstd = s.sb([128, 512], F32, "rstd")

        def ev0(ps, key, tq):
            vcopy(cq[:, 0, tqs(tq)], ps[:, :], [key], [("cq", 0, tq)])
        proj_fm([(0, W[:, 2316:2444])], 128, ev0)

        def ev1(ps, key, tq):
            vcopy(cq[0:64, 1, tqs(tq)], ps[0:64, :], [key], [("cq", 1, tq)])
        proj_fm([(0, W[:, 2444:2508])], 64, ev1)

        def ev2(ps, key, tq):
            vcopy(ckv[:, tqs(tq)], ps[:, :], [key], [("ckv", tq)])
        proj_fm([(0, W[:, 2508:2636])], 128, ev2)
        for tq in range(4):
            V(lambda e, tq=tq: e.tensor_tensor(out=sq[:, 0, :], in0=cq[:, 0, tqs(tq)], in1=cq[:, 0, tqs(tq)], op=ALU.mult), [("cq", 0, tq)], [("sq", 0)])
            V(lambda e, tq=tq: e.tensor_tensor(out=sq[0:64, 1, :], in0=cq[0:64, 1, tqs(tq)], in1=cq[0:64, 1, tqs(tq)], op=ALU.mult), [("cq", 1, tq)], [("sq", 1)])
            b = gbank()
            MM(PS[b][:], onesF[:, :], sq[:, 0, :], True, False, ["onesF", ("sq", 0)], [("ps", b)])
            MM(PS[b][:], onesF[0:64, :], sq[0:64, 1, :], False, True, ["onesF", ("sq", 1)], [("ps", b)])
            V(lambda e, b=b: e.tensor_scalar(out=rstd[:], in0=PS[b][:], scalar1=1.0 / 192, scalar2=1e-6, op0=ALU.mult, op1=ALU.add), [("ps", b)], ["rstd"])
            A(lambda e: e.activation(out=rstd[:], in_=rstd[:], func=AF.Ln), ["rstd"], ["rstd"])
            A(lambda e: e.activation(out=rstd[:], in_=rstd[:], func=AF.Exp, scale=-0.5), ["rstd"], ["rstd"])
            V(lambda e, tq=tq: e.scalar_tensor_tensor(out=cqn[:, 0, tqs(tq)], in0=cq[:, 0, tqs(tq)], scalar=gq[:, 0:1], in1=rstd[:], op0=ALU.mult, op1=ALU.mult),
              [("cq", 0, tq), ("gq", 0), "rstd", "cqn0"], [("cqn", tq, 0)])
            V(lambda e, tq=tq: e.scalar_tensor_tensor(out=cqn[0:64, 1, tqs(tq)], in0=cq[0:64, 1, tqs(tq)], scalar=gq[0:64, 1:2], in1=rstd[0:64, :], op0=ALU.mult, op1=ALU.mult),
              [("cq", 1, tq), ("gq", 1), "rstd", "cqn0"], [("cqn", tq, 1)])
            V(lambda e, tq=tq: e.tensor_tensor(out=sq[:, 0, :], in0=ckv[:, tqs(tq)], in1=ckv[:, tqs(tq)], op=ALU.mult), [("ckv", tq)], [("sq", 0)])
            b = gbank()
            MM(PS[b][:], onesF[:, :], sq[:, 0, :], True, True, ["onesF", ("sq", 0)], [("ps", b)])
            V(lambda e, b=b: e.tensor_scalar(out=rstd[:], in0=PS[b][:], scalar1=1.0 / 128, scalar2=1e-6, op0=ALU.mult, op1=ALU.add), [("ps", b)], ["rstd"])
            A(lambda e: e.activation(out=rstd[:], in_=rstd[:], func=AF.Ln), ["rstd"], ["rstd"])
            A(lambda e: e.activation(out=rstd[:], in_=rstd[:], func=AF.Exp, scale=-0.5), ["rstd"], ["rstd"])
            V(lambda e, tq=tq: e.scalar_tensor_tensor(out=ckvn[:, tqs(tq)], in0=ckv[:, tqs(tq)], scalar=gkv[:, 0:1], in1=rstd[:], op0=ALU.mult, op1=ALU.mult),
              [("ckv", tq), "gkv", "rstd"], [("ckvn", tq)])
        s.pop()

        s.push()
        QT = [s.sb([96, S], BF16, f"mq{h}") for h in range(4)]
        KT = [s.sb([96, S], BF16, f"mk{h}") for h in range(4)]
        vA = s.sb([128, 16, 4, 65], BF16, "mv")
        ropec = s.sb([96, S], F32, "ropec")
        ropes = s.sb([96, S], F32, "ropes")
        t1 = s.sb([96, 512], F32, "t1")
        t2 = s.sb([96, 512], F32, "t2")
        krw = s.sb([128, 8, 96], BF16, "krw")
        krwS = s.sb([128, 8, 96], BF16, "krwS")
        DMA("sync", ropec[:], D["c_ropec"], w=["ropec"])
        DMA("sync", ropes[:], D["c_ropes"], w=["ropes"])
        V(lambda e: e.memset(vA[:, :, :, 64:65], 1.0), [], [("mv", tt) for tt in range(16)])
        V(lambda e: e.memset(krw[:], 0.0), [], ["krw"])
        V(lambda e: e.memset(krwS[:], 0.0), [], ["krwS"])
        DMA("gpsimd", krw[:, :, 64:96], W[:, 2636:2668].rearrange("(c p) m -> p c m", p=128), r=["krw"], w=["krw1"])
        DMA("gpsimd", krwS[:, :, 64:80], W[:, 2652:2668].rearrange("(c p) m -> p c m", p=128), r=["krwS"], w=["krwS1"])
        DMA("gpsimd", krwS[:, :, 80:96], W[:, 2636:2652].rearrange("(c p) m -> p c m", p=128), r=["krwS"], w=["krwS2"])

        def rope_comb(pa, pb, ka, kb, dst, dkey, tq):
            V(lambda e: e.tensor_tensor(out=t1[64:96, :], in0=pa[64:96, :], in1=ropec[64:96, tqs(tq)], op=ALU.mult), [ka, "ropec"], ["t1"])
            V(lambda e: e.tensor_tensor(out=t2[64:96, :], in0=pb[64:96, :], in1=ropes[64:96, tqs(tq)], op=ALU.mult), [kb, "ropes"], ["t2"])
            V(lambda e: e.tensor_tensor(out=dst[64:96, tqs(tq)], in0=t1[64:96, :], in1=t2[64:96, :], op=ALU.add), ["t1", "t2"], [dkey])

        for tq in range(4):
            for c in range(8):
                MM(PS[4][0:96, :], krw[:, c, :], xT[:, c, tqs(tq)], c == 0, c == 7, ["krw", "krw1"] + xkeys(tq), [("ps", 4)])
            for c in range(8):
                MM(PS[5][0:96, :], krwS[:, c, :], xT[:, c, tqs(tq)], c == 0, c == 7, ["krwS", "krwS1", "krwS2"] + xkeys(tq), [("ps", 5)])
            rope_comb(PS[4], PS[5], ("ps", 4), ("ps", 5), KT[0], ("mk", 0, tq, "r"), tq)
            for h in range(1, 4):
                vcopy(KT[h][64:96, tqs(tq)], KT[0][64:96, tqs(tq)], [("mk", 0, tq, "r")], [("mk", h, tq, "r")])
        cqk = lambda tq: [("cqn", tq, 0), ("cqn", tq, 1), "cqn0"]
        for h in range(4):
            for tq in range(4):
                hs = slice(h * 96, (h + 1) * 96)
                MM(PS[4][0:96, :], wuq[:, 0, hs], cqn[:, 0, tqs(tq)], True, False, [("wuq", 0)] + cqk(tq), [("ps", 4)])
                MM(PS[4][0:96, :], wuq[0:64, 1, hs], cqn[0:64, 1, tqs(tq)], False, True, [("wuq", 1)] + cqk(tq), [("ps", 4)])
                wsk = ["wuqS"] + [("wuqS", h, ci, j) for ci in range(2) for j in range(2)]
                MM(PS[5][0:96, :], wuqS[:, 0, hs], cqn[:, 0, tqs(tq)], True, False, wsk + cqk(tq), [("ps", 5)])
                MM(PS[5][0:96, :], wuqS[0:64, 1, hs], cqn[0:64, 1, tqs(tq)], False, True, wsk + cqk(tq), [("ps", 5)])
                vcopy(QT[h][0:64, tqs(tq)], PS[4][0:64, :], [("ps", 4)], [("mq", h, tq)])
                rope_comb(PS[4], PS[5], ("ps", 4), ("ps", 5), QT[h], ("mq", h, tq, "r"), tq)
                b = 6 + tq % 2
                MM(PS[b][0:64, :], wukv[:, h * 128:h * 128 + 64], ckvn[:, tqs(tq)], True, True, ["wukv", ("ckvn", tq)], [("ps", b)])
                vcopy(KT[h][0:64, tqs(tq)], PS[b][0:64, :], [("ps", b)], [("mk", h, tq)])
        for tt in range(16):
            b = 6 + tt % 2
            for h in range(4):
                MM(PS[b][:, h * 64:(h + 1) * 64], ckvn[:, tt * 128:(tt + 1) * 128], wukv[:, h * 128 + 64:h * 128 + 128], True, True,
                   ["wukv", ("ckvn", tt // 4)], [("ps", b)])
            vcopy(vA[:, tt, :, 0:64], PS[b][:, 0:256].rearrange("p (h d) -> p h d", h=4), [("ps", b)], [("mv", tt)])
        it = 0
        for h in range(4):
            for qt in range(4):
                it += 1
                bank = 2 + it % 2

                def terms(kc, h=h, qt=qt):
                    tl = [(KT[h][:, kc * 128:(kc + 1) * 128], QT[h][:, tqs(qt)],
                           [("mk", h, kc // 4), ("mk", h, kc // 4, "r"), ("mq", h, qt), ("mq", h, qt, "r")])]
                    if kc >= 4 * qt:
                        tl.append((identb[:], masks[:, kc - 4 * qt, :], ["identb", "masks"]))
                    return tl
                accmap = lambda qs, bank=bank: (bank, qs * 65)
                attn(terms, range(0, 4 * qt + 4), lambda kc, h=h: (vA[:, kc, h, :], ("mv", kc)), 65,
                     lambda kc, qs, qt=qt: kc <= 4 * qt + qs, accmap, scale=96 ** -0.5)
                attn_out(accmap, 64, 4 * qt, None, lambda tt, h=h: oacc[:, tt, h * 64:(h + 1) * 64], False)
        s.pop()
        s.pop()

    def merge_ln1(l, W, oT, xsrc):
        s.push()
        mT = s.sb([128, 8, S], BF16, "mT")
        macc = s.sb([128, S], F32, "macc")
        sg = [s.sb([128, 512], F32, f"sg{i}") for i in range(2)]
        wbr = [s.sb([128, 2, 128], BF16, f"wbr{i}") for i in range(2)]
        k = 0
        for dc in range(8):
            for n in range(4):
                k += 1
                i = next_wbuf()
                wb = wbuf[i]
                c0 = 2668 + n * 1024 + dc * 128
                DMA("gpsimd", wb[:, :, 0:128], W[:, c0:c0 + 128].rearrange("(c p) m -> p c m", p=128), w=[("wbuf", i, 0)])
                DMA("gpsimd", wbr[k % 2][:], D["w_branch"][l, n][:, dc * 128:(dc + 1) * 128].rearrange("(c p) m -> p c m", p=128), w=[("wbr", k % 2)])
                for tq in range(4):
                    bg = 4 + tq % 2
                    bu = 6 + tq % 2
                    for c in range(8):
                        MM(PS[bg][:], wb[:, c, 0:128], xT[:, c, tqs(tq)], c == 0, c == 7, [("wbuf", i, 0)] + xkeys(tq), [("ps", bg)])
                    for c in range(2):
                        MM(PS[bu][:], wbr[k % 2][:, c, :], oT[:, n, c, tqs(tq)], c == 0, c == 1, [("wbr", k % 2), ("oT", n, c)] + [("oT", n, tt) for tt in range(4 * tq, 4 * tq + 4)], [("ps", bu)])
                    A(lambda e, bg=bg, tq=tq: e.activation(out=sg[tq % 2][:], in_=PS[bg][:], func=AF.Sigmoid), [("ps", bg)], [("sg", tq % 2)])
                    if n == 0:
                        V(lambda e, bu=bu, tq=tq: e.tensor_tensor(out=macc[:, tqs(tq)], in0=sg[tq % 2][:], in1=PS[bu][:], op=ALU.mult),
                          [("sg", tq % 2), ("ps", bu)], [("macc", tq)])
                    else:
                        V(lambda e, bu=bu, tq=tq: e.tensor_tensor(out=sg[tq % 2][:], in0=sg[tq % 2][:], in1=PS[bu][:], op=ALU.mult),
                          [("sg", tq % 2), ("ps", bu)], [("sg", tq % 2)])
                        if n < 3:
                            G(lambda e, tq=tq: e.tensor_tensor(out=macc[:, tqs(tq)], in0=macc[:, tqs(tq)], in1=sg[tq % 2][:], op=ALU.add),
                              [("sg", tq % 2), ("macc", tq)], [("macc", tq)])
                        else:
                            G(lambda e, tq=tq, dc=dc: e.tensor_tensor(out=mT[:, dc, tqs(tq)], in0=macc[:, tqs(tq)], in1=sg[tq % 2][:], op=ALU.add),
                              [("sg", tq % 2), ("macc", tq)], [("mT", dc, tq)])
        s.barrier()
        wo = s.sb([128, 8, 1024], BF16, "wo")
        lng_, lnb_ = s.sb([128, DM], F32, "lng"), s.sb([128, DM], F32, "lnb")
        LN["g"], LN["b"] = lng_, lnb_
        load_ln(l, "ln1_g", "ln1_b")
        DMA("gpsimd", wo[:], D["w_out"][l].rearrange("(c p) m -> p c m", p=128), w=["wo"])

        def p0(tt, xt, key, sl):
            DMA("sync", xt[:], xsrc[tt * 128:(tt + 1) * 128, :], w=[key])

        def p1(tt, xt, key, sl):
            for hf in range(2):
                b = 4 + hf
                for dc in range(8):
                    MM(PS[b][:], mT[:, dc, tt * 128:(tt + 1) * 128], wo[:, dc, hf * 512:(hf + 1) * 512], dc == 0, dc == 7,
                       ["wo", ("mT", dc, tt // 4)], [("ps", b)])
                V(lambda e, b=b, hf=hf: e.scalar_tensor_tensor(out=xt[:, hf * 512:(hf + 1) * 512], in0=xt[:, hf * 512:(hf + 1) * 512],
                                                              scalar=DN_ALPHA, in1=PS[b][:], op0=ALU.mult, op1=ALU.add), [key, ("ps", b)], [key])
        ln_run([p0, p1], xs)
        s.pop()

    LN = {}

    def cross(l):
        s.push()
        AT["P"] = [s.sb([128, 512], BF16, f"P{i}") for i in range(3)]
        AT["rd"] = s.sb([128, 4, 4], F32, "rd")
        memT = s.sb([128, 8, 256], BF16, "memT")
        KxT = [s.sb([128, 256], BF16, f"KxT{h}") for h in range(4)]
        vxA = s.sb([128, 2, 4, 129], BF16, "vxA")
        QxT = [s.sb([128, S], BF16, f"QxT{h}") for h in range(4)]
        ox = s.sb([128, 16, 512], F32, "ox")
        mt = [s.sb([128, DM], F32, f"mt{i}") for i in range(2)]
        V(lambda e: e.memset(vxA[:, :, :, 128:129], 1.0), [], [("vxA", 0), ("vxA", 1)])
        for mc in range(2):
            DMA("sync", mt[mc][:], D["mem"][mc * 128:(mc + 1) * 128, :], w=[("mt", mc)])
            for half in range(2):
                b = 6 + half
                for j in range(4):
                    c = half * 4 + j
                    TR(PS[b][:, j * 128:(j + 1) * 128], mt[mc][:, c * 128:(c + 1) * 128], ident[:], [("mt", mc), "ident"], [("ps", b)])
                vcopy(memT[:, half * 4:(half + 1) * 4, mc * 128:(mc + 1) * 128], PS[b][:].rearrange("p (j q) -> p j q", j=4), [("ps", b)], [("memT", mc)])
        mk = [("memT", 0), ("memT", 1)]
        Wkv = D["x_wkv"][l]
        for h in range(4):
            i = next_wbuf()
            wb = wbuf[i]
            DMA("gpsimd", wb[:, :, 0:128], Wkv[:, h * 128:(h + 1) * 128].rearrange("(c p) m -> p c m", p=128), w=[("wbuf", i, 0)])
            b = gbank()
            for c in range(8):
                MM(PS[b][:, 0:256], wb[:, c, 0:128], memT[:, c, :], c == 0, c == 7, [("wbuf", i, 0)] + mk, [("ps", b)])
            vcopy(KxT[h][:], PS[b][:, 0:256], [("ps", b)], [("KxT", h)])
        i = next_wbuf()
        wb = wbuf[i]
        DMA("gpsimd", wb[:, :, 0:512], Wkv[:, 512:1024].rearrange("(c p) m -> p c m", p=128), w=[("wbuf", i, 0)])
        for mc in range(2):
            b = gbank()
            for c in range(8):
                MM(PS[b][:], memT[:, c, mc * 128:(mc + 1) * 128], wb[:, c, 0:512], c == 0, c == 7, [("wbuf", i, 0)] + mk, [("ps", b)])
            vcopy(vxA[:, mc, :, 0:128], PS[b][:].rearrange("p (h d) -> p h d", h=4), [("ps", b)], [("vxA", mc)])
        for h in range(4):
            def evq(ps, key, tq, h=h):
                vcopy(QxT[h][:, tqs(tq)], ps[:, :], [key], [("QxT", h, tq)])
            proj_fm([(0, D["x_wq"][l][:, h * 128:(h + 1) * 128])], 128, evq)
        for h in range(4):
            for qt in range(4):
                accmap = lambda qs: (2 + qs // 2, (qs % 2) * 129)
                attn(lambda kc, h=h, qt=qt: [(KxT[h][:, kc * 128:(kc + 1) * 128], QxT[h][:, tqs(qt)], [("KxT", h), ("QxT", h, qt)])],
                     range(2), lambda kc, h=h: (vxA[:, kc, h, :], ("vxA", kc)), 129, lambda kc, qs: True, accmap, scale=128 ** -0.5)
                attn_out(accmap, 128, 4 * qt, None, lambda tt, h=h: ox[:, tt, h * 128:(h + 1) * 128], False)
        s.barrier()
        oxT = s.sb([128, 4, S], BF16, "oxT")
        to_fm(ox, 4, oxT, "oxT")
        wo = s.sb([128, 4, 1024], BF16, "xwo")
        lng_, lnb_ = s.sb([128, DM], F32, "lng"), s.sb([128, DM], F32, "lnb")
        LN["g"], LN["b"] = lng_, lnb_
        load_ln(l, "ln2_g", "ln2_b")
        DMA("gpsimd", wo[:], D["x_wo"][l].rearrange("(c p) m -> p c m", p=128), w=["xwo"])

        def p0(tt, xt, key, sl):
            DMA("sync", xt[:], xs[tt * 128:(tt + 1) * 128, :], w=[key])

        def p1(tt, xt, key, sl):
            for hf in range(2):
                b = 4 + hf
                for c in range(4):
                    MM(PS[b][:], oxT[:, c, tt * 128:(tt + 1) * 128], wo[:, c, hf * 512:(hf + 1) * 512], c == 0, c == 3,
                       ["xwo", ("oxT", tt)], [("ps", b)])
                V(lambda e, b=b, hf=hf: e.scalar_tensor_tensor(out=xt[:, hf * 512:(hf + 1) * 512], in0=xt[:, hf * 512:(hf + 1) * 512],
                                                              scalar=DN_ALPHA, in1=PS[b][:], op0=ALU.mult, op1=ALU.add), [key, ("ps", b)], [key])
        ln_run([p0, p1], xs)
        s.pop()

    def moe(l, dst, last):
        s.push()
        cwall = s.sb([128, 16, 32], F32, "cwall")
        ohall = s.sb([128, 16, 32], BF16, "ohall")
        idxs = nc.alloc_sbuf_tensor_at(f"idxs_{l}", [128, 16, 2], I32, offset=(s.sb_off + 63) // 64 * 64)
        s.sb_off = (s.sb_off + 63) // 64 * 64 + 128
        wts = s.sb([128, 16, 2], F32, "wts")
        s.push()
        wr = s.sb([128, 8, 36], F32, "wr")
        brow = s.sb([1, 36], F32, "brow")
        ones1 = s.sb([1, 128], F32, "ones1")
        onesb = s.sb([128, 128], BF16, "onesb")
        ltri = s.sb([128, 128], BF16, "ltri")
        ebase = s.sb([128, 32], F32, "ebase")
        V(lambda e: e.memset(ones1[:], 1.0), [], ["ones1"])
        V(lambda e: e.memset(onesb[:], 1.0), [], ["onesb"])
        DMA("gpsimd", ltri[:], D["c_ltri"], w=["ltri"])
        DMA("sync", ebase[:], D["c_ebase"], w=["ebase"])
        DMA("sync", wr[:, :, 0:4], D["moe_rg_w"][l].rearrange("(c p) m -> p c m", p=128), w=["wr0"])
        DMA("sync", wr[:, :, 4:36], D["moe_re_w"][l].rearrange("(c p) m -> p c m", p=128), w=["wr1"])
        DMA("sync", brow[:, 0:4], D["moe_rg_b"][l].rearrange("(o m) -> o m", o=1), w=["br0"])
        DMA("sync", brow[:, 4:36], D["moe_re_b"][l].rearrange("(o m) -> o m", o=1), w=["br1"])
        RB = (4, 5, 0, 1)
        RB2 = (2, 3, 4, 5)
        xt = [s.sb([128, DM], F32, f"rx{i}") for i in range(4)]
        xTfs = [s.sb([128, 8, 128], F32, f"xTf{i}") for i in range(4)]
        SC = [dict(lg=s.sb([128, 36], F32, "lg"), sm=s.sb([128, 16], F32, "sm"), gm=s.sb([128, 4], F32, "gm"), es=s.sb([128, 8], F32, "es"),
                   t8=s.sb([128, 8], F32, "t8"), ta=s.sb([128, 8], F32, "ta"), tb=s.sb([128, 8], F32, "tb"), rk=s.sb([128, 32], F32, "rk"),
                   vl=s.sb([128, 32], F32, "vl"), val=s.sb([128, 32], F32, "val"), fidx=s.sb([128, 4], F32, "fidx")) for _ in range(4)]

        def router_tile(tt, j):
            x_, xTf, bnk = xt[j], xTfs[j], RB[j]
            lg, sm, gm, es, t8, ta, tb = (SC[j][n_] for n_ in ("lg", "sm", "gm", "es", "t8", "ta", "tb"))
            key = ("rx", j)
            K_ = lambda n_: (n_, j)
            DMA("sync", x_[:], xs[tt * 128:(tt + 1) * 128, :], w=[key])
            yield
            for half in range(2):
                b = 6 + half
                for jj in range(4):
                    c = half * 4 + jj
                    TR(PS[b][:, jj * 128:(jj + 1) * 128], x_[:, c * 128:(c + 1) * 128], ident[:], [key, "ident"], [("ps", b)])
                vcopy(xTf[:, half * 4:(half + 1) * 4, :], PS[b][:].rearrange("p (j q) -> p j q", j=4), [("ps", b)], [("xTf", j, half)])
            yield
            for c in range(8):
                MM(PS[bnk][:, 0:36], xTf[:, c, :], wr[:, c, :], c == 0, False, [("xTf", j, c // 4), "wr0", "wr1"], [("ps", bnk)])
            MM(PS[bnk][:, 0:36], ones1[:, :], brow[:, :], False, True, ["ones1", "br0", "br1"], [("ps", bnk)])
            vcopy(lg[:], PS[bnk][:, 0:36], [("ps", bnk)], [K_("lg")])
            yield
            V(lambda e: e.tensor_reduce(out=sm[:, 0:1], in_=lg[:, 0:4], axis=AX.X, op=ALU.max), [K_("lg")], [K_("sm0")])
            yield
            V(lambda e: e.tensor_scalar(out=sm[:, 1:2], in0=sm[:, 0:1], scalar1=-1.0, scalar2=None, op0=ALU.mult), [K_("sm0")], [K_("sm1")])
            yield
            A(lambda e: e.activation(out=gm[:], in_=lg[:, 0:4], func=AF.Exp, bias=sm[:, 1:2]), [K_("lg"), K_("sm1")], [K_("gm")])
            yield
            V(lambda e: e.tensor_reduce(out=sm[:, 2:3], in_=gm[:], axis=AX.X, op=ALU.add), [K_("gm")], [K_("sm2")])
            yield
            V(lambda e: e.reciprocal(out=sm[:, 3:4], in_=sm[:, 2:3]), [K_("sm2")], [K_("sm3")])
            yield
            V(lambda e: e.tensor_scalar(out=gm[:], in0=lg[:, 0:4], scalar1=sm[:, 0:1], scalar2=None, op0=ALU.is_ge), [K_("lg"), K_("sm0"), K_("gm")], [K_("gm")])
            yield
            V(lambda e: e.tensor_scalar(out=es[:], in0=lg[:, 4:12], scalar1=gm[:, 0:1], scalar2=None, op0=ALU.mult), [K_("lg"), K_("gm")], [K_("es")])
            yield
            for g in range(1, 4):
                V(lambda e, g=g: e.scalar_tensor_tensor(out=es[:], in0=lg[:, 4 + 8 * g:12 + 8 * g], scalar=gm[:, g:g + 1], in1=es[:], op0=ALU.mult, op1=ALU.add),
                  [K_("lg"), K_("gm"), K_("es")], [K_("es")])
                yield
            V(lambda e: e.max(out=t8[:], in_=es[:]), [K_("es")], [K_("t8")])
            yield
            V(lambda e: e.tensor_tensor(out=sm[:, 4:5], in0=t8[:, 0:1], in1=t8[:, 1:2], op=ALU.subtract), [K_("t8")], [K_("sm4")])
            yield
            A(lambda e: e.activation(out=sm[:, 5:6], in_=sm[:, 4:5], func=AF.Sigmoid), [K_("sm4")], [K_("sm5")])
            yield
            V(lambda e: e.tensor_tensor(out=sm[:, 6:7], in0=sm[:, 5:6], in1=sm[:, 3:4], op=ALU.mult), [K_("sm5"), K_("sm3")], [K_("sm6")])
            yield
            V(lambda e: e.tensor_tensor(out=sm[:, 7:8], in0=sm[:, 3:4], in1=sm[:, 6:7], op=ALU.subtract), [K_("sm6"), K_("sm3")], [K_("sm7")])
            yield
            for g in range(4):
                eg = lg[:, 4 + 8 * g:12 + 8 * g]
                V(lambda e, eg=eg: e.tensor_scalar(out=ta[:], in0=eg, scalar1=t8[:, 0:1], scalar2=sm[:, 6:7], op0=ALU.is_equal, op1=ALU.mult),
                  [K_("lg"), K_("t8"), K_("sm6"), K_("ta")], [K_("ta")])
                yield
                V(lambda e, eg=eg: e.tensor_scalar(out=tb[:], in0=eg, scalar1=t8[:, 1:2], scalar2=sm[:, 7:8], op0=ALU.is_equal, op1=ALU.mult),
                  [K_("lg"), K_("t8"), K_("sm7"), K_("tb")], [K_("tb")])
                yield
                V(lambda e: e.tensor_tensor(out=ta[:], in0=ta[:], in1=tb[:], op=ALU.add), [K_("ta"), K_("tb")], [K_("ta")])
                yield
                V(lambda e, g=g: e.tensor_scalar(out=cwall[:, tt, 8 * g:8 * g + 8], in0=ta[:], scalar1=gm[:, g:g + 1], scalar2=None, op0=ALU.mult),
                  [K_("ta"), K_("gm")], [("cw", tt, g)])
                yield
            V(lambda e: e.tensor_scalar(out=ohall[:, tt, :], in0=cwall[:, tt, :], scalar1=0.0, scalar2=None, op0=ALU.is_gt),
              [("cw", tt, g) for g in range(4)], [("oh", tt)])
            yield

        for base in range(0, 16, 4):
            interleave([router_tile(base + j, j) for j in range(4)])

        def rank_tile(tt, j):
            x_, bnk = xt[j], RB2[j]
            rk, vl, val, fidx = (SC[j][n_] for n_ in ("rk", "vl", "val", "fidx"))
            key = ("rx", j)
            K_ = lambda n_: (n_, j)
            DMA("sync", x_[:], xs[tt * 128:(tt + 1) * 128, :], w=[key])
            for t2 in range(tt):
                MM(PS[bnk][:, 0:32], onesb[:, :], ohall[:, t2, :], t2 == 0, False, ["onesb", ("oh", t2)], [("ps", bnk)])
            MM(PS[bnk][:, 0:32], ltri[:, :], ohall[:, tt, :], tt == 0, True, ["ltri", ("oh", tt)], [("ps", bnk)])
            vcopy(rk[:], PS[bnk][:, 0:32], [("ps", bnk)], [K_("rk")])
            yield
            V(lambda e: e.tensor_scalar(out=vl[:], in0=rk[:], scalar1=float(CAP) - 0.5, scalar2=None, op0=ALU.is_lt), [K_("rk")], [K_("vl")])
            yield
            V(lambda e: e.tensor_tensor(out=vl[:], in0=vl[:], in1=ohall[:, tt, :], op=ALU.mult), [K_("vl"), ("oh", tt)], [K_("vl")])
            yield
            V(lambda e: e.tensor_tensor(out=cwall[:, tt, :], in0=cwall[:, tt, :], in1=vl[:], op=ALU.mult), [K_("vl")] + [("cw", tt, g) for g in range(4)], [("cwv", tt)])
            yield
            V(lambda e: e.tensor_tensor(out=val[:], in0=rk[:], in1=ebase[:], op=ALU.add), [K_("rk"), "ebase"], [K_("val")])
            yield
            V(lambda e: e.tensor_tensor(out=val[:], in0=val[:], in1=vl[:], op=ALU.mult), [K_("val"), K_("vl")], [K_("val")])
            yield
            V(lambda e: e.tensor_reduce(out=fidx[:, 1:2], in_=val[:], axis=AX.X, op=ALU.max), [K_("val")], [K_("f1")])
            yield
            V(lambda e: e.tensor_reduce(out=fidx[:, 0:1], in_=val[:], axis=AX.X, op=ALU.add), [K_("val")], [K_("f0")])
            yield
            V(lambda e: e.tensor_tensor(out=fidx[:, 0:1], in0=fidx[:, 0:1], in1=fidx[:, 1:2], op=ALU.subtract), [K_("f0"), K_("f1")], [K_("f0")])
            yield
            V(lambda e: e.tensor_scalar(out=rk[:], in0=val[:], scalar1=fidx[:, 1:2], scalar2=None, op0=ALU.is_equal), [K_("val"), K_("f1"), K_("rk")], [K_("rk")])
            yield
            V(lambda e: e.tensor_tensor(out=rk[:], in0=rk[:], in1=cwall[:, tt, :], op=ALU.mult), [K_("rk"), ("cwv", tt)], [K_("rk")])
            yield
            V(lambda e: e.tensor_reduce(out=wts[:, tt, 1:2], in_=rk[:], axis=AX.X, op=ALU.add), [K_("rk")], [("wts", tt, 1)])
            yield
            V(lambda e: e.tensor_reduce(out=wts[:, tt, 0:1], in_=cwall[:, tt, :], axis=AX.X, op=ALU.add), [("cwv", tt)], [("wts", tt, 0)])
            yield
            V(lambda e: e.tensor_tensor(out=wts[:, tt, 0:1], in0=wts[:, tt, 0:1], in1=wts[:, tt, 1:2], op=ALU.subtract),
              [("wts", tt, 0), ("wts", tt, 1)], [("wts", tt, 0)])
            yield
            V(lambda e: e.tensor_scalar(out=fidx[:, 2:4], in0=fidx[:, 0:2], scalar1=0.5, scalar2=1.0e6, op0=ALU.is_lt, op1=ALU.mult),
              [K_("f0"), K_("f1")], [K_("f2")])
            yield
            V(lambda e: e.scalar_tensor_tensor(out=fidx[:, 2:4], in0=fidx[:, 0:2], scalar=-1.0, in1=fidx[:, 2:4], op0=ALU.add, op1=ALU.add),
              [K_("f0"), K_("f1"), K_("f2")], [K_("f2")])
            yield
            V(lambda e: e.tensor_copy(out=idxs[:, tt, :], in_=fidx[:, 2:4]), [K_("f2")], [("idx", tt)])
            yield
            for k_ in range(2):
                s.op("gpsimd", lambda e, k_=k_: e.indirect_dma_start(
                    out=xg[:, :], out_offset=bass.IndirectOffsetOnAxis(ap=idxs[:, tt, k_:k_ + 1], axis=0), in_=x_[:, :], in_offset=None,
                    bounds_check=bc_reg, oob_is_err=False), [key, ("idx", tt)], [("xgs", tt, k_)], dma=True)
            yield

        for base in range(0, 16, 4):
            interleave([rank_tile(base + j, j) for j in range(4)])
        s.pop()
        s.push()
        NJ = CAP // 128
        wgu = [s.sb([128, 8, 1024], BF16, f"wgu{i}") for i in range(2)]
        wd = [s.sb([128, 4, 1024], BF16, f"wd{i}") for i in range(2)]
        xgt = [s.sb([128, NJ, DM], F32, f"xgt{i}") for i in range(2)]
        xgT = [s.sb([128, 8, CAP], BF16, f"xgT{i}") for i in range(2)]
        actT = s.sb([128, 4, CAP], BF16, "actT")
        sl = [s.sb([128, CAP], F32, f"sl{i}") for i in range(2)]
        yrow = [s.sb([128, DM], F32, f"yrow{i}") for i in range(2)]
        k = 0
        for ee in range(32):
            wi = ee % 2
            DMA("gpsimd", wgu[wi][:], D["moe_w_gu"][l, ee].rearrange("(c p) m -> p c m", p=128), w=[("wgu", wi)])
            DMA("gpsimd", wd[wi][:], D["moe_w_down"][l, ee].rearrange("(c p) m -> p c m", p=128), w=[("wd", wi)])
            DMA("sync", xgt[wi][:], xg[ee * CAP:(ee + 1) * CAP, :].rearrange("(j p) d -> p j d", p=128), w=[("xgt", wi)])
            for j in range(NJ):
                for half in range(2):
                    b = 6 + half
                    for jj in range(4):
                        c = half * 4 + jj
                        TR(PS[b][:, jj * 128:(jj + 1) * 128], xgt[wi][:, j, c * 128:(c + 1) * 128], ident[:], [("xgt", wi), "ident"], [("ps", b)])
                    vcopy(xgT[wi][:, half * 4:(half + 1) * 4, j * 128:(j + 1) * 128], PS[b][:].rearrange("p (j q) -> p j q", j=4),
                          [("ps", b)], [("xgT", wi, j, half)])
            xk = [("xgT", wi, j, half) for j in range(NJ) for half in range(2)]
            for fc in range(4):
                k += 1
                bg = 0 + k % 2
                bu = 2 + k % 2
                for c in range(8):
                    MM(PS[bg][:, 0:CAP], wgu[wi][:, c, fc * 128:(fc + 1) * 128], xgT[wi][:, c, :], c == 0, c == 7, [("wgu", wi)] + xk, [("ps", bg)])
                for c in range(8):
                    MM(PS[bu][:, 0:CAP], wgu[wi][:, c, 512 + fc * 128:512 + (fc + 1) * 128], xgT[wi][:, c, :], c == 0, c == 7, [("wgu", wi)] + xk, [("ps", bu)])
                A(lambda e, bg=bg, k=k: e.activation(out=sl[k % 2][:], in_=PS[bg][:, 0:CAP], func=AF.Silu), [("ps", bg)], [("sl", k % 2)])
                V(lambda e, bu=bu, k=k, fc=fc: e.tensor_tensor(out=actT[:, fc, :], in0=sl[k % 2][:], in1=PS[bu][:, 0:CAP], op=ALU.mult),
                  [("sl", k % 2), ("ps", bu)], [("actT", fc)])
            for j in range(NJ):
                yr = yrow[(ee * NJ + j) % 2]
                yk = ("yrow", (ee * NJ + j) % 2)
                for hf in range(2):
                    b = 4 + hf
                    for fc in range(4):
                        MM(PS[b][:], actT[:, fc, j * 128:(j + 1) * 128], wd[wi][:, fc, hf * 512:(hf + 1) * 512], fc == 0, fc == 3,
                           [("wd", wi), ("actT", fc)], [("ps", b)])
                    if hf == 0:
                        vcopy(yr[:, 0:512], PS[b][:], [("ps", b)], [yk])
                    else:
                        acopy(yr[:, 512:1024], PS[b][:], [("ps", b)], [yk])
                DMA("sync", yg[ee * CAP + j * 128:ee * CAP + (j + 1) * 128, :], yr[:], r=[yk], w=[("ygs", ee, j)])
        s.pop()
        gA = [s.sb([128, DM], F32, f"gA{i}") for i in range(NBUF)]
        gB = [s.sb([128, DM], F32, f"gB{i}") for i in range(NBUF)]
        lng_, lnb_ = s.sb([128, DM], F32, "lng"), s.sb([128, DM], F32, "lnb")
        LN["g"], LN["b"] = lng_, lnb_
        load_ln(l, "ln3_g", "ln3_b")
        for i in range(NBUF):
            V(lambda e, i=i: e.memset(gA[i][:], 0.0), [], [("gA", i)])
            V(lambda e, i=i: e.memset(gB[i][:], 0.0), [], [("gB", i)])

        def p0(tt, xt_, key, sl):
            DMA("sync", xt_[:], xs[tt * 128:(tt + 1) * 128, :], w=[key])
            for k_, gt, gk in ((0, gA[sl], ("gA", sl)), (1, gB[sl], ("gB", sl))):
                s.op("gpsimd", lambda e, tt=tt, k_=k_, gt=gt: e.indirect_dma_start(
                    out=gt[:, :], out_offset=None, in_=yg[:, :], in_offset=bass.IndirectOffsetOnAxis(ap=idxs[:, tt, k_:k_ + 1], axis=0),
                    bounds_check=bc_reg, oob_is_err=False), [("idx", tt)], [gk], dma=True)

        def p1(tt, xt_, key, sl):
            ga, gb = gA[sl], gB[sl]
            V(lambda e: e.tensor_scalar(out=ga[:], in0=ga[:], scalar1=wts[:, tt, 0:1], scalar2=None, op0=ALU.mult),
              [("gA", sl), ("wts", tt, 0)], [("gA", sl)])
            V(lambda e: e.scalar_tensor_tensor(out=ga[:], in0=gb[:], scalar=wts[:, tt, 1:2], in1=ga[:], op0=ALU.mult, op1=ALU.add),
              [("gA", sl), ("gB", sl), ("wts", tt, 1)], [("gA", sl)])
            V(lambda e: e.scalar_tensor_tensor(out=xt_[:], in0=xt_[:], scalar=DN_ALPHA, in1=ga[:], op0=ALU.mult, op1=ALU.add),
              [key, ("gA", sl)], [key])
        ln_run([p0, p1], dst, do_T=not last)
        s.pop()

    def mixer(l, xsrc):
        W = D["w_in"][l]
        s.push()
        oT = s.sb([128, 4, 2, S], BF16, "oT")
        s.push()
        oacc = s.sb([128, 16, 256], F32, "oacc")
        AT["P"] = [s.sb([128, 512], BF16, f"P{i}") for i in range(3)]
        AT["rd"] = s.sb([128, 4, 4], F32, "rd")
        nsa(l, W, oacc)
        to_fm(oacc, 2, oT[:, 0], ("oT", 0))
        sbranch(l, W, oacc)
        to_fm(oacc, 2, oT[:, 1], ("oT", 1))
        rglru(l, W, oT)
        mla(l, W, oacc)
        to_fm(oacc, 2, oT[:, 3], ("oT", 3))
        s.pop()
        if l == 0:
            dump_sb("oT", oT[:], [128, 4, 2, S])
        merge_ln1(l, W, oT, xsrc)
        s.pop()

    s.push()
    zt = s.sb([128, DM], F32, "zt")
    V(lambda e: e.memset(zt[:], 0.0), [], ["zt"])
    for r in range(NSL // 128):
        DMA("sync", xg[r * 128:(r + 1) * 128, :], zt[:], r=["zt"], w=[("xgz", r)])
    s.pop()
    load_xT(D["x"])
    for l in range(NL):
        s.push()
        wbuf = [s.sb([128, 8, 512], BF16, f"wbuf{i}") for i in range(3)]
        masks = s.sb([128, 12, 512], BF16, "masks")
        DMA("gpsimd", masks[:], D["c_masks"], w=["masks"])
        if "mix" in stages:
            mixer(l, D["x"] if l == 0 else xs)
        if "cross" in stages:
            cross(l)
        s.pop()
        if "moe" in stages:
            last = l == NL - 1
            moe(l, out_d if last else xs, last)
    if "moe" not in stages:
        s.barrier()
        s.push()
        tmp = s.sb([128, DM], F32, "fin")
        for tt in range(16):
            DMA("sync", tmp[:], xs[tt * 128:(tt + 1) * 128, :], w=["fin"])
            DMA("sync", out_d[tt * 128:(tt + 1) * 128, :], tmp[:], r=["fin"])
        s.pop()
    s.barrier()
    s.emit()
    return nc, dumps


_CACHE = {}


def kernel(**inputs):
    NL = 4
    if "nc" not in _CACHE:
        _CACHE["nc"] = build(NL)[0]
    nc = _CACHE["nc"]
    HC = host_consts()
    shared = {n: np.ascontiguousarray(np.asarray(inputs[n], dtype=np.float32)) for n in WNAMES}
    for n, a in HC.items():
        shared["c_" + n] = a
    x = np.asarray(inputs["x"], dtype=np.float32)
    mem = np.asarray(inputs["mem"], dtype=np.float32)
    in_maps = []
    for b in range(8):
        m = dict(shared)
        m["x"] = np.ascontiguousarray(x[b])
        m["mem"] = np.ascontiguousarray(mem[b])
        in_maps.append(m)
    res = run_bass_kernel_spmd(nc, in_maps, core_ids=list(range(8)))
    return np.stack([np.asarray(r["out"], dtype=np.float32) for r in res.results], axis=0)
```

```python
import numpy as np
import contextlib
import concourse.bass as bass
import concourse.mybir as mybir
from concourse.bass_utils import run_bass_kernel_spmd

F32 = mybir.dt.float32
BF16 = mybir.dt.bfloat16
AF = mybir.ActivationFunctionType
ALU = mybir.AluOpType
AX = mybir.AxisListType
ENGS = ("tensor", "vector", "scalar", "gpsimd", "sync")
DSIZE = {F32: 4, BF16: 2}
NEG = -30000.0
S = 2048
DM = 1024
DN_ALPHA = (2.0 * 4) ** 0.25
CAP = 384
NSL = 32 * CAP
I32 = mybir.dt.int32


class Sched:
    NSLOT = 4

    def __init__(self, nc):
        self.nc = nc
        self.ops = {e: [] for e in ENGS}
        self.ncomp = {e: 0 for e in ENGS}
        self.ndma = {e: 0 for e in ENGS}
        self.lastw = {}
        self.readers = {}
        self.synced = {e: {} for e in ENGS}
        self.sb_off = 16640
        self.sb_stack = []
        self.uid = 0

    def sb(self, shape, dtype, name=None):
        self.uid += 1
        name = f"{name or 't'}_{self.uid}"
        nbytes = int(np.prod(shape[1:])) * DSIZE[dtype]
        off = (self.sb_off + 63) // 64 * 64
        assert off + nbytes <= 228000, f"SBUF overflow {name} {off + nbytes}"
        t = self.nc.alloc_sbuf_tensor_at(name, list(shape), dtype, offset=off)
        self.sb_off = off + nbytes
        return t

    def push(self):
        self.sb_stack.append(self.sb_off)

    def pop(self):
        self.barrier()
        self.sb_off = self.sb_stack.pop()

    def _need(self, E, dep, waits):
        key, val = dep
        if key == ("c", "tensor") and E == "tensor":
            return
        if self.synced[E].get(key, 0) >= val:
            return
        if waits.get(key, 0) < val:
            waits[key] = val

    def op(self, E, fn, reads=(), writes=(), dma=False):
        waits = {}
        for r in reads:
            lw = self.lastw.get(r)
            if lw is not None:
                self._need(E, lw, waits)
        for w in writes:
            lw = self.lastw.get(w)
            if lw is not None:
                self._need(E, lw, waits)
            for k, v in self.readers.get(w, {}).items():
                self._need(E, (k, v), waits)
        if dma:
            k = self.ndma[E]
            self.ndma[E] += 1
            key = ("d", E, k % self.NSLOT)
            val = 16 * (k // self.NSLOT + 1)
            if val > 16:
                self._need(E, (key, val - 16), waits)
        else:
            self.ncomp[E] += 1
            key = ("c", E)
            val = self.ncomp[E]
        for k_, v_ in waits.items():
            self.synced[E][k_] = v_
        me = (key, val)
        self.ops[E].append((fn, waits, me))
        for r in reads:
            d = self.readers.setdefault(r, {})
            if d.get(key, 0) < val:
                d[key] = val
        for w in writes:
            self.lastw[w] = me
            self.readers[w] = {}
        return me

    def raw(self, E, fn):
        self.ops[E].append((fn, {}, None))

    def barrier(self):
        state = {}
        for e in ENGS:
            if self.ncomp[e]:
                state[("c", e)] = self.ncomp[e]
            for sl in range(self.NSLOT):
                k = self.ndma[e]
                cnt = (k - sl + self.NSLOT - 1) // self.NSLOT if k > sl else 0
                if cnt:
                    state[("d", e, sl)] = 16 * cnt
        for e in ENGS:
            waits = {}
            for k_, v_ in state.items():
                if self.synced[e].get(k_, 0) < v_:
                    waits[k_] = v_
                    self.synced[e][k_] = v_
            if waits:
                self.ops[e].append((None, waits, None))
        self.lastw = {}
        self.readers = {}

    def emit(self):
        nc = self.nc
        sems = {}
        with contextlib.ExitStack() as st:
            for e in ENGS:
                sems[("c", e)] = st.enter_context(nc.semaphore(f"c_{e}"))
                for sl in range(self.NSLOT):
                    sems[("d", e, sl)] = st.enter_context(nc.semaphore(f"d_{e}_{sl}"))
            block = st.enter_context(nc.Block())

            def run(eng, name):
                for fn, waits, me in self.ops[name]:
                    for k_, v_ in waits.items():
                        eng.wait_ge(sems[k_], v_)
                    if fn is not None and me is None:
                        fn(eng)
                    elif fn is not None:
                        fn(eng).then_inc(sems[me[0]], 16 if me[0][0] == "d" else 1)

            @block.tensor
            def _(e):
                run(e, "tensor")

            @block.vector
            def _(e):
                run(e, "vector")

            @block.scalar
            def _(e):
                run(e, "scalar")

            @block.gpsimd
            def _(e):
                run(e, "gpsimd")

            @block.sync
            def _(e):
                run(e, "sync")


def host_consts():
    c = {}
    c["ident"] = np.eye(128, dtype=np.float32)
    t = np.arange(S)
    slopes = 2.0 ** (-2.0 * (np.arange(4) + 1))
    qaug = np.zeros((4, 4, S), np.float32)
    for h in range(4):
        qaug[h, 0] = -slopes[h] * 128 * (t // 128)
        qaug[h, 1] = -slopes[h] * (t % 128)
        qaug[h, 2] = slopes[h]
        qaug[h, 3] = slopes[h]
    c["qaug"] = qaug
    c["kaug"] = np.stack([np.ones(S), np.ones(S), 128.0 * (t // 128), (t % 128)]).astype(np.float32)
    be = np.arange(128) * 16 + 31
    c["kcaug"] = np.stack([np.ones(128), np.ones(128), 128.0 * (be // 128), (be % 128)]).astype(np.float32)
    k = np.arange(128)[:, None]
    q = np.arange(512)[None, :]
    masks = np.zeros((128, 12, 512), np.float32)
    for j in range(4):
        masks[:, j] = np.where(q >= 128 * j + k, 0.0, NEG)
        masks[:, 4 + j] = np.where(128 * j + k < q, 0.0, NEG)
        masks[:, 8 + j] = np.where(q < 128 * j + k, 0.0, NEG)
    c["masks"] = masks
    cc = np.arange(128)[:, None]
    c["cmask"] = np.where((t[None, :] >= 16 * cc + 31) & (cc < 127), 0.0, NEG).astype(np.float32)
    tt = t[:, None]
    j = np.arange(32)[None, :]
    forced = (j == 0) | (j == tt // 64)
    valid = j * 64 <= tt
    vm = (valid & ~forced).astype(np.float32)
    am = np.where(forced, 1e4, np.where(valid, 0.0, -1.0)).astype(np.float32)
    c["impvm"] = vm.reshape(16, 128, 32).transpose(1, 0, 2).copy()
    c["impam"] = am.reshape(16, 128, 32).transpose(1, 0, 2).copy()
    E = np.zeros((32, 16, 128), np.float32)
    for kc in range(16):
        for kk in range(128):
            E[2 * kc + kk // 64, kc, kk] = 1.0
    c["selE"] = E
    c0 = np.arange(128)[:, None] * 16
    j0 = np.arange(32)[None, :] * 64
    cover = np.clip(np.minimum(c0 + 32, j0 + 64) - np.maximum(c0, j0), 0, None) / 32.0
    cover[127] = 0.0
    c["cover"] = cover.astype(np.float32)
    inv = (10000.0 ** (-np.arange(0, 32, 2, dtype=np.float32) / 32)).astype(np.float32)
    ang = t.astype(np.float32)[:, None] * inv[None, :]
    cs, sn = np.cos(ang).astype(np.float32).T, np.sin(ang).astype(np.float32).T
    rc = np.zeros((96, S), np.float32)
    rs = np.zeros((96, S), np.float32)
    rc[64:80] = cs
    rc[80:96] = cs
    rs[64:80] = -sn
    rs[80:96] = sn
    c["ropec"] = rc
    c["ropes"] = rs
    jj = np.arange(128)[:, None]
    ss = np.arange(128)[None, :]
    c["negU"] = np.where(jj >= ss, -1.0, 0.0).astype(np.float32)
    sr = np.zeros((32, 32, 128), np.float32)
    for e_ in range(32):
        sr[e_, e_, :] = 1.0
    c["selrow"] = sr
    c["ltri"] = (jj < ss).astype(np.float32)
    c["ebase"] = np.tile((np.arange(32) * CAP + 1).astype(np.float32)[None, :], (128, 1))
    return c


WNAMES = ["w_in", "nsa_cmp_pos", "nsa_cmp_w1", "nsa_cmp_w2", "rnn_conv_w", "rnn_conv_b", "rnn_ga_w", "rnn_ga_b",
          "rnn_gx_w", "rnn_gx_b", "rnn_lambda", "mla_q_norm", "mla_kv_norm", "mla_w_uq", "mla_w_ukv", "w_branch",
          "w_out", "ln1_g", "ln1_b", "x_wq", "x_wkv", "x_wo", "ln2_g", "ln2_b", "moe_rg_w", "moe_rg_b", "moe_re_w",
          "moe_re_b", "moe_w_gu", "moe_w_down", "ln3_g", "ln3_b"]
WSHAPES = {"w_in": (1024, 6764), "nsa_cmp_pos": (2, 32, 64), "nsa_cmp_w1": (2, 2048, 256), "nsa_cmp_w2": (2, 256, 64),
           "rnn_conv_w": (4, 256), "rnn_conv_b": (256,), "rnn_ga_w": (4, 64, 64), "rnn_ga_b": (256,),
           "rnn_gx_w": (4, 64, 64), "rnn_gx_b": (256,), "rnn_lambda": (256,), "mla_q_norm": (192,),
           "mla_kv_norm": (128,), "mla_w_uq": (192, 384), "mla_w_ukv": (128, 512), "w_branch": (4, 256, 1024),
           "w_out": (1024, 1024), "ln1_g": (1024,), "ln1_b": (1024,), "x_wq": (1024, 512), "x_wkv": (1024, 1024),
           "x_wo": (512, 1024), "ln2_g": (1024,), "ln2_b": (1024,), "moe_rg_w": (1024, 4), "moe_rg_b": (4,),
           "moe_re_w": (1024, 32), "moe_re_b": (32,), "moe_w_gu": (32, 1024, 1024), "moe_w_down": (32, 512, 1024),
           "ln3_g": (1024,), "ln3_b": (1024,)}


def build(NL=4, stages=("mix", "cross", "moe"), dump=None):
    nc = bass.Bass("TRN2", target_bir_lowering=False)
    s = Sched(nc)
    D = {}

    def din(name, shape):
        D[name] = nc.dram_tensor(name, list(shape), F32, kind="ExternalInput").ap()
        return D[name]

    din("x", (S, DM))
    din("mem", (256, DM))
    for n in WNAMES:
        din(n, (NL,) + WSHAPES[n])
    HC = host_consts()
    for n, a in HC.items():
        din("c_" + n, a.shape)
    out_d = nc.dram_tensor("out", [S, DM], F32, kind="ExternalOutput").ap()
    xs = nc.dram_tensor("xs_scr", [S, DM], F32, kind="Internal").ap()
    xg = nc.dram_tensor("xg_scr", [NSL, DM], F32, kind="Internal").ap()
    yg = nc.dram_tensor("yg_scr", [NSL, DM], F32, kind="Internal").ap()
    dumps = {}

    PS = [nc.alloc_psum_tensor(f"ps{i}", [128, 512], F32) for i in range(8)]
    bc_reg = nc.gpsimd.alloc_register("bc_reg")
    s.raw("gpsimd", lambda e: e.reg_mov(bc_reg, NSL - 1))

    def V(fn, r=(), w=()):
        return s.op("vector", fn, r, w)

    def A(fn, r=(), w=()):
        return s.op("scalar", fn, r, w)

    def G(fn, r=(), w=()):
        return s.op("gpsimd", fn, r, w)

    def MM(out, lhsT, rhs, start, stop, r, w):
        return s.op("tensor", lambda e: e.matmul(out, lhsT=lhsT, rhs=rhs, start=start, stop=stop), r, w)

    def MMs(out, lhsT, rhs, start, stop, r, w):
        return s.op("tensor", lambda e: e.matmul(out, lhsT=lhsT, rhs=rhs, start=start, stop=stop, skip_group_check=True), r, w)

    def TR(out, in_, idn, r, w):
        return s.op("tensor", lambda e: e.transpose(out, in_, idn), r, w)

    def DMA(q, out, in_, r=(), w=()):
        return s.op(q, lambda e: e.dma_start(out=out, in_=in_), r, w, dma=True)

    def vcopy(out, in_, r, w):
        return V(lambda e: e.tensor_copy(out=out, in_=in_), r, w)

    def acopy(out, in_, r, w):
        return A(lambda e: e.activation(out=out, in_=in_, func=AF.Copy), r, w)

    def dump_sb(name, ap, shape):
        if dump is None or name not in dump:
            return
        d = nc.dram_tensor("dbg_" + name, list(shape), ap.dtype if hasattr(ap, "dtype") else F32, kind="ExternalOutput").ap()
        dumps[name] = d
        s.barrier()
        DMA("sync", d, ap)
        s.barrier()

    ident = s.sb([128, 128], F32, "ident")
    identb = s.sb([128, 128], BF16, "identb")
    xT = s.sb([128, 8, S], BF16, "xT")
    wbuf = None
    masks = None
    stat = s.sb([128, 2, 6], F32, "stat")
    mv = s.sb([128, 4], F32, "mv")
    DMA("sync", ident[:], D["c_ident"], w=["ident"])
    DMA("gpsimd", identb[:], D["c_ident"], w=["identb"])
    cnt = {"w": 0, "g": 0}

    def next_wbuf():
        cnt["w"] += 1
        return cnt["w"] % 3

    def gbank():
        cnt["g"] += 1
        return 4 + cnt["g"] % 2

    def xkeys(tq):
        return [("xT", 4 * tq + u) for u in range(4)]

    def transpose_tile(src, key, tt, f32dst=None, f32key=None):
        for half in range(2):
            b = 6 + half
            for j in range(4):
                c = half * 4 + j
                TR(PS[b][:, j * 128:(j + 1) * 128], src[:, c * 128:(c + 1) * 128], ident[:], [key, "ident"], [("ps", b)])
            vcopy(xT[:, half * 4:(half + 1) * 4, tt * 128:(tt + 1) * 128], PS[b][:].rearrange("p (j q) -> p j q", j=4),
                  [("ps", b)], [("xT", tt)])
            if f32dst is not None:
                acopy(f32dst[:, half * 4:(half + 1) * 4, :], PS[b][:].rearrange("p (j q) -> p j q", j=4), [("ps", b)], [f32key])

    def load_xT(src):
        s.push()
        xtile = [s.sb([128, DM], F32, f"xtile{i}") for i in range(2)]
        for tt in range(16):
            xt = xtile[tt % 2]
            DMA("sync", xt[:], src[tt * 128:(tt + 1) * 128, :], w=[("xtile", tt % 2)])
            transpose_tile(xt, ("xtile", tt % 2), tt)
        s.pop()

    def load_ln(l, gname, bname):
        DMA("sync", LN["g"][:], D[gname][l].partition_broadcast(128), w=["lng"])
        DMA("sync", LN["b"][:], D[bname][l].partition_broadcast(128), w=["lnb"])

    def ln_tile(xt, key, tt, dst, f32dst=None, f32key=None, do_T=True):
        for hf in range(2):
            V(lambda e, hf=hf: e.bn_stats(out=stat[:, hf, :], in_=xt[:, hf * 512:(hf + 1) * 512]), [key], ["stat"])
        V(lambda e: e.bn_aggr(out=mv[:, 0:2], in_=stat[:]), ["stat"], ["mv"])
        V(lambda e: e.tensor_scalar(out=mv[:, 3:4], in0=mv[:, 1:2], scalar1=1e-5, scalar2=None, op0=ALU.add), ["mv"], ["mv3"])
        A(lambda e: e.activation(out=mv[:, 3:4], in_=mv[:, 3:4], func=AF.Ln), ["mv3"], ["mv3"])
        A(lambda e: e.activation(out=mv[:, 2:3], in_=mv[:, 3:4], func=AF.Exp, scale=-0.5), ["mv3"], ["mv2"])
        V(lambda e: e.tensor_scalar(out=xt[:], in0=xt[:], scalar1=mv[:, 0:1], scalar2=mv[:, 2:3], op0=ALU.subtract,
                                    op1=ALU.mult), [key, "mv", "mv2"], [key])
        lg_, lb_ = LN["g"], LN["b"]
        G(lambda e: e.tensor_tensor(out=xt[:], in0=xt[:], in1=lg_[:], op=ALU.mult), [key, "lng"], [key])
        G(lambda e: e.tensor_tensor(out=xt[:], in0=xt[:], in1=lb_[:], op=ALU.add), [key, "lnb"], [key])
        DMA("sync", dst[tt * 128:(tt + 1) * 128, :], xt[:], r=[key], w=[("xs", tt)])
        if do_T:
            transpose_tile(xt, key, tt, f32dst, f32key)

    NBUF = 6

    def interleave(gens):
        gens = list(gens)
        while gens:
            for g_ in list(gens):
                try:
                    next(g_)
                except StopIteration:
                    gens.remove(g_)

    def ln_run(prod_phases, dst, do_T=True):
        bufs = [s.sb([128, DM], F32, f"lnx{i}") for i in range(NBUF)]
        stt = s.sb([128, NBUF, 2, 6], F32, "lnstat")
        mvt = s.sb([128, NBUF, 4], F32, "lnmv")
        lg_, lb_ = LN["g"], LN["b"]

        def p_stats(tt, xt, key, sl):
            for hf in range(2):
                V(lambda e, hf=hf: e.bn_stats(out=stt[:, sl, hf, :], in_=xt[:, hf * 512:(hf + 1) * 512]), [key], [("lnstat", sl)])
            V(lambda e: e.bn_aggr(out=mvt[:, sl, 0:2], in_=stt[:, sl, :, :]), [("lnstat", sl)], [("lnmv", sl)])
            V(lambda e: e.tensor_scalar(out=mvt[:, sl, 3:4], in0=mvt[:, sl, 1:2], scalar1=1e-5, scalar2=None, op0=ALU.add), [("lnmv", sl)], [("lnmv3", sl)])
            A(lambda e: e.activation(out=mvt[:, sl, 3:4], in_=mvt[:, sl, 3:4], func=AF.Ln), [("lnmv3", sl)], [("lnmv3", sl)])
            A(lambda e: e.activation(out=mvt[:, sl, 2:3], in_=mvt[:, sl, 3:4], func=AF.Exp, scale=-0.5), [("lnmv3", sl)], [("lnmv2", sl)])

        def p_norm(tt, xt, key, sl):
            V(lambda e: e.tensor_scalar(out=xt[:], in0=xt[:], scalar1=mvt[:, sl, 0:1], scalar2=mvt[:, sl, 2:3], op0=ALU.subtract,
                                        op1=ALU.mult), [key, ("lnmv", sl), ("lnmv2", sl)], [key])
            G(lambda e: e.tensor_tensor(out=xt[:], in0=xt[:], in1=lg_[:], op=ALU.mult), [key, "lng"], [key])
            G(lambda e: e.tensor_tensor(out=xt[:], in0=xt[:], in1=lb_[:], op=ALU.add), [key, "lnb"], [key])
            DMA("sync", dst[tt * 128:(tt + 1) * 128, :], xt[:], r=[key], w=[("xs", tt)])

        def p_T(tt, xt, key, sl):
            transpose_tile(xt, key, tt)

        phases = list(prod_phases) + [p_stats, p_norm] + ([p_T] if do_T else [])
        for step in range(16 + len(phases) - 1):
            for pi, ph in enumerate(phases):
                tt = step - pi
                if 0 <= tt < 16:
                    sl = tt % NBUF
                    ph(tt, bufs[sl], ("lnx", sl), sl)

    def proj_fm(pieces, m, evac, kchunks=8, rhsT=None, rkeys=None):
        i = next_wbuf()
        wb = wbuf[i]
        wk = []
        for pi, (o, ap) in enumerate(pieces):
            wd = ap.shape[1]
            DMA("gpsimd", wb[:, 0:kchunks, o:o + wd], ap.rearrange("(c p) m -> p c m", p=128), w=[("wbuf", i, pi)])
            wk.append(("wbuf", i, pi))
        for tq in range(4):
            b = gbank()
            for c in range(kchunks):
                MM(PS[b][0:m, :], wb[:, c, 0:m], xT[:, c, tq * 512:(tq + 1) * 512], c == 0, c == kchunks - 1,
                   wk + xkeys(tq), [("ps", b)])
            evac(PS[b], ("ps", b), tq)

    def proj_tm(pieces, n, evac):
        i = next_wbuf()
        wb = wbuf[i]
        wk = []
        for pi, (o, ap) in enumerate(pieces):
            wd = ap.shape[1]
            DMA("gpsimd", wb[:, :, o:o + wd], ap.rearrange("(c p) m -> p c m", p=128), w=[("wbuf", i, pi)])
            wk.append(("wbuf", i, pi))
        for tt in range(16):
            b = gbank()
            for c in range(8):
                MM(PS[b][:, 0:n], xT[:, c, tt * 128:(tt + 1) * 128], wb[:, c, 0:n], c == 0, c == 7,
                   wk + [("xT", tt)], [("ps", b)])
            evac(PS[b], ("ps", b), tt)

    ctr = {"sc": 0, "p": 0}
    AT = {}

    def attn(terms, kcs, vaug, ncols, pv_ok, accmap, scale=1.0):
        Pt = AT["P"]
        kcs = list(kcs)
        rng = {}
        for qs in range(4):
            ok = [k for k in kcs if pv_ok(k, qs)]
            rng[qs] = (ok[0], ok[-1])

        def score(kc):
            ctr["sc"] += 1
            sbk = ctr["sc"] % 2
            tl = terms(kc)
            for i, (lt, rh, rk) in enumerate(tl):
                MM(PS[sbk][:], lt, rh, i == 0, i == len(tl) - 1, rk, [("ps", sbk)])
            return sbk

        nxt = score(kcs[0])
        started = set()
        for idx, kc in enumerate(kcs):
            sbk = nxt
            ctr["p"] += 1
            pi = ctr["p"] % 3
            A(lambda e, pi=pi, sbk=sbk: e.activation(out=Pt[pi][:], in_=PS[sbk][:], func=AF.Exp, scale=scale),
              [("ps", sbk)], [("P", pi)])
            if idx + 1 < len(kcs):
                nxt = score(kcs[idx + 1])
            vap, vkey = vaug(kc)
            for qs in range(4):
                if not pv_ok(kc, qs):
                    continue
                bank, c0 = accmap(qs)
                first = bank not in started
                started.add(bank)
                MMs(PS[bank][:, c0:c0 + ncols], Pt[pi][:, qs * 128:(qs + 1) * 128], vap, first, kc == rng[qs][1],
                    [("P", pi), vkey], [("ps", bank)])

    def attn_out(accmap, dv, tt0, gate, dst, accumulate, normalize=True):
        rd = AT["rd"]

        def chain(qs):
            bank, c0 = accmap(qs)
            tt = tt0 + qs
            src = PS[bank][:, c0:c0 + dv]
            if not normalize:
                vcopy(dst(tt), src, [("ps", bank)], [("oacc", tt)])
                return
            V(lambda e: e.tensor_scalar(out=rd[:, qs, 2:3], in0=PS[bank][:, c0 + dv:c0 + dv + 1], scalar1=1e-30,
                                        scalar2=None, op0=ALU.max), [("ps", bank)], [("rd2", qs)])
            yield
            V(lambda e: e.reciprocal(out=rd[:, qs, 0:1], in_=rd[:, qs, 2:3]), [("rd2", qs)], [("rd0", qs)])
            yield
            sc = rd[:, qs, 0:1]
            rk = [("rd0", qs)]
            if gate is not None:
                gap = gate(tt)
                V(lambda e: e.tensor_tensor(out=rd[:, qs, 1:2], in0=rd[:, qs, 0:1], in1=gap, op=ALU.mult), [("rd0", qs), "gate"], [("rd1", qs)])
                yield
                sc = rd[:, qs, 1:2]
                rk = [("rd1", qs)]
            d = dst(tt)
            if accumulate:
                V(lambda e: e.scalar_tensor_tensor(out=d, in0=src, scalar=sc, in1=d, op0=ALU.mult, op1=ALU.add),
                  [("ps", bank), ("oacc", tt)] + rk, [("oacc", tt)])
            else:
                V(lambda e: e.tensor_scalar(out=d, in0=src, scalar1=sc, scalar2=None, op0=ALU.mult),
                  [("ps", bank)] + rk, [("oacc", tt)])
            yield
        interleave([chain(qs) for qs in range(4)])

    def to_fm(src, nchunk, dstT, dkey):
        for tt in range(16):
            b = 6 + tt % 2
            for c in range(nchunk):
                TR(PS[b][:, c * 128:(c + 1) * 128], src[:, tt, c * 128:(c + 1) * 128], ident[:], [("oacc", tt), "ident"], [("ps", b)])
            vcopy(dstT[:, 0:nchunk, tt * 128:(tt + 1) * 128], PS[b][:, 0:nchunk * 128].rearrange("p (j q) -> p j q", j=nchunk),
                  [("ps", b)], [(dkey, tt)])

    def tqs(tq):
        return slice(tq * 512, (tq + 1) * 512)

    def nsa(l, W, oacc):
        s.push()
        qa = [s.sb([68, S], BF16, f"qa{h}") for h in range(4)]
        kcA = [s.sb([68, 128], BF16, f"kcA{g}") for g in range(2)]
        vcA = [s.sb([128, 97], BF16, f"vcA{g}") for g in range(2)]
        gate = s.sb([128, 16, 12], F32, "gate")
        selTb = [s.sb([32, S], BF16, f"selTb{g}") for g in range(2)]
        cmaskb = s.sb([128, S], BF16, "cmaskb")
        selE = s.sb([32, 16, 128], BF16, "selE")
        vm = s.sb([128, 16, 32], F32, "vm")
        am = s.sb([128, 16, 32], F32, "am")
        imp = s.sb([128, 32], F32, "imp")
        impf = s.sb([128, 32], F32, "impf")
        top8 = s.sb([128, 8], F32, "top8")
        selb = s.sb([128, 32], F32, "selb")
        rdn = s.sb([128, 8], F32, "rdn")
        DMA("gpsimd", cmaskb[:], D["c_cmask"], w=["cmaskb"])
        DMA("gpsimd", selE[:], D["c_selE"], w=["selE"])
        DMA("sync", vm[:], D["c_impvm"], w=["vm"])
        DMA("sync", am[:], D["c_impam"], w=["am"])
        for h in range(4):
            DMA("gpsimd", qa[h][64:68, :], D["c_qaug"][h], w=[("qa", h, "aug")])
        for g in range(2):
            DMA("gpsimd", kcA[g][64:68, :], D["c_kcaug"], w=[("kcA", g, "aug")])
            V(lambda e, g=g: e.memset(vcA[g][:, 0:64], 0.0), [], [("vcA", g, "v")])
            V(lambda e, g=g: e.memset(vcA[g][:, 64:65], 1.0), [], [("vcA", g, "one")])
            DMA("gpsimd", vcA[g][:, 65:97], D["c_cover"], w=[("vcA", g, "cov")])
            V(lambda e, g=g: e.memset(kcA[g][0:64, :], 0.0), [], [("kcA", g, "k")])
        for h in range(4):
            def ev(ps, key, tq, h=h):
                V(lambda e: e.tensor_scalar(out=qa[h][0:64, tqs(tq)], in0=ps[0:64, :], scalar1=0.125, scalar2=None, op0=ALU.mult),
                  [key], [("qa", h, tq)])
            proj_fm([(0, W[:, h * 64:(h + 1) * 64])], 64, ev)

        s.push()
        srcT = [s.sb([64, S], BF16, f"srcT{g}") for g in range(2)]
        w1t = s.sb([64, 32, 256], BF16, "w1t")
        w2t = s.sb([128, 2, 64], BF16, "w2t")
        posr = s.sb([32, 64], F32, "posr")
        posT = s.sb([64, 32], BF16, "posT")
        hb = s.sb([128, 2], F32, "hb")
        hidT = s.sb([128, 2, 128], BF16, "hidT")
        for j in range(2):
            for g in range(2):
                def ev(ps, key, tq, g=g):
                    vcopy(srcT[g][:, tqs(tq)], ps[0:64, :], [key], [("srcT", g, tq)])
                c0 = 256 + 128 * j + 64 * g
                proj_fm([(0, W[:, c0:c0 + 64])], 64, ev)
            DMA("gpsimd", w1t[:], D["nsa_cmp_w1"][l, j].rearrange("(l d) h -> d l h", d=64), w=["w1t"])
            DMA("gpsimd", w2t[:], D["nsa_cmp_w2"][l, j].rearrange("(c p) d -> p c d", p=128), w=["w2t"])
            DMA("sync", posr[:], D["nsa_cmp_pos"][l, j], w=["posr"])
            TR(PS[6][0:64, 0:32], posr[:, :], ident[0:32, 0:32], ["posr", "ident"], [("ps", 6)])
            vcopy(posT[:], PS[6][0:64, 0:32], [("ps", 6)], ["posT"])
            for hc in range(2):
                for li in range(32):
                    MM(PS[7][:, hc:hc + 1], w1t[:, li, hc * 128:(hc + 1) * 128], posT[:, li:li + 1], li == 0, li == 31,
                       ["w1t", "posT"], [("ps", 7)])
            vcopy(hb[:], PS[7][:, 0:2], [("ps", 7)], ["hb"])
            for g in range(2):
                sk = [("srcT", g, tq) for tq in range(4)]
                for hc in range(2):
                    b = gbank()
                    for li in range(32):
                        MM(PS[b][:, 0:127], w1t[:, li, hc * 128:(hc + 1) * 128], srcT[g][:, li:li + 2017:16], li == 0, li == 31,
                           ["w1t"] + sk, [("ps", b)])
                    A(lambda e, b=b, hc=hc: e.activation(out=hidT[:, hc, 0:127], in_=PS[b][:, 0:127], func=AF.Gelu, bias=hb[:, hc:hc + 1]),
                      [("ps", b), "hb"], [("hidT", hc)])
                b = gbank()
                if j == 0:
                    for hc in range(2):
                        MM(PS[b][0:64, 0:127], w2t[:, hc, :], hidT[:, hc, 0:127], hc == 0, hc == 1, ["w2t", ("hidT", hc)], [("ps", b)])
                    vcopy(kcA[g][0:64, 0:127], PS[b][0:64, 0:127], [("ps", b)], [("kcA", g, "k")])
                else:
                    for hc in range(2):
                        MM(PS[b][0:127, 0:64], hidT[:, hc, 0:127], w2t[:, hc, :], hc == 0, hc == 1, ["w2t", ("hidT", hc)], [("ps", b)])
                    vcopy(vcA[g][0:127, 0:64], PS[b][0:127, 0:64], [("ps", b)], [("vcA", g, "v")])
        s.pop()

        s.push()
        ksA = [s.sb([68, S], BF16, f"ksA{g}") for g in range(2)]
        kwA = [s.sb([68, S], BF16, f"kwA{g}") for g in range(2)]
        vsA = s.sb([128, 16, 2, 65], BF16, "vsA")
        vwA = s.sb([128, 16, 2, 65], BF16, "vwA")
        V(lambda e: e.memset(vsA[:, :, :, 64:65], 1.0), [], [("vsA", tt) for tt in range(16)])
        V(lambda e: e.memset(vwA[:, :, :, 64:65], 1.0), [], [("vwA", tt) for tt in range(16)])
        for g in range(2):
            DMA("gpsimd", ksA[g][64:68, :], D["c_kaug"], w=[("ksA", g, "aug")])
            DMA("gpsimd", kwA[g][64:68, :], D["c_kaug"], w=[("kwA", g, "aug")])
            for nm, dst, c0 in (("ksA", ksA, 512), ("kwA", kwA, 768)):
                def ev(ps, key, tq, dst=dst, nm=nm, g=g):
                    vcopy(dst[g][0:64, tqs(tq)], ps[0:64, :], [key], [(nm, g, tq)])
                proj_fm([(0, W[:, c0 + 64 * g:c0 + 64 * g + 64])], 64, ev)

        def evv(ps, key, tt):
            vcopy(vsA[:, tt, :, 0:64], ps[:, 0:128].rearrange("p (g d) -> p g d", g=2), [key], [("vsA", tt)])
            vcopy(vwA[:, tt, :, 0:64], ps[:, 128:256].rearrange("p (g d) -> p g d", g=2), [key], [("vwA", tt)])
            vcopy(gate[:, tt, :], ps[:, 256:268], [key], [("gateraw", tt)])
            A(lambda e: e.activation(out=gate[:, tt, :], in_=gate[:, tt, :], func=AF.Sigmoid), [("gateraw", tt)], ["gate"])
        proj_tm([(0, W[:, 640:768]), (128, W[:, 896:1024]), (256, W[:, 1024:1036])], 268, evv)

        for g in range(2):
            for qt in range(4):
                for n in range(2):
                    h = 2 * g + n
                    MM(PS[n][:], kcA[g][:, :], qa[h][:, tqs(qt)], True, False,
                       [("kcA", g, "k"), ("kcA", g, "aug"), ("qa", h, qt), ("qa", h, "aug")], [("ps", n)])
                    MM(PS[n][:], identb[:], cmaskb[:, tqs(qt)], False, True, ["identb", "cmaskb"], [("ps", n)])
                    Pn = AT["P"][n]
                    A(lambda e, n=n, Pn=Pn: e.activation(out=Pn[:], in_=PS[n][:], func=AF.Exp), [("ps", n)], [("P", n)])
                    for qs in range(4):
                        MM(PS[2 + n][:, qs * 97:(qs + 1) * 97], Pn[:, qs * 128:(qs + 1) * 128], vcA[g][:, :], True, True,
                           [("P", n), ("vcA", g, "v"), ("vcA", g, "one"), ("vcA", g, "cov")], [("ps", 2 + n)])
                for qs in range(4):
                    tt = 4 * qt + qs
                    for n in range(2):
                        h = 2 * g + n
                        c0 = qs * 97
                        V(lambda e, n=n, c0=c0: e.tensor_scalar(out=rdn[:, 4 + n:5 + n], in0=PS[2 + n][:, c0 + 64:c0 + 65], scalar1=1e-30,
                                                               scalar2=None, op0=ALU.max), [("ps", 2 + n)], [("rdn", 4 + n)])
                        V(lambda e, n=n: e.reciprocal(out=rdn[:, n:n + 1], in_=rdn[:, 4 + n:5 + n]), [("rdn", 4 + n)], [("rdn", n)])
                        V(lambda e, n=n, h=h, tt=tt: e.tensor_tensor(out=rdn[:, 2 + n:3 + n], in0=rdn[:, n:n + 1], in1=gate[:, tt, 3 * h:3 * h + 1],
                                                                      op=ALU.mult), [("rdn", n), "gate"], [("rdn", 2 + n)])
                        V(lambda e, n=n, h=h, tt=tt, c0=c0: e.tensor_scalar(out=oacc[:, tt, h * 64:(h + 1) * 64], in0=PS[2 + n][:, c0:c0 + 64],
                                                                         scalar1=rdn[:, 2 + n:3 + n], scalar2=None, op0=ALU.mult),
                          [("ps", 2 + n), ("rdn", 2 + n)], [("oacc", tt)])
                    c0 = qs * 97
                    V(lambda e, c0=c0: e.tensor_scalar(out=imp[:], in0=PS[2][:, c0 + 65:c0 + 97], scalar1=rdn[:, 0:1], scalar2=None, op0=ALU.mult),
                      [("ps", 2), ("rdn", 0)], ["imp"])
                    V(lambda e, c0=c0: e.scalar_tensor_tensor(out=imp[:], in0=PS[3][:, c0 + 65:c0 + 97], scalar=rdn[:, 1:2], in1=imp[:],
                                                             op0=ALU.mult, op1=ALU.add), [("ps", 3), ("rdn", 1), "imp"], ["imp"])
                    V(lambda e, tt=tt: e.tensor_tensor(out=impf[:], in0=imp[:], in1=vm[:, tt, :], op=ALU.mult), ["imp", "vm"], ["impf"])
                    V(lambda e, tt=tt: e.tensor_tensor(out=impf[:], in0=impf[:], in1=am[:, tt, :], op=ALU.add), ["impf", "am"], ["impf"])
                    V(lambda e: e.max(out=top8[:], in_=impf[:]), ["impf"], ["top8"])
                    V(lambda e: e.tensor_scalar(out=selb[:], in0=impf[:], scalar1=top8[:, 7:8], scalar2=None, op0=ALU.is_ge),
                      ["impf", "top8"], ["selb"])
                    V(lambda e: e.tensor_scalar(out=selb[:], in0=selb[:], scalar1=-1.0, scalar2=-NEG, op0=ALU.add, op1=ALU.mult),
                      ["selb"], ["selb"])
                    TR(PS[6][0:32, qs * 128:(qs + 1) * 128], selb[:, :], ident[:], ["selb", "ident"], [("ps", 6)])
                vcopy(selTb[g][:, tqs(qt)], PS[6][0:32, :], [("ps", 6)], [("selTb", g, qt)])

        it = 0
        for br, kA, vA, nm, vnm in ((1, ksA, vsA, "ksA", "vsA"), (2, kwA, vwA, "kwA", "vwA")):
            for h in range(4):
                g = h // 2
                for qt in range(4):
                    it += 1
                    bank = 2 + it % 2
                    if br == 1:
                        kcs = range(0, 4 * qt + 4)
                        pv_ok = lambda kc, qs, qt=qt: kc <= 4 * qt + qs
                    else:
                        kcs = range(max(0, 4 * qt - 4), 4 * qt + 4)
                        pv_ok = lambda kc, qs, qt=qt: 4 * qt + qs - 4 <= kc <= 4 * qt + qs

                    def terms(kc, h=h, g=g, qt=qt, br=br, kA=kA, nm=nm):
                        tl = [(kA[g][:, kc * 128:(kc + 1) * 128], qa[h][:, tqs(qt)],
                               [(nm, g, kc // 4), (nm, g, "aug"), ("qa", h, qt), ("qa", h, "aug")])]
                        if br == 1:
                            tl.append((selE[:, kc, :], selTb[g][:, tqs(qt)], ["selE", ("selTb", g, qt)]))
                        if kc >= 4 * qt:
                            tl.append((identb[:], masks[:, kc - 4 * qt, :], ["identb", "masks"]))
                        elif br == 2:
                            tl.append((identb[:], masks[:, 8 + kc - (4 * qt - 4), :], ["identb", "masks"]))
                        return tl

                    def vaug(kc, g=g, vA=vA, vnm=vnm):
                        return vA[:, kc, g, :], (vnm, kc)

                    accmap = lambda qs, bank=bank: (bank, qs * 65)
                    attn(terms, kcs, vaug, 65, pv_ok, accmap)
                    attn_out(accmap, 64, 4 * qt, lambda tt, h=h, br=br: gate[:, tt, 3 * h + br:3 * h + br + 1],
                             lambda tt, h=h: oacc[:, tt, h * 64:(h + 1) * 64], True)
        s.pop()
        s.pop()

    def sbranch(l, W, oacc):
        s.push()
        qT = [s.sb([64, S], BF16, f"sbq{h}") for h in range(4)]
        kT = [s.sb([64, S], BF16, f"sbk{h}") for h in range(4)]
        vB = s.sb([128, 16, 256], BF16, "sbv")
        negU = s.sb([128, 128], BF16, "negU")
        negO = s.sb([128, 128], F32, "negO")
        et = [s.sb([128, 512], F32, f"et{i}") for i in range(2)]
        spt = [s.sb([128, 512], BF16, f"spt{i}") for i in range(3)]
        acc = [s.sb([128, 512], F32, f"sbacc{i}") for i in range(2)]
        DMA("gpsimd", negU[:], D["c_negU"], w=["negU"])
        V(lambda e: e.memset(negO[:], -1.0), [], ["negO"])
        for h in range(4):
            def evq(ps, key, tq, h=h):
                V(lambda e: e.tensor_scalar(out=qT[h][:, tqs(tq)], in0=ps[0:64, :], scalar1=0.125, scalar2=None, op0=ALU.mult),
                  [key], [("sbq", h, tq)])
            proj_fm([(0, W[:, 1036 + h * 64:1036 + (h + 1) * 64])], 64, evq)

            def evk(ps, key, tq, h=h):
                vcopy(kT[h][:, tqs(tq)], ps[0:64, :], [key], [("sbk", h, tq)])
            proj_fm([(0, W[:, 1292 + h * 64:1292 + (h + 1) * 64])], 64, evk)

        def evv(ps, key, tt):
            vcopy(vB[:, tt, :], ps[:, 0:256], [key], [("sbv", tt)])
        proj_tm([(0, W[:, 1548:1804])], 256, evv)

        SB3 = (0, 1, 5)
        st = {"i": 0, "a": 0, "it": 0}
        for h in range(4):
            for qt in range(4):
                st["it"] += 1
                bank = 2 + st["it"] % 2
                kcs = list(range(4 * qt + 3, -1, -1))

                def stageA(kc, h=h, qt=qt):
                    st["i"] += 1
                    i = st["i"]
                    sbk = SB3[i % 3]
                    MM(PS[sbk][:], kT[h][:, kc * 128:(kc + 1) * 128], qT[h][:, tqs(qt)], True, False,
                       [("sbk", h, kc // 4), ("sbq", h, qt)], [("ps", sbk)])
                    if kc >= 4 * qt:
                        MM(PS[sbk][:], identb[:], masks[:, 4 + kc - 4 * qt, :], False, False, ["identb", "masks"], [("ps", sbk)])
                    A(lambda e: e.activation(out=et[i % 2][:], in_=PS[sbk][:], func=AF.Exp), [("ps", sbk)], [("et", i % 2)])
                    A(lambda e: e.activation(out=spt[i % 3][:], in_=et[i % 2][:], func=AF.Ln, bias=1.0), [("et", i % 2)], [("spt", i % 3)])
                    return i

                def stageB(kc, i, idx, h=h, qt=qt, bank=bank):
                    sbk = SB3[i % 3]
                    a = st["a"]
                    MM(PS[sbk][:], negU[:], spt[i % 3][:], False, idx == 0, ["negU", ("spt", i % 3)], [("ps", sbk)])
                    if idx > 0:
                        MM(PS[sbk][:], negO[:], acc[a % 2][:], False, True, ["negO", ("sbacc", a % 2)], [("ps", sbk)])
                    ctr["p"] += 1
                    pi = ctr["p"] % 3
                    Pp = AT["P"][pi]
                    A(lambda e: e.activation(out=Pp[:], in_=PS[sbk][:], func=AF.Exp), [("ps", sbk)], [("P", pi)])
                    for qs in range(4):
                        if kc > 4 * qt + qs:
                            continue
                        MMs(PS[bank][:, qs * 64:(qs + 1) * 64], Pp[:, qs * 128:(qs + 1) * 128], vB[:, kc, h * 64:(h + 1) * 64],
                            idx == 0 and qs == 3, kc == 0, [("P", pi), ("sbv", kc)], [("ps", bank)])
                    if kc > 0:
                        if idx == 0:
                            G(lambda e: e.tensor_copy(out=acc[(a + 1) % 2][:], in_=spt[i % 3][:]), [("spt", i % 3)], [("sbacc", (a + 1) % 2)])
                        else:
                            G(lambda e: e.tensor_tensor(out=acc[(a + 1) % 2][:], in0=acc[a % 2][:], in1=spt[i % 3][:], op=ALU.add),
                              [("spt", i % 3), ("sbacc", a % 2)], [("sbacc", (a + 1) % 2)])
                        st["a"] += 1

                cur = stageA(kcs[0])
                for idx, kc in enumerate(kcs):
                    nxt = stageA(kcs[idx + 1]) if idx + 1 < len(kcs) else None
                    stageB(kc, cur, idx)
                    cur = nxt
                accmap = lambda qs, bank=bank: (bank, qs * 64)
                attn_out(accmap, 64, 4 * qt, None, lambda tt, h=h: oacc[:, tt, h * 64:(h + 1) * 64], False, normalize=False)
        s.pop()

    def rglru(l, W, oT):
        s.push()
        xr = s.sb([128, S + 3], F32, "xr")
        xg = s.sb([128, S], F32, "xg")
        u = s.sb([128, S], F32, "u")
        ub = s.sb([128, S], BF16, "ub")
        ra = s.sb([128, S], F32, "ra")
        ib = s.sb([128, S], F32, "ib")
        hh = s.sb([128, S], F32, "hh")
        prm = s.sb([128, 12], F32, "prm")
        gw = [s.sb([128, 128], BF16, f"gw{i}") for i in range(2)]
        for ch in range(2):
            cs = slice(ch * 128, (ch + 1) * 128)
            for tap in range(4):
                DMA("sync", prm[:, tap:tap + 1], D["rnn_conv_w"][l, tap, cs].rearrange("(c o) -> c o", o=1), w=[("prm", tap)])
            for i, nm in enumerate(("rnn_conv_b", "rnn_ga_b", "rnn_gx_b", "rnn_lambda")):
                DMA("sync", prm[:, 4 + i:5 + i], D[nm][l, cs].rearrange("(c o) -> c o", o=1), w=[("prm", 4 + i)])
            for i, nm in enumerate(("rnn_ga_w", "rnn_gx_w")):
                V(lambda e, i=i: e.memset(gw[i][:], 0.0), [], [("gw", i)])
                for n in range(2):
                    DMA("gpsimd", gw[i][n * 64:(n + 1) * 64, n * 64:(n + 1) * 64], D[nm][l, 2 * ch + n], w=[("gw", i)])
            A(lambda e: e.activation(out=prm[:, 9:10], in_=prm[:, 7:8], func=AF.Exp, scale=-1.0), [("prm", 7)], [("prm", 9)])
            A(lambda e: e.activation(out=prm[:, 9:10], in_=prm[:, 9:10], func=AF.Ln, bias=1.0), [("prm", 9)], [("prm", 9)])
            V(lambda e: e.tensor_scalar(out=prm[:, 8:9], in0=prm[:, 9:10], scalar1=-8.0, scalar2=None, op0=ALU.mult), [("prm", 9)], [("prm", 8)])
            V(lambda e: e.memset(xr[:, 0:3], 0.0), [], [("xr", "pad")])

            def evx(ps, key, tq):
                vcopy(xr[:, 3 + tq * 512:3 + (tq + 1) * 512], ps[:, :], [key], [("xr", tq)])
            proj_fm([(0, W[:, 1804 + ch * 128:1804 + (ch + 1) * 128])], 128, evx)

            def evg(ps, key, tq):
                A(lambda e: e.activation(out=xg[:, tqs(tq)], in_=ps[:, :], func=AF.Gelu), [key], [("xg", tq)])
            proj_fm([(0, W[:, 2060 + ch * 128:2060 + (ch + 1) * 128])], 128, evg)
            xk = [("xr", tq) for tq in range(4)] + [("xr", "pad")]
            V(lambda e: e.tensor_scalar(out=u[:], in0=xr[:, 0:S], scalar1=prm[:, 0:1], scalar2=prm[:, 4:5], op0=ALU.mult, op1=ALU.add),
              xk + [("prm", 0), ("prm", 4)], ["u"])
            for tap in range(1, 4):
                V(lambda e, tap=tap: e.scalar_tensor_tensor(out=u[:], in0=xr[:, tap:tap + S], scalar=prm[:, tap:tap + 1], in1=u[:],
                                                            op0=ALU.mult, op1=ALU.add), xk + [("prm", tap), "u"], ["u"])
            vcopy(ub[:], u[:], ["u"], ["ub"])
            for tq in range(4):
                for i, dst, bcol in ((0, ra, 5), (1, ib, 6)):
                    b = gbank()
                    MM(PS[b][:], gw[i][:], ub[:, tqs(tq)], True, True, [("gw", i), "ub"], [("ps", b)])
                    A(lambda e, b=b, dst=dst, bcol=bcol, tq=tq: e.activation(out=dst[:, tqs(tq)], in_=PS[b][:], func=AF.Sigmoid,
                                                                             bias=prm[:, bcol:bcol + 1]), [("ps", b), ("prm", bcol)], [(("ra", "ib")[i], tq)])
            rk = [("ra", tq) for tq in range(4)]
            ik = [("ib", tq) for tq in range(4)]
            A(lambda e: e.activation(out=ra[:], in_=ra[:], func=AF.Exp, scale=prm[:, 8:9]), rk + [("prm", 8)], rk)
            V(lambda e: e.tensor_tensor(out=ib[:], in0=ib[:], in1=u[:], op=ALU.mult), ik + ["u"], ik)
            V(lambda e: e.tensor_tensor(out=hh[:], in0=ra[:], in1=ra[:], op=ALU.mult), rk, ["hh"])
            V(lambda e: e.tensor_scalar(out=hh[:], in0=hh[:], scalar1=-1.0, scalar2=1.0, op0=ALU.mult, op1=ALU.add), ["hh"], ["hh"])
            V(lambda e: e.tensor_scalar(out=hh[:], in0=hh[:], scalar1=0.0, scalar2=None, op0=ALU.max), ["hh"], ["hh"])
            A(lambda e: e.activation(out=hh[:], in_=hh[:], func=AF.Sqrt), ["hh"], ["hh"])
            V(lambda e: e.tensor_tensor(out=ib[:], in0=ib[:], in1=hh[:], op=ALU.mult), ik + ["hh"], ik)
            V(lambda e: e.tensor_tensor_scan(out=hh[:], data0=ra[:], data1=ib[:], initial=0.0, op0=ALU.mult, op1=ALU.add),
              rk + ik + ["hh"], ["hh"])
            V(lambda e, ch=ch: e.tensor_tensor(out=oT[:, 2, ch, :], in0=hh[:], in1=xg[:], op=ALU.mult),
              ["hh"] + [("xg", tq) for tq in range(4)], [("oT", 2, ch)])
        s.pop()

    def mla(l, W, oacc):
        s.push()
        cqn = s.sb([128, 2, S], BF16, "cqn")
        ckvn = s.sb([128, S], BF16, "ckvn")
        wuq = s.sb([128, 2, 384], BF16, "wuq")
        wuqS = s.sb([128, 2, 384], BF16, "wuqS")
        wukv = s.sb([128, 512], BF16, "wukv")
        gq = s.sb([128, 2], F32, "gq")
        gkv = s.sb([128, 1], F32, "gkv")
        onesF = s.sb([128, 128], F32, "onesF")
        V(lambda e: e.memset(onesF[:], 1.0), [], ["onesF"])
        V(lambda e: e.memset(wuqS[:], 0.0), [], ["wuqS"])
        V(lambda e: e.memset(cqn[:], 0.0), [], ["cqn0"])
        Wq = D["mla_w_uq"][l]
        DMA("gpsimd", wuq[:, 0, :], Wq[0:128, :], w=[("wuq", 0)])
        DMA("gpsimd", wuq[0:64, 1, :], Wq[128:192, :], w=[("wuq", 1)])
        for h in range(4):
            for (r0, r1, cidx) in ((0, 128, 0), (128, 192, 1)):
                np_ = r1 - r0
                DMA("gpsimd", wuqS[0:np_, cidx, h * 96 + 64:h * 96 + 80], Wq[r0:r1, h * 96 + 80:h * 96 + 96], r=["wuqS"], w=[("wuqS", h, cidx, 0)])
                DMA("gpsimd", wuqS[0:np_, cidx, h * 96 + 80:h * 96 + 96], Wq[r0:r1, h * 96 + 64:h * 96 + 80], r=["wuqS"], w=[("wuqS", h, cidx, 1)])
        DMA("gpsimd", wukv[:], D["mla_w_ukv"][l], w=["wukv"])
        DMA("sync", gq[:, 0:1], D["mla_q_norm"][l, 0:128].rearrange("(c o) -> c o", o=1), w=[("gq", 0)])
        DMA("sync", gq[0:64, 1:2], D["mla_q_norm"][l, 128:192].rearrange("(c o) -> c o", o=1), w=[("gq", 1)])
        DMA("sync", gkv[:, 0:1], D["mla_kv_norm"][l].rearrange("(c o) -> c o", o=1), w=["gkv"])

        s.push()
        cq = s.sb([128, 2, S], F32, "cq")
        ckv = s.sb([128, S], F32, "ckv")
        sq = s.sb([128, 2, 512], F32, "sq")
        rstd = s.sb([128, 512], F32, "rstd")

        def ev0(ps, key, tq):
            vcopy(cq[:, 0, tqs(tq)], ps[:, :], [key], [("cq", 0, tq)])
        proj_fm([(0, W[:, 2316:2444])], 128, ev0)

        def ev1(ps, key, tq):
            vcopy(cq[0:64, 1, tqs(tq)], ps[0:64, :], [key], [("cq", 1, tq)])
        proj_fm([(0, W[:, 2444:2508])], 64, ev1)

        def ev2(ps, key, tq):
            vcopy(ckv[:, tqs(tq)], ps[:, :], [key], [("ckv", tq)])
        proj_fm([(0, W[:, 2508:2636])], 128, ev2)
        for tq in range(4):
            V(lambda e, tq=tq: e.tensor_tensor(out=sq[:, 0, :], in0=cq[:, 0, tqs(tq)], in1=cq[:, 0, tqs(tq)], op=ALU.mult), [("cq", 0, tq)], [("sq", 0)])
            V(lambda e, tq=tq: e.tensor_tensor(out=sq[0:64, 1, :], in0=cq[0:64, 1, tqs(tq)], in1=cq[0:64, 1, tqs(tq)], op=ALU.mult), [("cq", 1, tq)], [("sq", 1)])
            b = gbank()
            MM(PS[b][:], onesF[:, :], sq[:, 0, :], True, False, ["onesF", ("sq", 0)], [("ps", b)])
            MM(PS[b][:], onesF[0:64, :], sq[0:64, 1, :], False, True, ["onesF", ("sq", 1)], [("ps", b)])
            V(lambda e, b=b: e.tensor_scalar(out=rstd[:], in0=PS[b][:], scalar1=1.0 / 192, scalar2=1e-6, op0=ALU.mult, op1=ALU.add), [("ps", b)], ["rstd"])
            A(lambda e: e.activation(out=rstd[:], in_=rstd[:], func=AF.Ln), ["rstd"], ["rstd"])
            A(lambda e: e.activation(out=rstd[:], in_=rstd[:], func=AF.Exp, scale=-0.5), ["rstd"], ["rstd"])
            V(lambda e, tq=tq: e.scalar_tensor_tensor(out=cqn[:, 0, tqs(tq)], in0=cq[:, 0, tqs(tq)], scalar=gq[:, 0:1], in1=rstd[:], op0=ALU.mult, op1=ALU.mult),
              [("cq", 0, tq), ("gq", 0), "rstd", "cqn0"], [("cqn", tq, 0)])
            V(lambda e, tq=tq: e.scalar_tensor_tensor(out=cqn[0:64, 1, tqs(tq)], in0=cq[0:64, 1, tqs(tq)], scalar=gq[0:64, 1:2], in1=rstd[0:64, :], op0=ALU.mult, op1=ALU.mult),
              [("cq", 1, tq), ("gq", 1), "rstd", "cqn0"], [("cqn", tq, 1)])
            V(lambda e, tq=tq: e.tensor_tensor(out=sq[:, 0, :], in0=ckv[:, tqs(tq)], in1=ckv[:, tqs(tq)], op=ALU.mult), [("ckv", tq)], [("sq", 0)])
            b = gbank()
            MM(PS[b][:], onesF[:, :], sq[:, 0, :], True, True, ["onesF", ("sq", 0)], [("ps", b)])
            V(lambda e, b=b: e.tensor_scalar(out=rstd[:], in0=PS[b][:], scalar1=1.0 / 128, scalar2=1e-6, op0=ALU.mult, op1=ALU.add), [("ps", b)], ["rstd"])
            A(lambda e: e.activation(out=rstd[:], in_=rstd[:], func=AF.Ln), ["rstd"], ["rstd"])
            A(lambda e: e.activation(out=rstd[:], in_=rstd[:], func=AF.Exp, scale=-0.5), ["rstd"], ["rstd"])
            V(lambda e, tq=tq: e.scalar_tensor_tensor(out=ckvn[:, tqs(tq)], in0=ckv[:, tqs(tq)], scalar=gkv[:, 0:1], in1=rstd[:], op0=ALU.mult, op1=ALU.mult),
              [("ckv", tq), "gkv", "rstd"], [("ckvn", tq)])
        s.pop()

        s.push()
        QT = [s.sb([96, S], BF16, f"mq{h}") for h in range(4)]
        KT = [s.sb([96, S], BF16, f"mk{h}") for h in range(4)]
        vA = s.sb([128, 16, 4, 65], BF16, "mv")
        ropec = s.sb([96, S], F32, "ropec")
        ropes = s.sb([96, S], F32, "ropes")
        t1 = s.sb([96, 512], F32, "t1")
        t2 = s.sb([96, 512], F32, "t2")
        krw = s.sb([128, 8, 96], BF16, "krw")
        krwS = s.sb([128, 8, 96], BF16, "krwS")
        DMA("sync", ropec[:], D["c_ropec"], w=["ropec"])
        DMA("sync", ropes[:], D["c_ropes"], w=["ropes"])
        V(lambda e: e.memset(vA[:, :, :, 64:65], 1.0), [], [("mv", tt) for tt in range(16)])
        V(lambda e: e.memset(krw[:], 0.0), [], ["krw"])
        V(lambda e: e.memset(krwS[:], 0.0), [], ["krwS"])
        DMA("gpsimd", krw[:, :, 64:96], W[:, 2636:2668].rearrange("(c p) m -> p c m", p=128), r=["krw"], w=["krw1"])
        DMA("gpsimd", krwS[:, :, 64:80], W[:, 2652:2668].rearrange("(c p) m -> p c m", p=128), r=["krwS"], w=["krwS1"])
        DMA("gpsimd", krwS[:, :, 80:96], W[:, 2636:2652].rearrange("(c p) m -> p c m", p=128), r=["krwS"], w=["krwS2"])

        def rope_comb(pa, pb, ka, kb, dst, dkey, tq):
            V(lambda e: e.tensor_tensor(out=t1[64:96, :], in0=pa[64:96, :], in1=ropec[64:96, tqs(tq)], op=ALU.mult), [ka, "ropec"], ["t1"])
            V(lambda e: e.tensor_tensor(out=t2[64:96, :], in0=pb[64:96, :], in1=ropes[64:96, tqs(tq)], op=ALU.mult), [kb, "ropes"], ["t2"])
            V(lambda e: e.tensor_tensor(out=dst[64:96, tqs(tq)], in0=t1[64:96, :], in1=t2[64:96, :], op=ALU.add), ["t1", "t2"], [dkey])

        for tq in range(4):
            for c in range(8):
                MM(PS[4][0:96, :], krw[:, c, :], xT[:, c, tqs(tq)], c == 0, c == 7, ["krw", "krw1"] + xkeys(tq), [("ps", 4)])
            for c in range(8):
                MM(PS[5][0:96, :], krwS[:, c, :], xT[:, c, tqs(tq)], c == 0, c == 7, ["krwS", "krwS1", "krwS2"] + xkeys(tq), [("ps", 5)])
            rope_comb(PS[4], PS[5], ("ps", 4), ("ps", 5), KT[0], ("mk", 0, tq, "r"), tq)
            for h in range(1, 4):
                vcopy(KT[h][64:96, tqs(tq)], KT[0][64:96, tqs(tq)], [("mk", 0, tq, "r")], [("mk", h, tq, "r")])
        cqk = lambda tq: [("cqn", tq, 0), ("cqn", tq, 1), "cqn0"]
        for h in range(4):
            for tq in range(4):
                hs = slice(h * 96, (h + 1) * 96)
                MM(PS[4][0:96, :], wuq[:, 0, hs], cqn[:, 0, tqs(tq)], True, False, [("wuq", 0)] + cqk(tq), [("ps", 4)])
                MM(PS[4][0:96, :], wuq[0:64, 1, hs], cqn[0:64, 1, tqs(tq)], False, True, [("wuq", 1)] + cqk(tq), [("ps", 4)])
                wsk = ["wuqS"] + [("wuqS", h, ci, j) for ci in range(2) for j in range(2)]
                MM(PS[5][0:96, :], wuqS[:, 0, hs], cqn[:, 0, tqs(tq)], True, False, wsk + cqk(tq), [("ps", 5)])
                MM(PS[5][0:96, :], wuqS[0:64, 1, hs], cqn[0:64, 1, tqs(tq)], False, True, wsk + cqk(tq), [("ps", 5)])
                vcopy(QT[h][0:64, tqs(tq)], PS[4][0:64, :], [("ps", 4)], [("mq", h, tq)])
                rope_comb(PS[4], PS[5], ("ps", 4), ("ps", 5), QT[h], ("mq", h, tq, "r"), tq)
                b = 6 + tq % 2
                MM(PS[b][0:64, :], wukv[:, h * 128:h * 128 + 64], ckvn[:, tqs(tq)], True, True, ["wukv", ("ckvn", tq)], [("ps", b)])
                vcopy(KT[h][0:64, tqs(tq)], PS[b][0:64, :], [("ps", b)], [("mk", h, tq)])
        for tt in range(16):
            b = 6 + tt % 2
            for h in range(4):
                MM(PS[b][:, h * 64:(h + 1) * 64], ckvn[:, tt * 128:(tt + 1) * 128], wukv[:, h * 128 + 64:h * 128 + 128], True, True,
                   ["wukv", ("ckvn", tt // 4)], [("ps", b)])
            vcopy(vA[:, tt, :, 0:64], PS[b][:, 0:256].rearrange("p (h d) -> p h d", h=4), [("ps", b)], [("mv", tt)])
        it = 0
        for h in range(4):
            for qt in range(4):
                it += 1
                bank = 2 + it % 2

                def terms(kc, h=h, qt=qt):
                    tl = [(KT[h][:, kc * 128:(kc + 1) * 128], QT[h][:, tqs(qt)],
                           [("mk", h, kc // 4), ("mk", h, kc // 4, "r"), ("mq", h, qt), ("mq", h, qt, "r")])]
                    if kc >= 4 * qt:
                        tl.append((identb[:], masks[:, kc - 4 * qt, :], ["identb", "masks"]))
                    return tl
                accmap = lambda qs, bank=bank: (bank, qs * 65)
                attn(terms, range(0, 4 * qt + 4), lambda kc, h=h: (vA[:, kc, h, :], ("mv", kc)), 65,
                     lambda kc, qs, qt=qt: kc <= 4 * qt + qs, accmap, scale=96 ** -0.5)
                attn_out(accmap, 64, 4 * qt, None, lambda tt, h=h: oacc[:, tt, h * 64:(h + 1) * 64], False)
        s.pop()
        s.pop()

    def merge_ln1(l, W, oT, xsrc):
        s.push()
        mT = s.sb([128, 8, S], BF16, "mT")
        macc = s.sb([128, S], F32, "macc")
        sg = [s.sb([128, 512], F32, f"sg{i}") for i in range(2)]
        wbr = [s.sb([128, 2, 128], BF16, f"wbr{i}") for i in range(2)]
        k = 0
        for dc in range(8):
            for n in range(4):
                k += 1
                i = next_wbuf()
                wb = wbuf[i]
                c0 = 2668 + n * 1024 + dc * 128
                DMA("gpsimd", wb[:, :, 0:128], W[:, c0:c0 + 128].rearrange("(c p) m -> p c m", p=128), w=[("wbuf", i, 0)])
                DMA("gpsimd", wbr[k % 2][:], D["w_branch"][l, n][:, dc * 128:(dc + 1) * 128].rearrange("(c p) m -> p c m", p=128), w=[("wbr", k % 2)])
                for tq in range(4):
                    bg = 4 + tq % 2
                    bu = 6 + tq % 2
                    for c in range(8):
                        MM(PS[bg][:], wb[:, c, 0:128], xT[:, c, tqs(tq)], c == 0, c == 7, [("wbuf", i, 0)] + xkeys(tq), [("ps", bg)])
                    for c in range(2):
                        MM(PS[bu][:], wbr[k % 2][:, c, :], oT[:, n, c, tqs(tq)], c == 0, c == 1, [("wbr", k % 2), ("oT", n, c)] + [("oT", n, tt) for tt in range(4 * tq, 4 * tq + 4)], [("ps", bu)])
                    A(lambda e, bg=bg, tq=tq: e.activation(out=sg[tq % 2][:], in_=PS[bg][:], func=AF.Sigmoid), [("ps", bg)], [("sg", tq % 2)])
                    if n == 0:
                        V(lambda e, bu=bu, tq=tq: e.tensor_tensor(out=macc[:, tqs(tq)], in0=sg[tq % 2][:], in1=PS[bu][:], op=ALU.mult),
                          [("sg", tq % 2), ("ps", bu)], [("macc", tq)])
                    else:
                        V(lambda e, bu=bu, tq=tq: e.tensor_tensor(out=sg[tq % 2][:], in0=sg[tq % 2][:], in1=PS[bu][:], op=ALU.mult),
                          [("sg", tq % 2), ("ps", bu)], [("sg", tq % 2)])
                        if n < 3:
                            G(lambda e, tq=tq: e.tensor_tensor(out=macc[:, tqs(tq)], in0=macc[:, tqs(tq)], in1=sg[tq % 2][:], op=ALU.add),
                              [("sg", tq % 2), ("macc", tq)], [("macc", tq)])
                        else:
                            G(lambda e, tq=tq, dc=dc: e.tensor_tensor(out=mT[:, dc, tqs(tq)], in0=macc[:, tqs(tq)], in1=sg[tq % 2][:], op=ALU.add),
                              [("sg", tq % 2), ("macc", tq)], [("mT", dc, tq)])
        s.barrier()
        wo = s.sb([128, 8, 1024], BF16, "wo")
        lng_, lnb_ = s.sb([128, DM], F32, "lng"), s.sb([128, DM], F32, "lnb")
        LN["g"], LN["b"] = lng_, lnb_
        load_ln(l, "ln1_g", "ln1_b")
        DMA("gpsimd", wo[:], D["w_out"][l].rearrange("(c p) m -> p c m", p=128), w=["wo"])

        def p0(tt, xt, key, sl):
            DMA("sync", xt[:], xsrc[tt * 128:(tt + 1) * 128, :], w=[key])

        def p1(tt, xt, key, sl):
            for hf in range(2):
                b = 4 + hf
                for dc in range(8):
                    MM(PS[b][:], mT[:, dc, tt * 128:(tt + 1) * 128], wo[:, dc, hf * 512:(hf + 1) * 512], dc == 0, dc == 7,
                       ["wo", ("mT", dc, tt // 4)], [("ps", b)])
                V(lambda e, b=b, hf=hf: e.scalar_tensor_tensor(out=xt[:, hf * 512:(hf + 1) * 512], in0=xt[:, hf * 512:(hf + 1) * 512],
                                                              scalar=DN_ALPHA, in1=PS[b][:], op0=ALU.mult, op1=ALU.add), [key, ("ps", b)], [key])
        ln_run([p0, p1], xs)
        s.pop()

    LN = {}

    def cross(l):
        s.push()
        AT["P"] = [s.sb([128, 512], BF16, f"P{i}") for i in range(3)]
        AT["rd"] = s.sb([128, 4, 4], F32, "rd")
        memT = s.sb([128, 8, 256], BF16, "memT")
        KxT = [s.sb([128, 256], BF16, f"KxT{h}") for h in range(4)]
        vxA = s.sb([128, 2, 4, 129], BF16, "vxA")
        QxT = [s.sb([128, S], BF16, f"QxT{h}") for h in range(4)]
        ox = s.sb([128, 16, 512], F32, "ox")
        mt = [s.sb([128, DM], F32, f"mt{i}") for i in range(2)]
        V(lambda e: e.memset(vxA[:, :, :, 128:129], 1.0), [], [("vxA", 0), ("vxA", 1)])
        for mc in range(2):
            DMA("sync", mt[mc][:], D["mem"][mc * 128:(mc + 1) * 128, :], w=[("mt", mc)])
            for half in range(2):
                b = 6 + half
                for j in range(4):
                    c = half * 4 + j
                    TR(PS[b][:, j * 128:(j + 1) * 128], mt[mc][:, c * 128:(c + 1) * 128], ident[:], [("mt", mc), "ident"], [("ps", b)])
                vcopy(memT[:, half * 4:(half + 1) * 4, mc * 128:(mc + 1) * 128], PS[b][:].rearrange("p (j q) -> p j q", j=4), [("ps", b)], [("memT", mc)])
        mk = [("memT", 0), ("memT", 1)]
        Wkv = D["x_wkv"][l]
        for h in range(4):
            i = next_wbuf()
            wb = wbuf[i]
            DMA("gpsimd", wb[:, :, 0:128], Wkv[:, h * 128:(h + 1) * 128].rearrange("(c p) m -> p c m", p=128), w=[("wbuf", i, 0)])
            b = gbank()
            for c in range(8):
                MM(PS[b][:, 0:256], wb[:, c, 0:128], memT[:, c, :], c == 0, c == 7, [("wbuf", i, 0)] + mk, [("ps", b)])
            vcopy(KxT[h][:], PS[b][:, 0:256], [("ps", b)], [("KxT", h)])
        i = next_wbuf()
        wb = wbuf[i]
        DMA("gpsimd", wb[:, :, 0:512], Wkv[:, 512:1024].rearrange("(c p) m -> p c m", p=128), w=[("wbuf", i, 0)])
        for mc in range(2):
            b = gbank()
            for c in range(8):
                MM(PS[b][:], memT[:, c, mc * 128:(mc + 1) * 128], wb[:, c, 0:512], c == 0, c == 7, [("wbuf", i, 0)] + mk, [("ps", b)])
            vcopy(vxA[:, mc, :, 0:128], PS[b][:].rearrange("p (h d) -> p h d", h=4), [("ps", b)], [("vxA", mc)])
        for h in range(4):
            def evq(ps, key, tq, h=h):
                vcopy(QxT[h][:, tqs(tq)], ps[:, :], [key], [("QxT", h, tq)])
            proj_fm([(0, D["x_wq"][l][:, h * 128:(h + 1) * 128])], 128, evq)
        for h in range(4):
            for qt in range(4):
                accmap = lambda qs: (2 + qs // 2, (qs % 2) * 129)
                attn(lambda kc, h=h, qt=qt: [(KxT[h][:, kc * 128:(kc + 1) * 128], QxT[h][:, tqs(qt)], [("KxT", h), ("QxT", h, qt)])],
                     range(2), lambda kc, h=h: (vxA[:, kc, h, :], ("vxA", kc)), 129, lambda kc, qs: True, accmap, scale=128 ** -0.5)
                attn_out(accmap, 128, 4 * qt, None, lambda tt, h=h: ox[:, tt, h * 128:(h + 1) * 128], False)
        s.barrier()
        oxT = s.sb([128, 4, S], BF16, "oxT")
        to_fm(ox, 4, oxT, "oxT")
        wo = s.sb([128, 4, 1024], BF16, "xwo")
        lng_, lnb_ = s.sb([128, DM], F32, "lng"), s.sb([128, DM], F32, "lnb")
        LN["g"], LN["b"] = lng_, lnb_
        load_ln(l, "ln2_g", "ln2_b")
        DMA("gpsimd", wo[:], D["x_wo"][l].rearrange("(c p) m -> p c m", p=128), w=["xwo"])

        def p0(tt, xt, key, sl):
            DMA("sync", xt[:], xs[tt * 128:(tt + 1) * 128, :], w=[key])

        def p1(tt, xt, key, sl):
            for hf in range(2):
                b = 4 + hf
                for c in range(4):
                    MM(PS[b][:], oxT[:, c, tt * 128:(tt + 1) * 128], wo[:, c, hf * 512:(hf + 1) * 512], c == 0, c == 3,
                       ["xwo", ("oxT", tt)], [("ps", b)])
                V(lambda e, b=b, hf=hf: e.scalar_tensor_tensor(out=xt[:, hf * 512:(hf + 1) * 512], in0=xt[:, hf * 512:(hf + 1) * 512],
                                                              scalar=DN_ALPHA, in1=PS[b][:], op0=ALU.mult, op1=ALU.add), [key, ("ps", b)], [key])
        ln_run([p0, p1], xs)
        s.pop()

    def moe(l, dst, last):
        s.push()
        cwall = s.sb([128, 16, 32], F32, "cwall")
        ohall = s.sb([128, 16, 32], BF16, "ohall")
        idxs = nc.alloc_sbuf_tensor_at(f"idxs_{l}", [128, 16, 2], I32, offset=(s.sb_off + 63) // 64 * 64)
        s.sb_off = (s.sb_off + 63) // 64 * 64 + 128
        wts = s.sb([128, 16, 2], F32, "wts")
        s.push()
        wr = s.sb([128, 8, 36], F32, "wr")
        brow = s.sb([1, 36], F32, "brow")
        ones1 = s.sb([1, 128], F32, "ones1")
        onesb = s.sb([128, 128], BF16, "onesb")
        ltri = s.sb([128, 128], BF16, "ltri")
        ebase = s.sb([128, 32], F32, "ebase")
        V(lambda e: e.memset(ones1[:], 1.0), [], ["ones1"])
        V(lambda e: e.memset(onesb[:], 1.0), [], ["onesb"])
        DMA("gpsimd", ltri[:], D["c_ltri"], w=["ltri"])
        DMA("sync", ebase[:], D["c_ebase"], w=["ebase"])
        DMA("sync", wr[:, :, 0:4], D["moe_rg_w"][l].rearrange("(c p) m -> p c m", p=128), w=["wr0"])
        DMA("sync", wr[:, :, 4:36], D["moe_re_w"][l].rearrange("(c p) m -> p c m", p=128), w=["wr1"])
        DMA("sync", brow[:, 0:4], D["moe_rg_b"][l].rearrange("(o m) -> o m", o=1), w=["br0"])
        DMA("sync", brow[:, 4:36], D["moe_re_b"][l].rearrange("(o m) -> o m", o=1), w=["br1"])
        RB = (4, 5, 0, 1)
        RB2 = (2, 3, 4, 5)
        xt = [s.sb([128, DM], F32, f"rx{i}") for i in range(4)]
        xTfs = [s.sb([128, 8, 128], F32, f"xTf{i}") for i in range(4)]
        SC = [dict(lg=s.sb([128, 36], F32, "lg"), sm=s.sb([128, 16], F32, "sm"), gm=s.sb([128, 4], F32, "gm"), es=s.sb([128, 8], F32, "es"),
                   t8=s.sb([128, 8], F32, "t8"), ta=s.sb([128, 8], F32, "ta"), tb=s.sb([128, 8], F32, "tb"), rk=s.sb([128, 32], F32, "rk"),
                   vl=s.sb([128, 32], F32, "vl"), val=s.sb([128, 32], F32, "val"), fidx=s.sb([128, 4], F32, "fidx")) for _ in range(4)]

        def router_tile(tt, j):
            x_, xTf, bnk = xt[j], xTfs[j], RB[j]
            lg, sm, gm, es, t8, ta, tb = (SC[j][n_] for n_ in ("lg", "sm", "gm", "es", "t8", "ta", "tb"))
            key = ("rx", j)
            K_ = lambda n_: (n_, j)
            DMA("sync", x_[:], xs[tt * 128:(tt + 1) * 128, :], w=[key])
            yield
            for half in range(2):
                b = 6 + half
                for jj in range(4):
                    c = half * 4 + jj
                    TR(PS[b][:, jj * 128:(jj + 1) * 128], x_[:, c * 128:(c + 1) * 128], ident[:], [key, "ident"], [("ps", b)])
                vcopy(xTf[:, half * 4:(half + 1) * 4, :], PS[b][:].rearrange("p (j q) -> p j q", j=4), [("ps", b)], [("xTf", j, half)])
            yield
            for c in range(8):
                MM(PS[bnk][:, 0:36], xTf[:, c, :], wr[:, c, :], c == 0, False, [("xTf", j, c // 4), "wr0", "wr1"], [("ps", bnk)])
            MM(PS[bnk][:, 0:36], ones1[:, :], brow[:, :], False, True, ["ones1", "br0", "br1"], [("ps", bnk)])
            vcopy(lg[:], PS[bnk][:, 0:36], [("ps", bnk)], [K_("lg")])
            yield
            V(lambda e: e.tensor_reduce(out=sm[:, 0:1], in_=lg[:, 0:4], axis=AX.X, op=ALU.max), [K_("lg")], [K_("sm0")])
            yield
            V(lambda e: e.tensor_scalar(out=sm[:, 1:2], in0=sm[:, 0:1], scalar1=-1.0, scalar2=None, op0=ALU.mult), [K_("sm0")], [K_("sm1")])
            yield
            A(lambda e: e.activation(out=gm[:], in_=lg[:, 0:4], func=AF.Exp, bias=sm[:, 1:2]), [K_("lg"), K_("sm1")], [K_("gm")])
            yield
            V(lambda e: e.tensor_reduce(out=sm[:, 2:3], in_=gm[:], axis=AX.X, op=ALU.add), [K_("gm")], [K_("sm2")])
            yield
            V(lambda e: e.reciprocal(out=sm[:, 3:4], in_=sm[:, 2:3]), [K_("sm2")], [K_("sm3")])
            yield
            V(lambda e: e.tensor_scalar(out=gm[:], in0=lg[:, 0:4], scalar1=sm[:, 0:1], scalar2=None, op0=ALU.is_ge), [K_("lg"), K_("sm0"), K_("gm")], [K_("gm")])
            yield
            V(lambda e: e.tensor_scalar(out=es[:], in0=lg[:, 4:12], scalar1=gm[:, 0:1], scalar2=None, op0=ALU.mult), [K_("lg"), K_("gm")], [K_("es")])
            yield
            for g in range(1, 4):
                V(lambda e, g=g: e.scalar_tensor_tensor(out=es[:], in0=lg[:, 4 + 8 * g:12 + 8 * g], scalar=gm[:, g:g + 1], in1=es[:], op0=ALU.mult, op1=ALU.add),
                  [K_("lg"), K_("gm"), K_("es")], [K_("es")])
                yield
            V(lambda e: e.max(out=t8[:], in_=es[:]), [K_("es")], [K_("t8")])
            yield
            V(lambda e: e.tensor_tensor(out=sm[:, 4:5], in0=t8[:, 0:1], in1=t8[:, 1:2], op=ALU.subtract), [K_("t8")], [K_("sm4")])
            yield
            A(lambda e: e.activation(out=sm[:, 5:6], in_=sm[:, 4:5], func=AF.Sigmoid), [K_("sm4")], [K_("sm5")])
            yield
            V(lambda e: e.tensor_tensor(out=sm[:, 6:7], in0=sm[:, 5:6], in1=sm[:, 3:4], op=ALU.mult), [K_("sm5"), K_("sm3")], [K_("sm6")])
            yield
            V(lambda e: e.tensor_tensor(out=sm[:, 7:8], in0=sm[:, 3:4], in1=sm[:, 6:7], op=ALU.subtract), [K_("sm6"), K_("sm3")], [K_("sm7")])
            yield
            for g in range(4):
                eg = lg[:, 4 + 8 * g:12 + 8 * g]
                V(lambda e, eg=eg: e.tensor_scalar(out=ta[:], in0=eg, scalar1=t8[:, 0:1], scalar2=sm[:, 6:7], op0=ALU.is_equal, op1=ALU.mult),
                  [K_("lg"), K_("t8"), K_("sm6"), K_("ta")], [K_("ta")])
                yield
                V(lambda e, eg=eg: e.tensor_scalar(out=tb[:], in0=eg, scalar1=t8[:, 1:2], scalar2=sm[:, 7:8], op0=ALU.is_equal, op1=ALU.mult),
                  [K_("lg"), K_("t8"), K_("sm7"), K_("tb")], [K_("tb")])
                yield
                V(lambda e: e.tensor_tensor(out=ta[:], in0=ta[:], in1=tb[:], op=ALU.add), [K_("ta"), K_("tb")], [K_("ta")])
                yield
                V(lambda e, g=g: e.tensor_scalar(out=cwall[:, tt, 8 * g:8 * g + 8], in0=ta[:], scalar1=gm[:, g:g + 1], scalar2=None, op0=ALU.mult),
                  [K_("ta"), K_("gm")], [("cw", tt, g)])
                yield
            V(lambda e: e.tensor_scalar(out=ohall[:, tt, :], in0=cwall[:, tt, :], scalar1=0.0, scalar2=None, op0=ALU.is_gt),
              [("cw", tt, g) for g in range(4)], [("oh", tt)])
            yield

        for base in range(0, 16, 4):
            interleave([router_tile(base + j, j) for j in range(4)])

        def rank_tile(tt, j):
            x_, bnk = xt[j], RB2[j]
            rk, vl, val, fidx = (SC[j][n_] for n_ in ("rk", "vl", "val", "fidx"))
            key = ("rx", j)
            K_ = lambda n_: (n_, j)
            DMA("sync", x_[:], xs[tt * 128:(tt + 1) * 128, :], w=[key])
            for t2 in range(tt):
                MM(PS[bnk][:, 0:32], onesb[:, :], ohall[:, t2, :], t2 == 0, False, ["onesb", ("oh", t2)], [("ps", bnk)])
            MM(PS[bnk][:, 0:32], ltri[:, :], ohall[:, tt, :], tt == 0, True, ["ltri", ("oh", tt)], [("ps", bnk)])
            vcopy(rk[:], PS[bnk][:, 0:32], [("ps", bnk)], [K_("rk")])
            yield
            V(lambda e: e.tensor_scalar(out=vl[:], in0=rk[:], scalar1=float(CAP) - 0.5, scalar2=None, op0=ALU.is_lt), [K_("rk")], [K_("vl")])
            yield
            V(lambda e: e.tensor_tensor(out=vl[:], in0=vl[:], in1=ohall[:, tt, :], op=ALU.mult), [K_("vl"), ("oh", tt)], [K_("vl")])
            yield
            V(lambda e: e.tensor_tensor(out=cwall[:, tt, :], in0=cwall[:, tt, :], in1=vl[:], op=ALU.mult), [K_("vl")] + [("cw", tt, g) for g in range(4)], [("cwv", tt)])
            yield
            V(lambda e: e.tensor_tensor(out=val[:], in0=rk[:], in1=ebase[:], op=ALU.add), [K_("rk"), "ebase"], [K_("val")])
            yield
            V(lambda e: e.tensor_tensor(out=val[:], in0=val[:], in1=vl[:], op=ALU.mult), [K_("val"), K_("vl")], [K_("val")])
            yield
            V(lambda e: e.tensor_reduce(out=fidx[:, 1:2], in_=val[:], axis=AX.X, op=ALU.max), [K_("val")], [K_("f1")])
            yield
            V(lambda e: e.tensor_reduce(out=fidx[:, 0:1], in_=val[:], axis=AX.X, op=ALU.add), [K_("val")], [K_("f0")])
            yield
            V(lambda e: e.tensor_tensor(out=fidx[:, 0:1], in0=fidx[:, 0:1], in1=fidx[:, 1:2], op=ALU.subtract), [K_("f0"), K_("f1")], [K_("f0")])
            yield
            V(lambda e: e.tensor_scalar(out=rk[:], in0=val[:], scalar1=fidx[:, 1:2], scalar2=None, op0=ALU.is_equal), [K_("val"), K_("f1"), K_("rk")], [K_("rk")])
            yield
            V(lambda e: e.tensor_tensor(out=rk[:], in0=rk[:], in1=cwall[:, tt, :], op=ALU.mult), [K_("rk"), ("cwv", tt)], [K_("rk")])
            yield
            V(lambda e: e.tensor_reduce(out=wts[:, tt, 1:2], in_=rk[:], axis=AX.X, op=ALU.add), [K_("rk")], [("wts", tt, 1)])
            yield
            V(lambda e: e.tensor_reduce(out=wts[:, tt, 0:1], in_=cwall[:, tt, :], axis=AX.X, op=ALU.add), [("cwv", tt)], [("wts", tt, 0)])
            yield
            V(lambda e: e.tensor_tensor(out=wts[:, tt, 0:1], in0=wts[:, tt, 0:1], in1=wts[:, tt, 1:2], op=ALU.subtract),
              [("wts", tt, 0), ("wts", tt, 1)], [("wts", tt, 0)])
            yield
            V(lambda e: e.tensor_scalar(out=fidx[:, 2:4], in0=fidx[:, 0:2], scalar1=0.5, scalar2=1.0e6, op0=ALU.is_lt, op1=ALU.mult),
              [K_("f0"), K_("f1")], [K_("f2")])
            yield
            V(lambda e: e.scalar_tensor_tensor(out=fidx[:, 2:4], in0=fidx[:, 0:2], scalar=-1.0, in1=fidx[:, 2:4], op0=ALU.add, op1=ALU.add),
              [K_("f0"), K_("f1"), K_("f2")], [K_("f2")])
            yield
            V(lambda e: e.tensor_copy(out=idxs[:, tt, :], in_=fidx[:, 2:4]), [K_("f2")], [("idx", tt)])
            yield
            for k_ in range(2):
                s.op("gpsimd", lambda e, k_=k_: e.indirect_dma_start(
                    out=xg[:, :], out_offset=bass.IndirectOffsetOnAxis(ap=idxs[:, tt, k_:k_ + 1], axis=0), in_=x_[:, :], in_offset=None,
                    bounds_check=bc_reg, oob_is_err=False), [key, ("idx", tt)], [("xgs", tt, k_)], dma=True)
            yield

        for base in range(0, 16, 4):
            interleave([rank_tile(base + j, j) for j in range(4)])
        s.pop()
        s.push()
        NJ = CAP // 128
        wgu = [s.sb([128, 8, 1024], BF16, f"wgu{i}") for i in range(2)]
        wd = [s.sb([128, 4, 1024], BF16, f"wd{i}") for i in range(2)]
        xgt = [s.sb([128, NJ, DM], F32, f"xgt{i}") for i in range(2)]
        xgT = [s.sb([128, 8, CAP], BF16, f"xgT{i}") for i in range(2)]
        actT = [s.sb([128, 4, CAP], BF16, f"actT{i}") for i in range(2)]
        sl = [s.sb([128, CAP], F32, f"sl{i}") for i in range(2)]
        yrow = [s.sb([128, DM], F32, f"yrow{i}") for i in range(2)]
        kk = {"k": 0}

        def ex_load(ee):
            wi = ee % 2
            DMA("gpsimd", wgu[wi][:], D["moe_w_gu"][l, ee].rearrange("(c p) m -> p c m", p=128), w=[("wgu", wi)])
            DMA("gpsimd", wd[wi][:], D["moe_w_down"][l, ee].rearrange("(c p) m -> p c m", p=128), w=[("wd", wi)])
            DMA("sync", xgt[wi][:], xg[ee * CAP:(ee + 1) * CAP, :].rearrange("(j p) d -> p j d", p=128), w=[("xgt", wi)])
            for j in range(NJ):
                for half in range(2):
                    b = 6 + half
                    for jj in range(4):
                        c = half * 4 + jj
                        TR(PS[b][:, jj * 128:(jj + 1) * 128], xgt[wi][:, j, c * 128:(c + 1) * 128], ident[:], [("xgt", wi), "ident"], [("ps", b)])
                    vcopy(xgT[wi][:, half * 4:(half + 1) * 4, j * 128:(j + 1) * 128], PS[b][:].rearrange("p (j q) -> p j q", j=4),
                          [("ps", b)], [("xgT", wi, j, half)])

        def ex_gu(ee):
            wi = ee % 2
            xk = [("xgT", wi, j, half) for j in range(NJ) for half in range(2)]
            for fc in range(4):
                kk["k"] += 1
                k = kk["k"]
                bg = 0 + k % 2
                bu = 2 + k % 2
                for c in range(8):
                    MM(PS[bg][:, 0:CAP], wgu[wi][:, c, fc * 128:(fc + 1) * 128], xgT[wi][:, c, :], c == 0, c == 7, [("wgu", wi)] + xk, [("ps", bg)])
                for c in range(8):
                    MM(PS[bu][:, 0:CAP], wgu[wi][:, c, 512 + fc * 128:512 + (fc + 1) * 128], xgT[wi][:, c, :], c == 0, c == 7, [("wgu", wi)] + xk, [("ps", bu)])
                A(lambda e, bg=bg, k=k: e.activation(out=sl[k % 2][:], in_=PS[bg][:, 0:CAP], func=AF.Silu), [("ps", bg)], [("sl", k % 2)])
                V(lambda e, bu=bu, k=k, fc=fc: e.tensor_tensor(out=actT[ee % 2][:, fc, :], in0=sl[k % 2][:], in1=PS[bu][:, 0:CAP], op=ALU.mult),
                  [("sl", k % 2), ("ps", bu)], [("actT", ee % 2, fc)])

        def ex_down(ee):
            wi = ee % 2
            for j in range(NJ):
                yr = yrow[(ee * NJ + j) % 2]
                yk = ("yrow", (ee * NJ + j) % 2)
                for hf in range(2):
                    b = 4 + hf
                    for fc in range(4):
                        MM(PS[b][:], actT[ee % 2][:, fc, j * 128:(j + 1) * 128], wd[wi][:, fc, hf * 512:(hf + 1) * 512], fc == 0, fc == 3,
                           [("wd", wi), ("actT", ee % 2, fc)], [("ps", b)])
                    if hf == 0:
                        vcopy(yr[:, 0:512], PS[b][:], [("ps", b)], [yk])
                    else:
                        acopy(yr[:, 512:1024], PS[b][:], [("ps", b)], [yk])
                DMA("sync", yg[ee * CAP + j * 128:ee * CAP + (j + 1) * 128, :], yr[:], r=[yk], w=[("ygs", ee, j)])

        ex_load(0)
        for ee in range(32):
            ex_gu(ee)
            if ee + 1 < 32:
                ex_load(ee + 1)
            ex_down(ee)
        s.pop()
        gA = [s.sb([128, DM], F32, f"gA{i}") for i in range(NBUF)]
        gB = [s.sb([128, DM], F32, f"gB{i}") for i in range(NBUF)]
        lng_, lnb_ = s.sb([128, DM], F32, "lng"), s.sb([128, DM], F32, "lnb")
        LN["g"], LN["b"] = lng_, lnb_
        load_ln(l, "ln3_g", "ln3_b")
        for i in range(NBUF):
            V(lambda e, i=i: e.memset(gA[i][:], 0.0), [], [("gA", i)])
            V(lambda e, i=i: e.memset(gB[i][:], 0.0), [], [("gB", i)])

        def p0(tt, xt_, key, sl):
            DMA("sync", xt_[:], xs[tt * 128:(tt + 1) * 128, :], w=[key])
            for k_, gt, gk in ((0, gA[sl], ("gA", sl)), (1, gB[sl], ("gB", sl))):
                s.op("gpsimd", lambda e, tt=tt, k_=k_, gt=gt: e.indirect_dma_start(
                    out=gt[:, :], out_offset=None, in_=yg[:, :], in_offset=bass.IndirectOffsetOnAxis(ap=idxs[:, tt, k_:k_ + 1], axis=0),
                    bounds_check=bc_reg, oob_is_err=False), [("idx", tt)], [gk], dma=True)

        def p1(tt, xt_, key, sl):
            ga, gb = gA[sl], gB[sl]
            V(lambda e: e.tensor_scalar(out=ga[:], in0=ga[:], scalar1=wts[:, tt, 0:1], scalar2=None, op0=ALU.mult),
              [("gA", sl), ("wts", tt, 0)], [("gA", sl)])
            V(lambda e: e.scalar_tensor_tensor(out=ga[:], in0=gb[:], scalar=wts[:, tt, 1:2], in1=ga[:], op0=ALU.mult, op1=ALU.add),
              [("gA", sl), ("gB", sl), ("wts", tt, 1)], [("gA", sl)])
            V(lambda e: e.scalar_tensor_tensor(out=xt_[:], in0=xt_[:], scalar=DN_ALPHA, in1=ga[:], op0=ALU.mult, op1=ALU.add),
              [key, ("gA", sl)], [key])
        ln_run([p0, p1], dst, do_T=not last)
        s.pop()

    def mixer(l, xsrc):
        W = D["w_in"][l]
        s.push()
        oT = s.sb([128, 4, 2, S], BF16, "oT")
        s.push()
        oacc = s.sb([128, 16, 256], F32, "oacc")
        AT["P"] = [s.sb([128, 512], BF16, f"P{i}") for i in range(3)]
        AT["rd"] = s.sb([128, 4, 4], F32, "rd")
        nsa(l, W, oacc)
        to_fm(oacc, 2, oT[:, 0], ("oT", 0))
        sbranch(l, W, oacc)
        to_fm(oacc, 2, oT[:, 1], ("oT", 1))
        rglru(l, W, oT)
        mla(l, W, oacc)
        to_fm(oacc, 2, oT[:, 3], ("oT", 3))
        s.pop()
        if l == 0:
            dump_sb("oT", oT[:], [128, 4, 2, S])
        merge_ln1(l, W, oT, xsrc)
        s.pop()

    s.push()
    zt = s.sb([128, DM], F32, "zt")
    V(lambda e: e.memset(zt[:], 0.0), [], ["zt"])
    for r in range(NSL // 128):
        DMA("sync", xg[r * 128:(r + 1) * 128, :], zt[:], r=["zt"], w=[("xgz", r)])
    s.pop()
    load_xT(D["x"])
    for l in range(NL):
        s.push()
        wbuf = [s.sb([128, 8, 512], BF16, f"wbuf{i}") for i in range(3)]
        masks = s.sb([128, 12, 512], BF16, "masks")
        DMA("gpsimd", masks[:], D["c_masks"], w=["masks"])
        if "mix" in stages:
            mixer(l, D["x"] if l == 0 else xs)
        if "cross" in stages:
            cross(l)
        s.pop()
        if "moe" in stages:
            last = l == NL - 1
            moe(l, out_d if last else xs, last)
    if "moe" not in stages:
        s.barrier()
        s.push()
        tmp = s.sb([128, DM], F32, "fin")
        for tt in range(16):
            DMA("sync", tmp[:], xs[tt * 128:(tt + 1) * 128, :], w=["fin"])
            DMA("sync", out_d[tt * 128:(tt + 1) * 128, :], tmp[:], r=["fin"])
        s.pop()
    s.barrier()
    s.emit()
    return nc, dumps


_CACHE = {}


def kernel(**inputs):
    NL = 4
    if "nc" not in _CACHE:
        _CACHE["nc"] = build(NL)[0]
    nc = _CACHE["nc"]
    HC = host_consts()
    shared = {n: np.ascontiguousarray(np.asarray(inputs[n], dtype=np.float32)) for n in WNAMES}
    for n, a in HC.items():
        shared["c_" + n] = a
    x = np.asarray(inputs["x"], dtype=np.float32)
    mem = np.asarray(inputs["mem"], dtype=np.float32)
    in_maps = []
    for b in range(8):
        m = dict(shared)
        m["x"] = np.ascontiguousarray(x[b])
        m["mem"] = np.ascontiguousarray(mem[b])
        in_maps.append(m)
    res = run_bass_kernel_spmd(nc, in_maps, core_ids=list(range(8)))
    return np.stack([np.asarray(r["out"], dtype=np.float32) for r in res.results], axis=0)
```
